# Optimizing a Trainium2 kernel written in Bass

```python
import math
import jax, jax.numpy as jnp
from jax import lax
import numpy as np

D_MODEL = 1024
BATCH = 8
SEQ = 4096
DEPTH = 4

N_HEADS = 16
HEAD_DIM = D_MODEL // N_HEADS
N_A_LAYERS = DEPTH // 2
N_B_LAYERS = DEPTH - N_A_LAYERS
MOBA_BLOCK = 256
MOBA_TOPK = 3
Q_BLOCK = 128
N_BUCKETS = 32
MAX_DISTANCE = 1024
D_FF = 2816
N_EXPERTS = 8
TOP_K_EXPERTS = 2
D_FF_EXPERT = 3584
N_DENSE = (DEPTH + 1) // 2
N_MOE = DEPTH // 2
EPS = 1e-6
NEG = -1e30

kernel_name = "yoco_moba_stickbreaking_moe_trunk"


def rmsnorm(x, g):
    xf = x.astype(jnp.float32)
    y = xf * lax.rsqrt(jnp.mean(xf * xf, axis=-1, keepdims=True) + EPS)
    return (y * g.astype(jnp.float32)).astype(x.dtype)


def modulate(h, shift, scale):
    return h * (1.0 + scale[:, None, :]) + shift[:, None, :]


def rel_bucket(dist):
    dist = jnp.maximum(dist, 0)
    max_exact = N_BUCKETS // 2
    d = jnp.maximum(dist, 1).astype(jnp.float32)
    large = max_exact + (jnp.log(d / max_exact) / math.log(MAX_DISTANCE / max_exact)
                         * (N_BUCKETS - max_exact)).astype(jnp.int32)
    large = jnp.minimum(large, N_BUCKETS - 1)
    return jnp.where(dist < max_exact, dist, large)


def moba_attend(q, k, v, rel_bias):
    H, S, dh = q.shape
    nb = -(-S // MOBA_BLOCK)
    pad = nb * MOBA_BLOCK - S
    kp = jnp.pad(k, ((0, 0), (0, pad), (0, 0)))
    vp = jnp.pad(v, ((0, 0), (0, pad), (0, 0)))
    kb = kp.reshape(H, nb, MOBA_BLOCK, dh)
    vb = vp.reshape(H, nb, MOBA_BLOCK, dh)
    kmean = jnp.mean(kb, axis=2)
    bias_t = rel_bias.T
    scale = HEAD_DIM ** -0.5
    topk = min(MOBA_TOPK, nb)
    h_idx = jnp.arange(H)[:, None, None]
    blk_ids = jnp.arange(nb)
    offs = jnp.arange(MOBA_BLOCK)

    def per_block(i):
        q0 = i * Q_BLOCK
        qi = lax.dynamic_slice_in_dim(q, q0, Q_BLOCK, axis=1)
        qpos = q0 + jnp.arange(Q_BLOCK)
        cur = q0 // MOBA_BLOCK
        gate = jnp.einsum('hqd,hnd->hqn', qi, kmean).astype(jnp.float32)
        gate = jnp.where(blk_ids[None, None, :] < cur, gate, NEG)
        _, sel = lax.top_k(gate, topk)
        valid = sel < cur
        kg = kb[h_idx, sel]
        vg = vb[h_idx, sel]
        kpos_sel = sel[..., None] * MOBA_BLOCK + offs
        bucket_sel = rel_bucket(qpos[None, :, None, None] - kpos_sel)
        b_sel = jnp.take_along_axis(bias_t, bucket_sel.reshape(H, -1), axis=1).reshape(kpos_sel.shape)
        s_sel = jnp.einsum('hqd,hqjkd->hqjk', qi, kg).astype(jnp.float32) * scale + b_sel
        s_sel = jnp.where(valid[..., None], s_sel, NEG).reshape(H, Q_BLOCK, topk * MOBA_BLOCK)
        k0 = cur * MOBA_BLOCK
        ko = lax.dynamic_slice_in_dim(kp, k0, MOBA_BLOCK, axis=1)
        vo = lax.dynamic_slice_in_dim(vp, k0, MOBA_BLOCK, axis=1)
        d_own = qpos[:, None] - (k0 + offs)[None, :]
        b_own = jnp.take(bias_t, rel_bucket(d_own), axis=1)
        s_own = jnp.einsum('hqd,hkd->hqk', qi, ko).astype(jnp.float32) * scale + b_own
        s_own = jnp.where(d_own[None] >= 0, s_own, NEG)
        p = jax.nn.softmax(jnp.concatenate([s_sel, s_own], axis=-1), axis=-1)
        p_sel = p[..., :topk * MOBA_BLOCK].reshape(H, Q_BLOCK, topk, MOBA_BLOCK).astype(v.dtype)
        p_own = p[..., topk * MOBA_BLOCK:].astype(v.dtype)
        return (jnp.einsum('hqjk,hqjkd->hqd', p_sel, vg)
                + jnp.einsum('hqk,hkd->hqd', p_own, vo))

    outs = lax.map(per_block, jnp.arange(S // Q_BLOCK))
    return outs.transpose(1, 0, 2, 3).reshape(H, S, dh)


def stick_breaking(q, k, v):
    H, S, dh = q.shape
    scale = HEAD_DIM ** -0.5
    kpos = jnp.arange(S)

    def per_block(i):
        q0 = i * Q_BLOCK
        qi = lax.dynamic_slice_in_dim(q, q0, Q_BLOCK, axis=1)
        qpos = q0 + jnp.arange(Q_BLOCK)
        z = jnp.einsum('hqd,hkd->hqk', qi, k).astype(jnp.float32) * scale
        causal = (kpos[None, :] < qpos[:, None])[None]
        log_beta = jax.nn.log_sigmoid(z)
        log_1mb = jnp.where(causal, jax.nn.log_sigmoid(-z), 0.0)
        tail = lax.cumsum(log_1mb, axis=2, reverse=True) - log_1mb
        a = jnp.where(causal, jnp.exp(log_beta + tail), 0.0)
        return jnp.einsum('hqk,hkd->hqd', a.astype(v.dtype), v)

    outs = lax.map(per_block, jnp.arange(S // Q_BLOCK))
    return outs.transpose(1, 0, 2, 3).reshape(H, S, dh)


def split_heads(t):
    B, S, _ = t.shape
    return t.reshape(B, S, N_HEADS, HEAD_DIM).transpose(0, 2, 1, 3)


def merge_heads(t):
    B, H, S, dh = t.shape
    return t.transpose(0, 2, 1, 3).reshape(B, S, H * dh)


def swiglu(t, w13, w2):
    g, u = jnp.split(t @ w13, 2, axis=-1)
    return (jax.nn.silu(g) * u) @ w2


def moe_ffn(h, router_w, w13, w2):
    B, S, D = h.shape
    t = h.reshape(B * S, D)
    logits = (t @ router_w).astype(jnp.float32)
    top_v, top_i = lax.top_k(logits, TOP_K_EXPERTS)
    w = jax.nn.softmax(top_v, axis=-1)
    combine = jnp.sum(jax.nn.one_hot(top_i, N_EXPERTS, dtype=jnp.float32) * w[..., None], axis=1)
    out = jnp.zeros_like(t)
    for e in range(N_EXPERTS):
        out = out + combine[:, e:e + 1].astype(t.dtype) * swiglu(t, w13[e], w2[e])
    return out.reshape(B, S, D)


def setup_inputs(seed: int = 0) -> dict:
    key = jax.random.key(seed)
    ks = jax.random.split(key, 24)
    D = D_MODEL

    def nrm(k, shape, s):
        return jax.random.normal(k, shape, jnp.float32) * s

    return {
        "x": nrm(ks[0], (BATCH, SEQ, D), 1.0),
        "c": nrm(ks[1], (BATCH, D), 1.0),
        "rel_bias": nrm(ks[2], (N_BUCKETS, N_HEADS), 0.5),
        "mod_w": nrm(ks[3], (DEPTH, D, 6 * D), 0.5 * D ** -0.5),
        "mod_b": nrm(ks[4], (DEPTH, 6 * D), 0.02),
        "norm_mix_g": 1.0 + nrm(ks[5], (DEPTH, D), 0.02),
        "norm_ffn_g": 1.0 + nrm(ks[6], (DEPTH, D), 0.02),
        "a_wqkv": nrm(ks[7], (N_A_LAYERS, D, 3 * D), D ** -0.5),
        "a_wo": nrm(ks[8], (N_A_LAYERS, D, D), D ** -0.5),
        "kv_norm_g": 1.0 + nrm(ks[9], (D,), 0.02),
        "kv_mod_w": nrm(ks[10], (D, 2 * D), 0.5 * D ** -0.5),
        "kv_mod_b": nrm(ks[11], (2 * D,), 0.02),
        "b_wkv": nrm(ks[12], (D, 2 * D), D ** -0.5),
        "b_wq": nrm(ks[13], (N_B_LAYERS, D, D), D ** -0.5),
        "b_wo": nrm(ks[14], (N_B_LAYERS, D, D), D ** -0.5),
        "ffn_w13": nrm(ks[15], (N_DENSE, D, 2 * D_FF), D ** -0.5),
        "ffn_w2": nrm(ks[16], (N_DENSE, D_FF, D), D_FF ** -0.5),
        "router_w": nrm(ks[17], (N_MOE, D, N_EXPERTS), D ** -0.5),
        "moe_w13": nrm(ks[18], (N_MOE, N_EXPERTS, D, 2 * D_FF_EXPERT), D ** -0.5),
        "moe_w2": nrm(ks[19], (N_MOE, N_EXPERTS, D_FF_EXPERT, D), D_FF_EXPERT ** -0.5),
        "final_norm_g": 1.0 + nrm(ks[20], (D,), 0.02),
    }


def reference(x, c, rel_bias, mod_w, mod_b, norm_mix_g, norm_ffn_g, a_wqkv, a_wo,
              kv_norm_g, kv_mod_w, kv_mod_b, b_wkv, b_wq, b_wo, ffn_w13, ffn_w2,
              router_w, moe_w13, moe_w2, final_norm_g):
    B, S, D = x.shape
    silu_c = jax.nn.silu(c)
    k_sh = None
    v_sh = None
    for l in range(DEPTH):
        mod = silu_c @ mod_w[l] + mod_b[l]
        sh_a, sc_a, g_a, sh_f, sc_f, g_f = jnp.split(mod, 6, axis=-1)
        h = modulate(rmsnorm(x, norm_mix_g[l]), sh_a, sc_a)
        if l < N_A_LAYERS:
            q, k, v = jnp.split(h @ a_wqkv[l], 3, axis=-1)
            attn = lax.map(lambda a: moba_attend(a[0], a[1], a[2], rel_bias),
                           (split_heads(q), split_heads(k), split_heads(v)))
            y = merge_heads(attn) @ a_wo[l]
        else:
            j = l - N_A_LAYERS
            if j == 0:
                kv_mod = silu_c @ kv_mod_w + kv_mod_b
                kv_sh, kv_sc = jnp.split(kv_mod, 2, axis=-1)
                hk = modulate(rmsnorm(x, kv_norm_g), kv_sh, kv_sc)
                k_s, v_s = jnp.split(hk @ b_wkv, 2, axis=-1)
                k_sh = split_heads(k_s)
                v_sh = split_heads(v_s)
            q = split_heads(h @ b_wq[j])
            attn = lax.map(lambda a: stick_breaking(a[0], a[1], a[2]), (q, k_sh, v_sh))
            y = merge_heads(attn) @ b_wo[j]
        x = x + g_a[:, None, :] * y
        h = modulate(rmsnorm(x, norm_ffn_g[l]), sh_f, sc_f)
        if l % 2 == 0:
            y = swiglu(h, ffn_w13[l // 2], ffn_w2[l // 2])
        else:
            y = moe_ffn(h, router_w[l // 2], moe_w13[l // 2], moe_w2[l // 2])
        x = x + g_f[:, None, :] * y
    return rmsnorm(x, final_norm_g)
```

```python
import bisect
import math
from contextlib import ExitStack
import numpy as np
import ml_dtypes
import concourse.bass as bass
import concourse.mybir as mybir
from concourse.bass_utils import run_bass_kernel_spmd

F32 = mybir.dt.float32
BF16 = mybir.dt.bfloat16
ALU = mybir.AluOpType
AF = mybir.ActivationFunctionType
AX = mybir.AxisListType
SEM_ROT = 30000
BIG = 1.0e30
EPS = 1e-6

CFG_FULL = dict(S=4096, D=1024, H=16, FF=2816, FE=3584, E=8, TS=1024)


class Ev:
    __slots__ = ("eng", "idx", "ins", "sem", "val", "slot", "kind", "waited")

    def __init__(self, eng, idx, ins):
        self.eng, self.idx, self.ins = eng, idx, ins
        self.sem = None
        self.val = None
        self.slot = None
        self.kind = None
        self.waited = False


class Eng:
    def __init__(self, name, e):
        self.name, self.e = name, e
        self.n = 0
        self.sem = None
        self.cnt = 0
        self.mat_idx = []
        self.mat_ev = []
        self.seen = {}
        self.last = None


class Slot:
    def __init__(self, name, t=None):
        self.name = name
        self.t = t
        self.w = None
        self.r = {}
        self.sems = {}
        self.lastd = {}


class Ctx:
    def __init__(self):
        self.nc = bass.Bass("TRN2", target_bir_lowering=False)
        nc = self.nc
        self.root = ExitStack()
        self.stacks = [self.root]
        self.engs = {
            "pe": Eng("pe", nc.tensor),
            "act": Eng("act", nc.scalar),
            "dve": Eng("dve", nc.vector),
            "pool": Eng("pool", nc.gpsimd),
            "sp": Eng("sp", nc.sync),
        }
        self.nsem = 0
        self.nname = 0
        self.slots = []
        self.phase_slots = [[]]
        self.sempool = {"sp": [], "pool": [], "act": []}
        self.ninstr = 0

    def new_sem(self, name):
        self.nsem += 1
        return self.root.enter_context(self.nc.semaphore(f"{name}_{self.nsem}"))

    def _reg(self, s):
        self.slots.append(s)
        self.phase_slots[-1].append(s)
        return s

    def sbuf(self, name, shape, dt):
        self.nname += 1
        t = self.stacks[-1].enter_context(self.nc.sbuf_tensor(f"{name}_{self.nname}", list(shape), dt))
        return self._reg(Slot(name, t))

    def psum(self, name, shape, dt=F32):
        self.nname += 1
        t = self.stacks[-1].enter_context(self.nc.psum_tensor(f"{name}_{self.nname}", list(shape), dt))
        return self._reg(Slot(name, t))

    def vslot(self, name):
        return self._reg(Slot(name, None))

    def dram(self, name, shape, dt, kind="Internal"):
        return self.nc.dram_tensor(name, list(shape), dt, kind=kind)

    def begin(self):
        self.stacks.append(ExitStack())
        self.phase_slots.append([])

    def end(self):
        self.barrier()
        for s in self.phase_slots.pop():
            for (d_, q_), sc in s.sems.items():
                self.sempool[q_].append(sc)
            self.slots.remove(s)
        self.stacks.pop().close()

    def _getsem(self, q):
        if self.sempool[q]:
            return self.sempool[q].pop(0)
        return [self.new_sem("d" + q), 0]

    def _materialize(self, ev):
        if ev.val is not None:
            return
        eng = ev.eng
        i = bisect.bisect_left(eng.mat_idx, ev.idx)
        if i < len(eng.mat_idx):
            o = eng.mat_ev[i]
            ev.sem, ev.val = o.sem, o.val
            return
        if eng.sem is None or eng.cnt >= SEM_ROT:
            eng.sem = self.new_sem("p" + eng.name)
            eng.cnt = 0
        eng.cnt += 1
        ev.ins.then_inc(eng.sem, 1)
        ev.sem, ev.val = eng.sem, eng.cnt
        eng.mat_idx.append(ev.idx)
        eng.mat_ev.append(ev)

    def _wait(self, eng, ev):
        if ev.kind == "dma":
            ev.waited = True
        else:
            self._materialize(ev)
        k = id(ev.sem)
        if eng.seen.get(k, 0) >= ev.val:
            return
        eng.seen[k] = ev.val
        eng.e.wait_ge(ev.sem, ev.val)

    def _deps(self, eng, reads, writes, is_dma):
        deps = []
        for s in reads:
            if s.w is not None:
                if is_dma or s.w.kind == "dma" or s.w.eng is not eng or eng.name != "pe":
                    deps.append(s.w)
        for s in writes:
            if s.w is not None and (is_dma or s.w.kind == "dma" or s.w.eng is not eng or eng.name != "pe"):
                deps.append(s.w)
            for r in s.r.values():
                if is_dma or r.kind == "dma" or r.eng is not eng or eng.name != "pe":
                    deps.append(r)
        return deps

    def op(self, engname, fn, reads=(), writes=()):
        eng = self.engs[engname]
        for d in self._deps(eng, reads, writes, False):
            self._wait(eng, d)
        ins = fn(eng.e)
        self.ninstr += 1
        ev = Ev(eng, eng.n, ins)
        eng.n += 1
        eng.last = ev
        for s in reads:
            s.r[engname] = ev
        for s in writes:
            s.w = ev
            s.r = {}
        return ev

    def dma(self, qname, pairs, reads=(), writes=(), **kw):
        eng = self.engs[qname]
        for d in self._deps(eng, reads, writes, True):
            self._wait(eng, d)
        s = writes[0] if writes else reads[0]
        key = ("w" if writes else "r", qname)
        if key not in s.sems:
            s.sems[key] = self._getsem(qname)
        sc = s.sems[key]
        prev = s.lastd.get(key)
        sc[1] += 16 * len(pairs)
        if prev is not None and not prev.waited:
            prev.val = sc[1]
        ins = None
        for (o, i) in pairs:
            ins = eng.e.dma_start(out=o, in_=i, **kw).then_inc(sc[0], 16)
            self.ninstr += 1
        ev = Ev(eng, -1, ins)
        ev.kind = "dma"
        ev.sem, ev.val, ev.slot = sc[0], sc[1], s
        s.lastd[key] = ev
        for x in reads:
            x.r["dma"] = ev
        for x in writes:
            x.w = ev
            x.r = {}
        return ev

    def barrier(self):
        evs = [e.last for e in self.engs.values() if e.last is not None]
        dm = []
        for s in self.slots:
            dm.extend(s.sems.values())
        for ev in evs:
            self._materialize(ev)
        for e in self.engs.values():
            for ev in evs:
                if ev.eng is e:
                    continue
                k = id(ev.sem)
                if e.seen.get(k, 0) < ev.val:
                    e.seen[k] = ev.val
                    e.e.wait_ge(ev.sem, ev.val)
            for (sem, val) in dm:
                k = id(sem)
                if val > 0 and e.seen.get(k, 0) < val:
                    e.seen[k] = val
                    e.e.wait_ge(sem, val)
        for s in self.slots:
            s.w = None
            s.r = {}
            for ev in s.lastd.values():
                ev.waited = True


class Rot:
    def __init__(self, items):
        self.items = items
        self.i = 0

    def next(self):
        x = self.items[self.i % len(self.items)]
        self.i += 1
        return x


def rel_bucket_table(nu):
    import jax
    import jax.numpy as jnp
    with jax.default_device(jax.devices("cpu")[0]):
        dist = jnp.arange(nu, dtype=jnp.int32) - 512
        distc = jnp.maximum(dist, 0)
        max_exact = 16
        d = jnp.maximum(distc, 1).astype(jnp.float32)
        large = max_exact + (jnp.log(d / max_exact) / math.log(1024 / max_exact) * (32 - max_exact)).astype(jnp.int32)
        large = jnp.minimum(large, 31)
        b = np.asarray(jnp.where(distc < max_exact, distc, large))
        dist = np.asarray(dist)
    oh = np.zeros((33, nu), np.float32)
    for u in range(nu):
        if dist[u] < 0:
            oh[32, u] = 1.0
        else:
            oh[b[u], u] = 1.0
    return oh


def make_consts(cfg):
    S, H = cfg["S"], cfg["H"]
    NU = max(S + 512, 2048)
    NB = S // 256
    cs = {}
    cs["identb"] = np.eye(128).astype(ml_dtypes.bfloat16)
    cs["identf"] = np.eye(128, dtype=np.float32)
    cs["oh"] = rel_bucket_table(NU)
    cs["jex"] = np.ascontiguousarray(np.eye(128, dtype=np.float32)[::-1])
    es = np.zeros((16, 16 * 128), np.float32)
    for n in range(16):
        es[n, n * 128:(n + 1) * 128] = 1.0
    cs["esel"] = es.astype(ml_dtypes.bfloat16)
    e2 = np.zeros((16, S), np.float32)
    for n in range(min(16, S // 256)):
        e2[n, n * 256:(n + 1) * 256] = 1.0
    cs["esel2"] = e2.astype(ml_dtypes.bfloat16)
    j = np.arange(128)[:, None]
    k = np.arange(128)[None, :]
    cs["ustrict"] = (j > k).astype(np.float32).astype(ml_dtypes.bfloat16)
    m = np.arange(512 + 384)[None, :]
    kk = np.arange(128)[:, None]
    cs["cmask"] = ((m - 384 - kk) > 0).astype(np.float32).astype(ml_dtypes.bfloat16)
    return cs


def build(cfg):
    S, D, H, FF, FE, E, TS = (cfg[k] for k in ("S", "D", "H", "FF", "FE", "E", "TS"))
    KC = D // 128
    NT = S // 128
    NG = S // 512
    NB = S // 256
    NBP = max(NB, 8)
    NU = max(S + 512, 2048)
    WB = 1792
    DMAXNEAR = 896
    c = Ctx()
    nc = c.nc
    di = lambda n, sh, dt=F32: c.dram(n, sh, dt, kind="ExternalInput")
    x_in = di("x", [S, D])
    c_in = di("c", [1, D])
    rel_bias = di("rel_bias", [32, H])
    mod_w = di("mod_w", [4, D, 6 * D])
    mod_b = di("mod_b", [1, 4 * 6 * D])
    norm_mix_g = di("norm_mix_g", [4, D])
    norm_ffn_g = di("norm_ffn_g", [4, D])
    a_wqkv = di("a_wqkv", [2, D, 3 * D])
    a_wo = di("a_wo", [2, D, D])
    kv_norm_g = di("kv_norm_g", [1, D])
    kv_mod_w = di("kv_mod_w", [D, 2 * D])
    kv_mod_b = di("kv_mod_b", [1, 2 * D])
    b_wkv = di("b_wkv", [D, 2 * D])
    b_wq = di("b_wq", [2, D, D])
    b_wo = di("b_wo", [2, D, D])
    ffn_w13 = di("ffn_w13", [2, D, 2 * FF])
    ffn_w2 = di("ffn_w2", [2, FF, D])
    router_w = di("router_w", [2, D, E])
    moe_w13 = di("moe_w13", [2, E, D, 2 * FE])
    moe_w2 = di("moe_w2", [2, E, FE, D])
    final_norm_g = di("final_norm_g", [1, D])
    identb_d = di("identb", [128, 128], BF16)
    identf_d = di("identf", [128, 128])
    oh_d = di("oh", [33, NU])
    jex_d = di("jex", [128, 128])
    esel_d = di("esel", [16, 16 * 128], BF16)
    esel2_d = di("esel2", [16, S], BF16)
    ustrict_d = di("ustrict", [128, 128], BF16)
    cmask_d = di("cmask", [128, 896], BF16)
    out_d = c.dram("out", [S, D], F32, kind="ExternalOutput")

    xres = c.dram("xres", [S, D], F32)
    hT = c.dram("hT", [D, S], BF16)
    qT = c.dram("qT", [D, S], BF16)
    kT = c.dram("kT", [D, S], BF16)
    v65 = c.dram("v65", [S, H * 65], BF16)
    attnT = c.dram("attnT", [D, S], BF16)
    NMOD = 4 * 6 * D + 2 * D
    NVR = NMOD // 128 + 4 * KC + 4 * KC + KC
    vecscr = c.dram("vecscr", [NVR, 128], F32)
    ftab = c.dram("ftab", [H, NU], F32)
    biasT = c.dram("biasT", [H, 128, WB], BF16)
    comb_tm = c.dram("comb_tm", [S, E], F32)

    identb = c.sbuf("identb", [128, 128], BF16)
    identf = c.sbuf("identf", [128, 128], F32)
    modT = c.sbuf("modT", [128, NVR], F32)
    geff = c.sbuf("geff", [128, 9 * KC], F32)
    onesf = c.sbuf("onesf", [128, 128], F32)
    onesb = c.sbuf("onesb", [128, 128], BF16)
    c.dma("sp", [(identb.t[:, :], identb_d[:, :])], writes=[identb])
    c.dma("sp", [(identf.t[:, :], identf_d[:, :])], writes=[identf])
    c.op("pool", lambda e: e.memset(onesf.t[:, :], 1.0), writes=[onesf])
    c.op("pool", lambda e: e.memset(onesb.t[:, :], 1.0), writes=[onesb])

    def col_mod(l, j):
        return l * 6 * KC + j * KC
    COL_KV = 4 * 6 * KC
    COL_GMIX = NMOD // 128
    COL_GFFN = COL_GMIX + 4 * KC
    COL_GKV = COL_GFFN + 4 * KC

    c.begin()
    crow = c.sbuf("crow", [KC, 128], F32)
    cT = c.sbuf("cT", [128, KC], F32)
    scT = c.sbuf("scT", [128, KC], F32)
    mbrow = c.sbuf("mbrow", [1, NMOD], F32)
    pA = c.psum("pA", [128, 512], F32)
    pB = Rot([c.psum("pB0", [128, 512], F32), c.psum("pB1", [128, 512], F32)])
    mw = Rot([c.sbuf("mw0", [128, KC, 512], F32), c.sbuf("mw1", [128, KC, 512], F32)])
    mst = Rot([c.sbuf("mst0", [1, 512], F32), c.sbuf("mst1", [1, 512], F32)])
    vs = c.vslot("vecscr")
    c.dma("sp", [(crow.t[:, :], c_in[0:1, :].rearrange("o (k p) -> (o k) p", p=128))], writes=[crow])
    c.dma("sp", [(mbrow.t[0:1, 0:4 * 6 * D], mod_b[0:1, :]), (mbrow.t[0:1, 4 * 6 * D:NMOD], kv_mod_b[0:1, :])], writes=[mbrow])
    c.dma("pool", [(vecscr[COL_GMIX:COL_GMIX + 4 * KC, :], norm_mix_g[:, :].rearrange("l (k p) -> (l k) p", p=128)),
                   (vecscr[COL_GFFN:COL_GFFN + 4 * KC, :], norm_ffn_g[:, :].rearrange("l (k p) -> (l k) p", p=128)),
                   (vecscr[COL_GKV:COL_GKV + KC, :], kv_norm_g[0:1, :].rearrange("o (k p) -> (o k) p", p=128))],
          writes=[vs])
    c.op("pe", lambda e: e.transpose(out=pA.t[:, 0:KC], in_=crow.t[:, :], identity=identf.t[0:KC, 0:KC]), reads=[crow, identf], writes=[pA])
    c.op("dve", lambda e: e.tensor_copy(cT.t[:, :], pA.t[:, 0:KC]), reads=[pA], writes=[cT])
    c.op("act", lambda e: e.activation(out=scT.t[:, :], in_=cT.t[:, :], func=AF.Silu), reads=[cT], writes=[scT])
    blocks = [(mod_w[l], cb, l * 6 * D + cb * 512) for l in range(4) for cb in range(6 * D // 512)]
    blocks += [(kv_mod_w, cb, 4 * 6 * D + cb * 512) for cb in range(2 * D // 512)]
    for (wsrc, cb, off) in blocks:
        w = mw.next()
        c.dma("sp", [(w.t[:, :, :], wsrc[:, cb * 512:(cb + 1) * 512].rearrange("(k p) f -> p k f", p=128))], writes=[w])
        ps = pB.next()
        for kc in range(KC):
            c.op("pe", lambda e, kc=kc, w=w, ps=ps: e.matmul(ps.t[0:1, :], lhsT=scT.t[:, kc:kc + 1], rhs=w.t[:, kc, :],
                                                            start=(kc == 0), stop=(kc == KC - 1)), reads=[scT, w], writes=[ps])
        st = mst.next()
        c.op("dve", lambda e, st=st, ps=ps, off=off: e.tensor_tensor(out=st.t[0:1, :], in0=ps.t[0:1, :], in1=mbrow.t[0:1, off:off + 512], op=ALU.add),
             reads=[ps, mbrow], writes=[st])
        r0 = off // 128
        c.dma("sp", [(vecscr[r0:r0 + 4, :].rearrange("(o r) p -> o (r p)", o=1), st.t[0:1, :])], reads=[st], writes=[vs])
    c.barrier()
    r = 0
    vrow = Rot([c.sbuf("vrow0", [128, 128], F32), c.sbuf("vrow1", [128, 128], F32)])
    while r < NVR:
        n = min(128, NVR - r)
        vr = vrow.next()
        c.dma("sp", [(vr.t[0:n, :], vecscr[r:r + n, :])], writes=[vr])
        c.op("pe", lambda e, vr=vr, n=n: e.transpose(out=pA.t[:, 0:n], in_=vr.t[0:n, :], identity=identf.t[0:n, 0:n]), reads=[vr, identf], writes=[pA])
        c.op("dve", lambda e, r=r, n=n: e.tensor_copy(modT.t[:, r:r + n], pA.t[:, 0:n]), reads=[pA], writes=[modT])
        r += n
    for l in range(4):
        c.op("dve", lambda e, l=l: e.scalar_tensor_tensor(out=geff.t[:, l * KC:(l + 1) * KC], in0=modT.t[:, col_mod(l, 1):col_mod(l, 1) + KC], scalar=1.0,
                                                          in1=modT.t[:, COL_GMIX + l * KC:COL_GMIX + (l + 1) * KC], op0=ALU.add, op1=ALU.mult), reads=[modT], writes=[geff])
        c.op("dve", lambda e, l=l: e.scalar_tensor_tensor(out=geff.t[:, (4 + l) * KC:(5 + l) * KC], in0=modT.t[:, col_mod(l, 4):col_mod(l, 4) + KC], scalar=1.0,
                                                          in1=modT.t[:, COL_GFFN + l * KC:COL_GFFN + (l + 1) * KC], op0=ALU.add, op1=ALU.mult), reads=[modT], writes=[geff])
    c.op("dve", lambda e: e.scalar_tensor_tensor(out=geff.t[:, 8 * KC:9 * KC], in0=modT.t[:, COL_KV + KC:COL_KV + 2 * KC], scalar=1.0,
                                                 in1=modT.t[:, COL_GKV:COL_GKV + KC], op0=ALU.add, op1=ALU.mult), reads=[modT], writes=[geff])
    c.end()

    c.begin()
    rbA = c.sbuf("rbA", [33, H], F32)
    ohs = c.sbuf("ohs", [33, NU], F32)
    fsb = c.sbuf("fsb", [H, NU], F32)
    jex = c.sbuf("jex", [128, 128], F32)
    pF = Rot([c.psum("pF0", [128, 512], F32), c.psum("pF1", [128, 512], F32)])
    c.op("pool", lambda e: e.memset(rbA.t[32:33, :], -BIG), writes=[rbA])
    c.dma("sp", [(rbA.t[0:32, :], rel_bias[:, :])], writes=[rbA])
    c.dma("sp", [(ohs.t[:, :], oh_d[:, :])], writes=[ohs])
    c.dma("sp", [(jex.t[:, :], jex_d[:, :])], writes=[jex])
    for ch in range(NU // 512):
        ps = pF.next()
        c.op("pe", lambda e, ps=ps, ch=ch: e.matmul(ps.t[0:H, :], lhsT=rbA.t[:, 0:H], rhs=ohs.t[:, ch * 512:(ch + 1) * 512], start=True, stop=True),
             reads=[rbA, ohs], writes=[ps])
        c.op("act", lambda e, ps=ps, ch=ch: e.activation(out=fsb.t[:, ch * 512:(ch + 1) * 512], in_=ps.t[0:H, :], func=AF.Copy), reads=[ps], writes=[fsb])
    c.dma("sp", [(ftab[:, :], fsb.t[:, :])], reads=[fsb])
    c.barrier()
    t2 = Rot([c.sbuf("t2a", [128, WB], F32), c.sbuf("t2b", [128, WB], F32)])
    bst = Rot([c.sbuf("bsta", [128, WB], BF16), c.sbuf("bstb", [128, WB], BF16)])
    for h in range(H):
        t = t2.next()
        src = bass.AP(tensor=ftab, offset=h * NU + 1, ap=[[1, 128], [1, WB]])
        c.dma("sp", [(t.t[:, :], src)], writes=[t])
        bs = bst.next()
        for ch in range(0, WB, 512):
            w_ = min(512, WB - ch)
            ps = pF.next()
            c.op("pe", lambda e, ps=ps, t=t, ch=ch, w_=w_: e.matmul(ps.t[:, 0:w_], lhsT=jex.t[:, :], rhs=t.t[:, ch:ch + w_], start=True, stop=True),
                 reads=[jex, t], writes=[ps])
            c.op("act", lambda e, ps=ps, bs=bs, ch=ch, w_=w_: e.activation(out=bs.t[:, ch:ch + w_], in_=ps.t[:, 0:w_], func=AF.Copy), reads=[ps], writes=[bs])
        c.dma("sp", [(biasT[h], bs.t[:, :])], reads=[bs])
    c.end()

    def norm_phase(src, gcol0, shcol0, dst):
        c.begin()
        xt_r = Rot([c.sbuf("nx0", [128, 4, D], F32), c.sbuf("nx1", [128, 4, D], F32)])
        junk = c.sbuf("njunk", [128, D], BF16)
        ss_r = Rot([c.sbuf("nss0", [128, 4], F32), c.sbuf("nss1", [128, 4], F32)])
        sq_r = Rot([c.sbuf("nsq0", [128, 4], F32), c.sbuf("nsq1", [128, 4], F32)])
        rs_r = Rot([c.sbuf("nrs0", [128, 4], F32), c.sbuf("nrs1", [128, 4], F32)])
        xn_r = Rot([c.sbuf("nxn0", [128, 4, D], BF16), c.sbuf("nxn1", [128, 4, D], BF16)])
        ht_r = Rot([c.sbuf("nht0", [128, KC, 512], BF16), c.sbuf("nht1", [128, KC, 512], BF16)])
        pt_r = Rot([c.psum("npt0", [128, 512], BF16), c.psum("npt1", [128, 512], BF16), c.psum("npt2", [128, 512], BF16)])
        epsc = c.sbuf("epsc", [128, 1], F32)
        c.op("pool", lambda e: e.memset(epsc.t[:, :], EPS), writes=[epsc])
        for g in range(NG):
            xt = xt_r.next()
            c.dma("sp", [(xt.t[:, :, :], src[g * 512:(g + 1) * 512, :].rearrange("(t p) d -> p t d", p=128))], writes=[xt])
            ss, sq, rs, xn, ht = ss_r.next(), sq_r.next(), rs_r.next(), xn_r.next(), ht_r.next()
            for t in range(4):
                c.op("act", lambda e, t=t, xt=xt, ss=ss: e.activation(out=junk.t[:, :], in_=xt.t[:, t, :], func=AF.Square, accum_out=ss.t[:, t:t + 1]),
                     reads=[xt], writes=[junk, ss])
            c.op("act", lambda e, ss=ss, sq=sq: e.activation(out=sq.t[:, :], in_=ss.t[:, :], func=AF.Sqrt, scale=1.0 / D, bias=epsc.t[:, 0:1]), reads=[ss, epsc], writes=[sq])
            c.op("dve", lambda e, sq=sq, rs=rs: e.reciprocal(out=rs.t[:, :], in_=sq.t[:, :]), reads=[sq], writes=[rs])
            for t in range(4):
                c.op("act", lambda e, t=t, xt=xt, xn=xn, rs=rs: e.activation(out=xn.t[:, t, :], in_=xt.t[:, t, :], func=AF.Copy, scale=rs.t[:, t:t + 1]),
                     reads=[xt, rs], writes=[xn])
            for kc in range(KC):
                pt = pt_r.next()
                for t in range(4):
                    c.op("pe", lambda e, t=t, kc=kc, pt=pt, xn=xn: e.transpose(out=pt.t[:, t * 128:(t + 1) * 128], in_=xn.t[:, t, kc * 128:(kc + 1) * 128], identity=identb.t[:, :]),
                         reads=[xn, identb], writes=[pt])
                c.op("dve", lambda e, kc=kc, pt=pt, ht=ht: e.tensor_scalar(out=ht.t[:, kc, :], in0=pt.t[:, :], scalar1=geff.t[:, gcol0 + kc:gcol0 + kc + 1],
                                                                           scalar2=modT.t[:, shcol0 + kc:shcol0 + kc + 1], op0=ALU.mult, op1=ALU.add),
                     reads=[pt, geff, modT], writes=[ht])
            c.dma("pool", [(dst[:, g * 512:(g + 1) * 512].rearrange("(k p) s -> p k s", p=128), ht.t[:, :, :])], reads=[ht])
        c.end()

    def load_w_bf16(dst_slot, dst_ap_fn, src2d, ncols, piece=1024):
        pairs = []
        for c0 in range(0, ncols, piece):
            w_ = min(piece, ncols - c0)
            pairs.append((dst_ap_fn(c0, w_), src2d[:, c0:c0 + w_].rearrange("(k p) f -> p k f", p=128)))
        c.dma("pool", pairs, writes=[dst_slot])

    def proj_phase(src_hT, w2d, ncols_fm, ncols_tm, fm_dsts, tm_dst65, q_scale_chunks):
        c.begin()
        ncols = ncols_fm + ncols_tm
        wsb = c.sbuf("pw", [128, KC, ncols], BF16)
        load_w_bf16(wsb, lambda c0, w_: wsb.t[:, :, c0:c0 + w_], w2d, ncols)
        ht_r = Rot([c.sbuf("pht0", [128, KC, 512], BF16), c.sbuf("pht1", [128, KC, 512], BF16)])
        NJ = ncols_fm // 128
        st_r = Rot([c.sbuf("pst0", [128, max(NJ, 1), 512], BF16), c.sbuf("pst1", [128, max(NJ, 1), 512], BF16)])
        ps_r = Rot([c.psum(f"pps{i}", [128, 512], F32) for i in range(4)])
        if ncols_tm:
            vst_r = Rot([c.sbuf("pvs0", [128, 4, H, 65], BF16), c.sbuf("pvs1", [128, 4, H, 65], BF16)])
            for v in vst_r.items:
                c.op("pool", lambda e, v=v: e.memset(v.t[:, :, :, :], 1.0), writes=[v])
        for g in range(NG):
            ht = ht_r.next()
            c.dma("sp", [(ht.t[:, :, :], src_hT[:, g * 512:(g + 1) * 512].rearrange("(k p) s -> p k s", p=128))], writes=[ht])
            if NJ:
                st = st_r.next()
                for j in range(NJ):
                    ps = ps_r.next()
                    for kc in range(KC):
                        c.op("pe", lambda e, ps=ps, j=j, kc=kc, ht=ht: e.matmul(ps.t[:, :], lhsT=wsb.t[:, kc, j * 128:(j + 1) * 128], rhs=ht.t[:, kc, :],
                                                                               start=(kc == 0), stop=(kc == KC - 1)), reads=[wsb, ht], writes=[ps])
                    if j in q_scale_chunks:
                        c.op("act", lambda e, ps=ps, st=st, j=j: e.activation(out=st.t[:, j, :], in_=ps.t[:, :], func=AF.Copy, scale=0.125), reads=[ps], writes=[st])
                    else:
                        c.op("dve", lambda e, ps=ps, st=st, j=j: e.tensor_copy(st.t[:, j, :], ps.t[:, :]), reads=[ps], writes=[st])
                pairs = []
                for j in range(NJ):
                    dt_, r0 = fm_dsts[j]
                    pairs.append((dt_[r0:r0 + 128, g * 512:(g + 1) * 512], st.t[:, j, :]))
                c.dma("pool", pairs, reads=[st])
            if ncols_tm:
                vst = vst_r.next()
                for tt in range(4):
                    for half in range(ncols_tm // 512):
                        ps = ps_r.next()
                        for kc in range(KC):
                            c.op("pe", lambda e, ps=ps, kc=kc, ht=ht, tt=tt, half=half: e.matmul(ps.t[:, :], lhsT=ht.t[:, kc, tt * 128:(tt + 1) * 128],
                                                                                                rhs=wsb.t[:, kc, ncols_fm + half * 512:ncols_fm + (half + 1) * 512],
                                                                                                start=(kc == 0), stop=(kc == KC - 1)), reads=[wsb, ht], writes=[ps])
                        c.op("act", lambda e, ps=ps, vst=vst, tt=tt, half=half: e.activation(out=vst.t[:, tt, half * 8:(half + 1) * 8, 0:64],
                                                                                             in_=ps.t[:, :].rearrange("p (h d) -> p h d", d=64), func=AF.Copy),
                             reads=[ps], writes=[vst])
                c.dma("pool", [(tm_dst65[g * 512:(g + 1) * 512, :].rearrange("(t p) f -> p t f", p=128), vst.t[:, :, :, :].rearrange("p t h d -> p t (h d)"))], reads=[vst])
        c.end()

    def wo_phase(w2d, gcol_row0):
        c.begin()
        wsb = c.sbuf("ow", [128, KC, D], BF16)
        load_w_bf16(wsb, lambda c0, w_: wsb.t[:, :, c0:c0 + w_], w2d, D)
        gb = c.sbuf("ogb", [128, D], F32)
        c.dma("sp", [(gb.t[:, :], vecscr[gcol_row0:gcol_row0 + KC, :].rearrange("(o k) p -> o (k p)", o=1).partition_broadcast(128))], writes=[gb])
        at_r = Rot([c.sbuf("oat0", [128, KC, 512], BF16), c.sbuf("oat1", [128, KC, 512], BF16)])
        xt_r = Rot([c.sbuf("ox0", [128, 4, D], F32), c.sbuf("ox1", [128, 4, D], F32)])
        tmp_r = Rot([c.sbuf("otm0", [128, 512], F32), c.sbuf("otm1", [128, 512], F32)])
        ps_r = Rot([c.psum(f"ops{i}", [128, 512], F32) for i in range(4)])
        for g in range(NG):
            at = at_r.next()
            xt = xt_r.next()
            c.dma("sp", [(at.t[:, :, :], attnT[:, g * 512:(g + 1) * 512].rearrange("(k p) s -> p k s", p=128))], writes=[at])
            c.dma("sp", [(xt.t[:, :, :], xres[g * 512:(g + 1) * 512, :].rearrange("(t p) d -> p t d", p=128))], writes=[xt])
            for tt in range(4):
                for half in range(D // 512):
                    ps = ps_r.next()
                    for kc in range(KC):
                        c.op("pe", lambda e, ps=ps, kc=kc, at=at, tt=tt, half=half: e.matmul(ps.t[:, :], lhsT=at.t[:, kc, tt * 128:(tt + 1) * 128],
                                                                                            rhs=wsb.t[:, kc, half * 512:(half + 1) * 512],
                                                                                            start=(kc == 0), stop=(kc == KC - 1)), reads=[wsb, at], writes=[ps])
                    tmp = tmp_r.next()
                    c.op("dve", lambda e, ps=ps, tmp=tmp, half=half: e.tensor_tensor(out=tmp.t[:, :], in0=ps.t[:, :], in1=gb.t[:, half * 512:(half + 1) * 512], op=ALU.mult),
                         reads=[ps, gb], writes=[tmp])
                    c.op("pool", lambda e, tmp=tmp, xt=xt, tt=tt, half=half: e.tensor_tensor(out=xt.t[:, tt, half * 512:(half + 1) * 512], in0=xt.t[:, tt, half * 512:(half + 1) * 512],
                                                                                           in1=tmp.t[:, :], op=ALU.add), reads=[tmp, xt], writes=[xt])
            c.dma("pool", [(xres[g * 512:(g + 1) * 512, :].rearrange("(t p) d -> p t d", p=128), xt.t[:, :, :])], reads=[xt])
        c.end()

    def bc_last(ap2d, n):
        a = [list(x) for x in ap2d.ap]
        return bass.AP(tensor=ap2d.tensor, offset=ap2d.offset, ap=a + [[0, n]])

    def moba_phase():
        c.begin()
        NGN = NT * NB
        vall = c.sbuf("mvall", [128, NT, H * 65], BF16)
        c.dma("sp", [(vall.t[:, :, :], v65[:, :].rearrange("(t p) f -> p t f", p=128))], writes=[vall])
        qp_r = Rot([c.sbuf("mq0", [128, S], BF16), c.sbuf("mq1", [128, S], BF16)])
        kp_r = Rot([c.sbuf("mk0", [128, S], BF16), c.sbuf("mk1", [128, S], BF16)])
        for t_ in qp_r.items + kp_r.items:
            c.op("pool", lambda e, t_=t_: e.memset(t_.t[:, :], 0.0), writes=[t_])
        for t_ in kp_r.items:
            c.dma("sp", [(t_.t[64:64 + NB, :], esel2_d[0:NB, :])], writes=[t_])
        bt_r = Rot([c.sbuf("mbt0", [128, WB], BF16), c.sbuf("mbt1", [128, WB], BF16)])
        b31_r = Rot([c.sbuf("mb31a", [128, 1], F32), c.sbuf("mb31b", [128, 1], F32)])
        km = c.sbuf("mkm", [128, NB], F32)
        kmb_r = Rot([c.sbuf("mkmb0", [128, NB], BF16), c.sbuf("mkmb1", [128, NB], BF16)])
        for t_ in kmb_r.items:
            c.op("pool", lambda e, t_=t_: e.memset(t_.t[:, :], 0.0), writes=[t_])
        VM = c.sbuf("mVM", [128, NT, NB], F32)
        NV = c.sbuf("mNV", [128, NT, NB], F32)
        TM = c.sbuf("mTM", [128, NT, NB], F32)
        gmA = c.sbuf("mgmA", [128, NGN], F32)
        gmB = c.sbuf("mgmB", [128, NGN], F32)
        gmC = c.sbuf("mgmC", [128, NGN], F32)
        tA = c.sbuf("mtA", [128, NGN], F32)
        mx = c.sbuf("mmx", [128, NT], F32)
        mvs = c.sbuf("mmvs", [128, NGN], F32)
        sb_r = Rot([c.sbuf(f"msb{i}", [128, 512], F32) for i in range(3)])
        a_r = Rot([c.sbuf(f"ma{i}", [128, 512], BF16) for i in range(4)])
        rsb_r = Rot([c.sbuf("mrs0", [128, 512], F32), c.sbuf("mrs1", [128, 512], F32)])
        rb_r = Rot([c.sbuf("mrb0", [64, 512], F32), c.sbuf("mrb1", [64, 512], F32)])
        ast_r = Rot([c.sbuf("mas0", [64, 512], BF16), c.sbuf("mas1", [64, 512], BF16)])
        pS = Rot([c.psum(f"mpS{i}", [128, 512], F32) for i in range(3)])
        pO = Rot([c.psum("mpO0", [128, 512], F32), c.psum("mpO1", [128, 512], F32)])
        pG = c.psum("mpG", [128, 512], F32)
        pX = c.psum("mpX", [128, 512], F32)
        pR = c.psum("mpR", [128, 512], F32)
        c.op("pool", lambda e: e.memset(VM.t[:, :, :], -BIG), writes=[VM])
        c.op("pool", lambda e: e.memset(NV.t[:, :, :], 0.0), writes=[NV])
        c.op("pool", lambda e: e.memset(TM.t[:, :, :], -BIG), writes=[TM])
        for i in range(NT):
            cur = i // 2
            if cur > 0:
                c.op("pool", lambda e, i=i, cur=cur: e.memset(VM.t[:, i, 0:cur], 0.0), writes=[VM])
                c.op("pool", lambda e, i=i, cur=cur: e.memset(NV.t[:, i, 0:cur], -BIG), writes=[NV])
            c.op("pool", lambda e, i=i, cur=cur: e.memset(TM.t[:, i, 0:cur + 1], 0.0), writes=[TM])
        v3 = lambda s_: s_.t[:, :].rearrange("p (i n) -> p i n", n=NB)
        state = {}

        def prepass(h):
            qp, kp, kmb = qp_r.next(), kp_r.next(), kmb_r.next()
            c.dma("sp", [(qp.t[0:64, :], qT[h * 64:h * 64 + 64, :])], writes=[qp])
            c.dma("sp", [(kp.t[0:64, :], kT[h * 64:h * 64 + 64, :])], writes=[kp])
            c.op("dve", lambda e: e.tensor_reduce(out=km.t[0:64, :], in_=kp.t[0:64, :].rearrange("p (n b) -> p n b", b=256), axis=AX.X, op=ALU.add), reads=[kp], writes=[km])
            c.op("dve", lambda e: e.tensor_scalar(out=kmb.t[0:64, :], in0=km.t[0:64, :], scalar1=1.0 / 256, scalar2=None, op0=ALU.mult), reads=[km], writes=[kmb])
            pb = 0
            bt = bt_r.next()
            c.dma("sp", [(bt.t[:, :], biasT[h])], writes=[bt])
            b31 = b31_r.next()
            c.dma("sp", [(b31.t[:, :], ftab[h:h + 1, 1512:1513].partition_broadcast(128))], writes=[b31])
            maskT = qp
            for i in range(NT):
                c.op("pe", lambda e, i=i: e.matmul(pG.t[:, i * NB:(i + 1) * NB], lhsT=qp.t[:, i * 128:(i + 1) * 128], rhs=kmb.t[:, 0:NB], start=True, stop=True),
                     reads=[qp, kmb], writes=[pG])
            c.op("dve", lambda e: e.tensor_tensor(out=gmA.t[:, :], in0=pG.t[:, 0:NGN], in1=VM.t[:, :, :].rearrange("p i n -> p (i n)"), op=ALU.add), reads=[pG, VM], writes=[gmA])
            src = gmA
            for (dst,) in ((gmB,), (gmC,)):
                c.op("dve", lambda e, src=src: e.tensor_reduce(out=mx.t[:, :], in_=v3(src), axis=AX.X, op=ALU.max), reads=[src], writes=[mx])
                c.op("dve", lambda e, src=src: e.tensor_tensor(out=v3(tA), in0=v3(src), in1=bc_last(mx.t[:, :], NB), op=ALU.is_ge), reads=[src, mx], writes=[tA])
                c.op("dve", lambda e, src=src, dst=dst: e.scalar_tensor_tensor(out=dst.t[:, :], in0=tA.t[:, :], scalar=-BIG, in1=src.t[:, :], op0=ALU.mult, op1=ALU.add),
                     reads=[tA, src], writes=[dst])
                src = dst
            c.op("dve", lambda e: e.tensor_reduce(out=mx.t[:, :], in_=v3(gmC), axis=AX.X, op=ALU.max), reads=[gmC], writes=[mx])
            c.op("dve", lambda e: e.tensor_tensor(out=v3(tA), in0=v3(gmA), in1=bc_last(mx.t[:, :], NB), op=ALU.is_lt), reads=[gmA, mx], writes=[tA])
            c.op("dve", lambda e: e.tensor_tensor(out=tA.t[:, :], in0=tA.t[:, :], in1=NV.t[:, :, :].rearrange("p i n -> p (i n)"), op=ALU.mult), reads=[tA, NV], writes=[tA])
            c.op("dve", lambda e: e.tensor_tensor(out=mvs.t[:, :], in0=tA.t[:, :], in1=TM.t[:, :, :].rearrange("p i n -> p (i n)"), op=ALU.add), reads=[tA, TM], writes=[mvs])
            for i0 in range(0, NT, 4):
                for i in range(i0, i0 + 4):
                    c.op("pe", lambda e, i=i, i0=i0: e.transpose(out=pX.t[0:NB, (i - i0) * 128:(i - i0 + 1) * 128], in_=mvs.t[:, i * NB:(i + 1) * NB], identity=identf.t[:, :]),
                         reads=[mvs, identf], writes=[pX])
                c.op("act", lambda e, i0=i0: e.activation(out=qp.t[64:64 + NB, i0 * 128:(i0 + 4) * 128], in_=pX.t[0:NB, 0:512], func=AF.Copy), reads=[pX], writes=[qp])
            return (qp, kp, bt, b31, maskT, pb)

        def main(h, ctxh):
            qp, kp, bt, b31, maskT, pb = ctxh
            tiles = [(Q, kt) for Q in range(NG) for kt in range(4 * Q + 4)]
            T = {}
            pos = {}
            for step in range(len(tiles) + 3):
                j = step - 2
                if 0 <= j < len(tiles):
                    Q, kt = tiles[j]
                    nkt = 4 * Q + 4
                    a = T.pop(j)["a"]
                    if kt == 0:
                        pos[Q] = pO.next()
                    po = pos[Q]
                    c.op("pe", lambda e, po=po, a=a, kt=kt, nkt=nkt: e.matmul(po.t[0:65, :], lhsT=vall.t[:, kt, h * 65:(h + 1) * 65], rhs=a.t[:, :],
                                                                            start=(kt == 0), stop=(kt == nkt - 1)), reads=[vall, a], writes=[po])
                    if kt == nkt - 1:
                        rsb = rsb_r.next()
                        c.op("dve", lambda e, po=po, rsb=rsb: e.reciprocal(out=rsb.t[64:65, :], in_=po.t[64:65, :]), reads=[po], writes=[rsb])
                        c.op("pe", lambda e, rsb=rsb: e.matmul(pR.t[0:64, :], lhsT=onesf.t[64:65, 0:64], rhs=rsb.t[64:65, :], start=True, stop=True), reads=[onesf, rsb], writes=[pR])
                        rb = rb_r.next()
                        c.op("dve", lambda e, rb=rb: e.tensor_copy(rb.t[:, :], pR.t[0:64, :]), reads=[pR], writes=[rb])
                        ast = ast_r.next()
                        c.op("dve", lambda e, po=po, rb=rb, ast=ast: e.tensor_tensor(out=ast.t[:, :], in0=po.t[0:64, :], in1=rb.t[:, :], op=ALU.mult), reads=[po, rb], writes=[ast])
                        c.dma("pool", [(attnT[h * 64:(h + 1) * 64, Q * 512:(Q + 1) * 512], ast.t[:, :])], reads=[ast])
                j = step - 1
                if 0 <= j < len(tiles):
                    Q, kt = tiles[j]
                    a = a_r.next()
                    ps = T[j].pop("ps")
                    if T[j].pop("near"):
                        c.op("act", lambda e, ps=ps, a=a: e.activation(out=a.t[:, :], in_=ps.t[:, :], func=AF.Exp), reads=[ps], writes=[a])
                    else:
                        c.op("act", lambda e, ps=ps, a=a: e.activation(out=a.t[:, :], in_=ps.t[:, :], func=AF.Exp, bias=b31.t[:, 0:1]), reads=[ps, b31], writes=[a])
                    T[j]["a"] = a
                j = step
                if j < len(tiles):
                    Q, kt = tiles[j]
                    delta = 512 * Q - 128 * kt
                    near = delta <= DMAXNEAR
                    ps = pS.next()
                    c.op("pe", lambda e, ps=ps, kt=kt, Q=Q, near=near: e.matmul(ps.t[:, :], lhsT=kp.t[:, kt * 128:(kt + 1) * 128], rhs=qp.t[:, Q * 512:(Q + 1) * 512],
                                                                              start=True, stop=(not near)), reads=[kp, qp], writes=[ps])
                    if near:
                        o = delta + 384
                        c.op("pe", lambda e, ps=ps, o=o: e.matmul(ps.t[:, :], lhsT=identb.t[:, :], rhs=bt.t[:, o:o + 512], start=False, stop=True), reads=[identb, bt], writes=[ps])
                    T[j] = {"ps": ps, "near": near}

        nxt = prepass(0)
        for h in range(H):
            cur_ctx = nxt
            if h + 1 < H:
                nxt = prepass(h + 1)
            main(h, cur_ctx)
        c.end()

    def sb_phase():
        c.begin()
        vall = c.sbuf("svall", [128, NT, H * 65], BF16)
        c.dma("sp", [(vall.t[:, :, :], v65[:, :].rearrange("(t p) f -> p t f", p=128))], writes=[vall])
        ustr = c.sbuf("sustr", [128, 128], BF16)
        cm = c.sbuf("scm", [128, 896], BF16)
        c.dma("sp", [(ustr.t[:, :], ustrict_d[:, :])], writes=[ustr])
        c.dma("sp", [(cm.t[:, :], cmask_d[:, :])], writes=[cm])
        qp_r = Rot([c.sbuf("sq0", [128, S], BF16), c.sbuf("sq1", [128, S], BF16)])
        kp_r = Rot([c.sbuf("sk0", [128, S], BF16), c.sbuf("sk1", [128, S], BF16)])
        for t_ in qp_r.items + kp_r.items:
            c.op("pool", lambda e, t_=t_: e.memset(t_.t[:, :], 0.0), writes=[t_])
        e_r = Rot([c.sbuf(f"se{i}", [128, 512], F32) for i in range(2)])
        lp_r = Rot([c.sbuf(f"slp{i}", [128, 512], BF16) for i in range(5)])
        ln_r = Rot([c.sbuf(f"sln{i}", [128, 512], BF16) for i in range(7)])
        a_r = Rot([c.sbuf(f"sa{i}", [128, 512], BF16) for i in range(4)])
        tot_r = Rot([c.sbuf(f"stot{i}", [128, 512], BF16) for i in range(4)])
        ast_r = Rot([c.sbuf("sas0", [64, 512], BF16), c.sbuf("sas1", [64, 512], BF16)])
        pZ = Rot([c.psum(f"spZ{i}", [128, 512], F32) for i in range(3)])
        pT = Rot([c.psum("spT0", [128, 512], F32), c.psum("spT1", [128, 512], F32)])
        pTOT = Rot([c.psum("spTOT0", [128, 512], F32)])
        pO = Rot([c.psum("spO0", [128, 512], F32), c.psum("spO1", [128, 512], F32)])
        for h in range(H):
            qp, kp = qp_r.next(), kp_r.next()
            c.dma("sp", [(qp.t[0:64, :], qT[h * 64:h * 64 + 64, :])], writes=[qp])
            c.dma("sp", [(kp.t[0:64, :], kT[h * 64:h * 64 + 64, :])], writes=[kp])
            tiles = [(Q, idx, 4 * Q + 3 - idx) for Q in range(NG) for idx in range(4 * Q + 4)]
            T = {}
            qstate = {}
            NST = 6
            for step in range(len(tiles) + NST):
                j = step - 5
                if 0 <= j < len(tiles):
                    Q, idx, kt = tiles[j]
                    nkt = 4 * Q + 4
                    a = T[j].pop("a")
                    po = qstate[Q]["po"]
                    c.op("pe", lambda e, po=po, a=a, kt=kt, idx=idx, nkt=nkt: e.matmul(po.t[0:64, :], lhsT=vall.t[:, kt, h * 65:h * 65 + 64], rhs=a.t[:, :],
                                                                                     start=(idx == 0), stop=(idx == nkt - 1)), reads=[vall, a], writes=[po])
                    if idx == nkt - 1:
                        ast = ast_r.next()
                        c.op("dve", lambda e, po=po, ast=ast: e.tensor_copy(ast.t[:, :], po.t[0:64, :]), reads=[po], writes=[ast])
                        c.dma("pool", [(attnT[h * 64:(h + 1) * 64, Q * 512:(Q + 1) * 512], ast.t[:, :])], reads=[ast])
                    del T[j]
                j = step - 4
                if 0 <= j < len(tiles):
                    Q, idx, kt = tiles[j]
                    pt = T[j].pop("pt")
                    a = a_r.next()
                    c.op("act", lambda e, pt=pt, a=a: e.activation(out=a.t[:, :], in_=pt.t[:, :], func=AF.Exp, scale=-1.0), reads=[pt], writes=[a])
                    if kt >= 4 * Q:
                        o = 512 * Q - 128 * kt + 384
                        c.op("pool", lambda e, a=a, o=o: e.tensor_tensor(out=a.t[:, :], in0=a.t[:, :], in1=cm.t[:, o:o + 512], op=ALU.mult), reads=[a, cm], writes=[a])
                    T[j]["a"] = a
                    if T[j].pop("ck", False):
                        qs = qstate[Q]
                        tsb = tot_r.next()
                        ptot = qs["ptot"]
                        c.op("dve", lambda e, ptot=ptot, tsb=tsb: e.tensor_copy(tsb.t[:, :], ptot.t[:, :]), reads=[ptot], writes=[tsb])
                        qs["ck"][idx + 1] = tsb
                j = step - 3
                if 0 <= j < len(tiles):
                    Q, idx, kt = tiles[j]
                    nkt = 4 * Q + 4
                    ln = T[j].pop("ln")
                    lp = T[j].pop("lp")
                    if idx == 0:
                        qstate[Q] = {"ptot": pTOT.next(), "ck": {}, "po": pO.next(), "ln": {}}
                    qs = qstate[Q]
                    qs["ln"][idx] = ln
                    n2 = max(0, idx - 2)
                    direct = list(range(n2, idx))
                    pt = pT.next()
                    c.op("pe", lambda e, pt=pt, ln=ln: e.matmul(pt.t[:, :], lhsT=ustr.t[:, :], rhs=ln.t[:, :], start=True, stop=False), reads=[ustr, ln], writes=[pt])
                    for dj in direct:
                        lnd = qs["ln"][dj]
                        c.op("pe", lambda e, pt=pt, lnd=lnd: e.matmul(pt.t[:, :], lhsT=onesb.t[:, :], rhs=lnd.t[:, :], start=False, stop=False), reads=[onesb, lnd], writes=[pt])
                    if n2 > 0:
                        tsb = qs["ck"].pop(n2)
                        c.op("pe", lambda e, pt=pt, tsb=tsb: e.matmul(pt.t[:, :], lhsT=identb.t[:, :], rhs=tsb.t[:, :], start=False, stop=False), reads=[identb, tsb], writes=[pt])
                    c.op("pe", lambda e, pt=pt, lp=lp: e.matmul(pt.t[:, :], lhsT=identb.t[:, :], rhs=lp.t[:, :], start=False, stop=True), reads=[identb, lp], writes=[pt])
                    if idx <= nkt - 4:
                        ptot = qs["ptot"]
                        c.op("pe", lambda e, ptot=ptot, ln=ln, idx=idx: e.matmul(ptot.t[:, :], lhsT=onesb.t[:, :], rhs=ln.t[:, :], start=(idx == 0), stop=True, skip_group_check=True),
                             reads=[onesb, ln], writes=[ptot])
                        T[j]["ck"] = True
                    qs["ln"].pop(idx - 2, None)
                    T[j]["pt"] = pt
                j = step - 2
                if 0 <= j < len(tiles):
                    Q, idx, kt = tiles[j]
                    pz = T[j].pop("pz")
                    lp = T[j]["lp"]
                    ln = ln_r.next()
                    c.op("dve", lambda e, pz=pz, lp=lp, ln=ln: e.tensor_tensor(out=ln.t[:, :], in0=pz.t[:, :], in1=lp.t[:, :], op=ALU.add), reads=[pz, lp], writes=[ln])
                    if kt >= 4 * Q:
                        o = 512 * Q - 128 * kt + 384
                        c.op("pool", lambda e, ln=ln, o=o: e.tensor_tensor(out=ln.t[:, :], in0=ln.t[:, :], in1=cm.t[:, o:o + 512], op=ALU.mult), reads=[ln, cm], writes=[ln])
                    T[j]["ln"] = ln
                j = step - 1
                if 0 <= j < len(tiles):
                    pz = T[j]["pz"]
                    ee, lp = e_r.next(), lp_r.next()
                    c.op("act", lambda e, pz=pz, ee=ee: e.activation(out=ee.t[:, :], in_=pz.t[:, :], func=AF.Exp, scale=-1.0), reads=[pz], writes=[ee])
                    c.op("act", lambda e, ee=ee, lp=lp: e.activation(out=lp.t[:, :], in_=ee.t[:, :], func=AF.Ln, bias=1.0), reads=[ee], writes=[lp])
                    T[j]["lp"] = lp
                j = step
                if j < len(tiles):
                    Q, idx, kt = tiles[j]
                    pz = pZ.next()
                    c.op("pe", lambda e, pz=pz, kt=kt, Q=Q: e.matmul(pz.t[:, :], lhsT=kp.t[:, kt * 128:(kt + 1) * 128], rhs=qp.t[:, Q * 512:(Q + 1) * 512],
                                                                   start=True, stop=True), reads=[kp, qp], writes=[pz])
                    T[j] = {"pz": pz}
        c.end()

    def router_phase(rw2d):
        c.begin()
        rw = c.sbuf("rrw", [128, KC, E], BF16)
        c.dma("pool", [(rw.t[:, :, :], rw2d.rearrange("(k p) e -> p k e", p=128))], writes=[rw])
        ht_r = Rot([c.sbuf("rht0", [128, KC, 512], BF16), c.sbuf("rht1", [128, KC, 512], BF16)])
        cst = c.sbuf("rcst", [128, NT, E], F32)
        mk = lambda nm, w: Rot([c.sbuf(nm + "0", [128, w], F32), c.sbuf(nm + "1", [128, w], F32)])
        lg_r, t8_r, d_r, ex_r, w1_r, w2_r, c1_r, c2_r = mk("rlg", 8), mk("rt8", 8), mk("rd", 1), mk("rex", 1), mk("rw1", 1), mk("rw2", 1), mk("rc1", 8), mk("rc2", 8)
        pL = Rot([c.psum("rpL0", [128, 512], F32), c.psum("rpL1", [128, 512], F32)])
        for g in range(NG):
            ht = ht_r.next()
            c.dma("sp", [(ht.t[:, :, :], hT[:, g * 512:(g + 1) * 512].rearrange("(k p) s -> p k s", p=128))], writes=[ht])
            for tt in range(4):
                pl = pL.next()
                for kc in range(KC):
                    c.op("pe", lambda e, pl=pl, kc=kc, ht=ht, tt=tt: e.matmul(pl.t[:, 0:E], lhsT=ht.t[:, kc, tt * 128:(tt + 1) * 128], rhs=rw.t[:, kc, :], start=(kc == 0), stop=(kc == KC - 1)),
                         reads=[ht, rw], writes=[pl])
                lg, t8, d_, ex, w1, w2, c1, c2 = (r_.next() for r_ in (lg_r, t8_r, d_r, ex_r, w1_r, w2_r, c1_r, c2_r))
                c.op("dve", lambda e, pl=pl, lg=lg: e.tensor_copy(lg.t[:, :], pl.t[:, 0:E]), reads=[pl], writes=[lg])
                c.op("dve", lambda e, lg=lg, t8=t8: e.max(out=t8.t[:, :], in_=lg.t[:, :]), reads=[lg], writes=[t8])
                c.op("dve", lambda e, t8=t8, d_=d_: e.tensor_tensor(out=d_.t[:, :], in0=t8.t[:, 1:2], in1=t8.t[:, 0:1], op=ALU.subtract), reads=[t8], writes=[d_])
                c.op("act", lambda e, d_=d_, ex=ex: e.activation(out=ex.t[:, :], in_=d_.t[:, :], func=AF.Exp), reads=[d_], writes=[ex])
                c.op("dve", lambda e, ex=ex, w1=w1: e.tensor_scalar(out=w1.t[:, :], in0=ex.t[:, :], scalar1=1.0, scalar2=None, op0=ALU.add), reads=[ex], writes=[w1])
                c.op("dve", lambda e, w1=w1: e.reciprocal(out=w1.t[:, :], in_=w1.t[:, :]), reads=[w1], writes=[w1])
                c.op("dve", lambda e, w1=w1, ex=ex, w2=w2: e.tensor_tensor(out=w2.t[:, :], in0=w1.t[:, :], in1=ex.t[:, :], op=ALU.mult), reads=[w1, ex], writes=[w2])
                c.op("dve", lambda e, lg=lg, t8=t8, w1=w1, c1=c1: e.tensor_scalar(out=c1.t[:, :], in0=lg.t[:, :], scalar1=t8.t[:, 0:1], scalar2=w1.t[:, 0:1], op0=ALU.is_equal, op1=ALU.mult),
                     reads=[lg, t8, w1], writes=[c1])
                c.op("dve", lambda e, lg=lg, t8=t8, w2=w2, c2=c2: e.tensor_scalar(out=c2.t[:, :], in0=lg.t[:, :], scalar1=t8.t[:, 1:2], scalar2=w2.t[:, 0:1], op0=ALU.is_equal, op1=ALU.mult),
                     reads=[lg, t8, w2], writes=[c2])
                ti = g * 4 + tt
                c.op("dve", lambda e, c1=c1, c2=c2, ti=ti: e.tensor_tensor(out=cst.t[:, ti, :], in0=c1.t[:, :], in1=c2.t[:, :], op=ALU.add), reads=[c1, c2], writes=[cst])
        c.dma("sp", [(comb_tm[:, :].rearrange("(t p) e -> p t e", p=128), cst.t[:, :, :])], reads=[cst])
        c.end()

    def ffn_phase(experts, F_, gcol_row0, use_comb):
        c.begin()
        NF = F_ // 128
        NTT = TS // 128
        NTG = TS // 512
        PF = 2
        PW2 = 7
        hts = c.sbuf("fht", [128, KC, TS], BF16)
        actT = c.sbuf("fact", [128, NF, TS], BF16)
        acc = c.sbuf("facc", [128, NTT, D], F32)
        gb = c.sbuf("fgb", [128, D], F32)
        c.dma("sp", [(gb.t[:, :], vecscr[gcol_row0:gcol_row0 + KC, :].rearrange("(o k) p -> o (k p)", o=1).partition_broadcast(128))], writes=[gb])
        wg_r = Rot([c.sbuf("fwg0", [128, KC, 2 * PF * 128], BF16), c.sbuf("fwg1", [128, KC, 2 * PF * 128], BF16)])
        w2_r = Rot([c.sbuf("fw20", [128, PW2, D], BF16), c.sbuf("fw21", [128, PW2, D], BF16)])
        cbt = c.sbuf("fcbt", [128, NTT, E], F32)
        sg_r = Rot([c.sbuf(f"fsg{i}", [128, 512], F32) for i in range(3)])
        xt_r = Rot([c.sbuf("fx0", [128, D], F32), c.sbuf("fx1", [128, D], F32)])
        pGU = Rot([c.psum(f"fpg{i}", [128, 512], F32) for i in range(4)])
        pY = Rot([c.psum(f"fpy{i}", [128, 512], F32) for i in range(3)])
        for st in range(S // TS):
            t0 = st * TS
            c.dma("sp", [(hts.t[:, :, :], hT[:, t0:t0 + TS].rearrange("(k p) s -> p k s", p=128))], writes=[hts])
            if use_comb:
                c.dma("sp", [(cbt.t[:, :, :], comb_tm[t0:t0 + TS, :].rearrange("(t p) e -> p t e", p=128))], writes=[cbt])
            for ei, (w13, w2) in enumerate(experts):
                for f0 in range(0, NF, PF):
                    nf = min(PF, NF - f0)
                    wg = wg_r.next()
                    c.dma("pool", [(wg.t[:, :, 0:nf * 128], w13[:, f0 * 128:(f0 + nf) * 128].rearrange("(k p) f -> p k f", p=128)),
                                   (wg.t[:, :, PF * 128:PF * 128 + nf * 128], w13[:, F_ + f0 * 128:F_ + (f0 + nf) * 128].rearrange("(k p) f -> p k f", p=128))], writes=[wg])
                    for j in range(nf):
                        fc = f0 + j
                        for tg in range(NTG):
                            pg, pu = pGU.next(), pGU.next()
                            for kc in range(KC):
                                c.op("pe", lambda e, pg=pg, wg=wg, j=j, kc=kc, tg=tg: e.matmul(pg.t[:, :], lhsT=wg.t[:, kc, j * 128:(j + 1) * 128], rhs=hts.t[:, kc, tg * 512:(tg + 1) * 512],
                                                                                              start=(kc == 0), stop=(kc == KC - 1)), reads=[wg, hts], writes=[pg])
                            for kc in range(KC):
                                c.op("pe", lambda e, pu=pu, wg=wg, j=j, kc=kc, tg=tg: e.matmul(pu.t[:, :], lhsT=wg.t[:, kc, (PF + j) * 128:(PF + j + 1) * 128], rhs=hts.t[:, kc, tg * 512:(tg + 1) * 512],
                                                                                              start=(kc == 0), stop=(kc == KC - 1)), reads=[wg, hts], writes=[pu])
                            sg = sg_r.next()
                            c.op("act", lambda e, pg=pg, sg=sg: e.activation(out=sg.t[:, :], in_=pg.t[:, :], func=AF.Silu), reads=[pg], writes=[sg])
                            sgc = sg
                            c.op("dve", lambda e, pu=pu, sgc=sgc, fc=fc, tg=tg: e.tensor_tensor(out=actT.t[:, fc, tg * 512:(tg + 1) * 512], in0=pu.t[:, :], in1=sgc.t[:, :], op=ALU.mult),
                                 reads=[pu, sgc], writes=[actT])
                for p0 in range(0, NF, PW2):
                    npc = min(PW2, NF - p0)
                    w2s = w2_r.next()
                    c.dma("pool", [(w2s.t[:, 0:npc, :], w2[p0 * 128:(p0 + npc) * 128, :].rearrange("(f p) d -> p f d", p=128))], writes=[w2s])
                    first = (ei == 0 and p0 == 0)
                    for tt in range(NTT):
                        for half in range(D // 512):
                            py = pY.next()
                            for j in range(npc):
                                c.op("pe", lambda e, py=py, j=j, p0=p0, tt=tt, half=half, w2s=w2s, npc=npc: e.matmul(py.t[:, :], lhsT=actT.t[:, p0 + j, tt * 128:(tt + 1) * 128],
                                                                                                                rhs=w2s.t[:, j, half * 512:(half + 1) * 512], start=(j == 0), stop=(j == npc - 1)),
                                     reads=[actT, w2s], writes=[py])
                            asl = acc.t[:, tt, half * 512:(half + 1) * 512]
                            if use_comb:
                                csc = cbt.t[:, tt, ei:ei + 1]
                                if first:
                                    c.op("act", lambda e, py=py, asl=asl, csc=csc: e.activation(out=asl, in_=py.t[:, :], func=AF.Copy, scale=csc), reads=[py, cbt], writes=[acc])
                                else:
                                    c.op("dve", lambda e, py=py, asl=asl, csc=csc: e.scalar_tensor_tensor(out=asl, in0=py.t[:, :], scalar=csc, in1=asl, op0=ALU.mult, op1=ALU.add),
                                         reads=[py, acc, cbt], writes=[acc])
                            elif first:
                                c.op("act", lambda e, py=py, asl=asl: e.activation(out=asl, in_=py.t[:, :], func=AF.Copy), reads=[py], writes=[acc])
                            else:
                                c.op("dve", lambda e, py=py, asl=asl: e.tensor_tensor(out=asl, in0=py.t[:, :], in1=asl, op=ALU.add), reads=[py, acc], writes=[acc])
            for tt in range(NTT):
                xt = xt_r.next()
                c.dma("sp", [(xt.t[:, :], xres[t0 + tt * 128:t0 + (tt + 1) * 128, :])], writes=[xt])
                c.op("pool", lambda e, tt=tt: e.tensor_tensor(out=acc.t[:, tt, :], in0=acc.t[:, tt, :], in1=gb.t[:, :], op=ALU.mult), reads=[acc, gb], writes=[acc])
                c.op("pool", lambda e, tt=tt, xt=xt: e.tensor_tensor(out=xt.t[:, :], in0=xt.t[:, :], in1=acc.t[:, tt, :], op=ALU.add), reads=[acc, xt], writes=[xt])
                c.dma("pool", [(xres[t0 + tt * 128:t0 + (tt + 1) * 128, :], xt.t[:, :])], reads=[xt])
        c.end()

    def final_phase():
        c.begin()
        gb = c.sbuf("zgb", [128, D], F32)
        c.dma("sp", [(gb.t[:, :], final_norm_g[0:1, :].partition_broadcast(128))], writes=[gb])
        xt_r = Rot([c.sbuf("zx0", [128, 4, D], F32), c.sbuf("zx1", [128, 4, D], F32)])
        junk = c.sbuf("zjunk", [128, D], BF16)
        ss_r = Rot([c.sbuf("zss0", [128, 4], F32), c.sbuf("zss1", [128, 4], F32)])
        sq_r = Rot([c.sbuf("zsq0", [128, 4], F32), c.sbuf("zsq1", [128, 4], F32)])
        rs_r = Rot([c.sbuf("zrs0", [128, 4], F32), c.sbuf("zrs1", [128, 4], F32)])
        epsc = c.sbuf("zeps", [128, 1], F32)
        c.op("pool", lambda e: e.memset(epsc.t[:, :], EPS), writes=[epsc])
        for g in range(NG):
            xt = xt_r.next()
            c.dma("sp", [(xt.t[:, :, :], xres[g * 512:(g + 1) * 512, :].rearrange("(t p) d -> p t d", p=128))], writes=[xt])
            ss, sq, rs = ss_r.next(), sq_r.next(), rs_r.next()
            for t in range(4):
                c.op("act", lambda e, t=t, xt=xt, ss=ss: e.activation(out=junk.t[:, :], in_=xt.t[:, t, :], func=AF.Square, accum_out=ss.t[:, t:t + 1]), reads=[xt], writes=[junk, ss])
            c.op("act", lambda e, ss=ss, sq=sq: e.activation(out=sq.t[:, :], in_=ss.t[:, :], func=AF.Sqrt, scale=1.0 / D, bias=epsc.t[:, 0:1]), reads=[ss, epsc], writes=[sq])
            c.op("dve", lambda e, sq=sq, rs=rs: e.reciprocal(out=rs.t[:, :], in_=sq.t[:, :]), reads=[sq], writes=[rs])
            for t in range(4):
                c.op("dve", lambda e, t=t, xt=xt, rs=rs: e.scalar_tensor_tensor(out=xt.t[:, t, :], in0=xt.t[:, t, :], scalar=rs.t[:, t:t + 1], in1=gb.t[:, :], op0=ALU.mult, op1=ALU.mult),
                     reads=[xt, rs, gb], writes=[xt])
            c.dma("pool", [(out_d[g * 512:(g + 1) * 512, :].rearrange("(t p) d -> p t d", p=128), xt.t[:, :, :])], reads=[xt])
        c.end()

    c.begin()
    vsx = c.vslot("xcopy")
    c.dma("sp", [(xres[:, :], x_in[:, :])], writes=[vsx])
    c.end()
    qdst = [(qT, j * 128) for j in range(KC)]
    kdst = [(kT, j * 128) for j in range(KC)]
    for l in range(4):
        norm_phase(xres, l * KC, col_mod(l, 0), hT)
        if l < 2:
            proj_phase(hT, a_wqkv[l], 2 * D, D, qdst + kdst, v65, set(range(KC)))
            moba_phase()
            wo_phase(a_wo[l], col_mod(l, 2))
        else:
            j = l - 2
            if j == 0:
                norm_phase(xres, 8 * KC, COL_KV, attnT)
                proj_phase(attnT, b_wkv, D, D, kdst, v65, set())
            proj_phase(hT, b_wq[j], D, 0, qdst, None, set(range(KC)))
            sb_phase()
            wo_phase(b_wo[j], col_mod(l, 2))
        norm_phase(xres, (4 + l) * KC, col_mod(l, 3), hT)
        if l % 2 == 0:
            ffn_phase([(ffn_w13[l // 2], ffn_w2[l // 2])], FF, col_mod(l, 5), False)
        else:
            router_phase(router_w[l // 2])
            ffn_phase([(moe_w13[l // 2, e], moe_w2[l // 2, e]) for e in range(E)], FE, col_mod(l, 5), True)
    final_phase()
    c.barrier()
    return c


_CACHE = {}


def kernel(**inputs):
    cfg = inputs.pop("_cfg", None) or CFG_FULL
    runner = inputs.pop("_runner", None)
    key = tuple(sorted(cfg.items()))
    if key not in _CACHE:
        _CACHE[key] = (build(cfg), make_consts(cfg))
    c, consts = _CACHE[key]
    S, D = cfg["S"], cfg["D"]
    x = np.asarray(inputs["x"], np.float32)
    B = x.shape[0]
    f = lambda k: np.ascontiguousarray(np.asarray(inputs[k], np.float32))
    shared = {
        "rel_bias": f("rel_bias"), "mod_w": f("mod_w"), "mod_b": f("mod_b").reshape(1, -1),
        "norm_mix_g": f("norm_mix_g"), "norm_ffn_g": f("norm_ffn_g"), "a_wqkv": f("a_wqkv"), "a_wo": f("a_wo"),
        "kv_norm_g": f("kv_norm_g").reshape(1, -1), "kv_mod_w": f("kv_mod_w"), "kv_mod_b": f("kv_mod_b").reshape(1, -1),
        "b_wkv": f("b_wkv"), "b_wq": f("b_wq"), "b_wo": f("b_wo"), "ffn_w13": f("ffn_w13"), "ffn_w2": f("ffn_w2"),
        "router_w": f("router_w"), "moe_w13": f("moe_w13"), "moe_w2": f("moe_w2"),
        "final_norm_g": f("final_norm_g").reshape(1, -1),
    }
    shared.update(consts)
    cc = np.asarray(inputs["c"], np.float32)
    in_maps = []
    for b in range(B):
        m = dict(shared)
        m["x"] = np.ascontiguousarray(x[b])
        m["c"] = np.ascontiguousarray(cc[b:b + 1])
        in_maps.append(m)
    if runner is not None:
        res = runner(c.nc, in_maps)
    else:
        res = run_bass_kernel_spmd(c.nc, in_maps, core_ids=list(range(B))).results
    return np.stack([np.asarray(r["out"], np.float32) for r in res], axis=0)
```

```python
import bisect
import math
from contextlib import ExitStack
import numpy as np
import ml_dtypes
import concourse.bass as bass
import concourse.mybir as mybir
from concourse.bass_utils import run_bass_kernel_spmd

F32 = mybir.dt.float32
BF16 = mybir.dt.bfloat16
ALU = mybir.AluOpType
AF = mybir.ActivationFunctionType
AX = mybir.AxisListType
SEM_ROT = 30000
BIG = 1.0e30
EPS = 1e-6

CFG_FULL = dict(S=4096, D=1024, H=16, FF=2816, FE=3584, E=8, TS=1024)


class Ev:
    __slots__ = ("eng", "idx", "ins", "sem", "val", "slot", "kind", "waited")

    def __init__(self, eng, idx, ins):
        self.eng, self.idx, self.ins = eng, idx, ins
        self.sem = None
        self.val = None
        self.slot = None
        self.kind = None
        self.waited = False


class Eng:
    def __init__(self, name, e):
        self.name, self.e = name, e
        self.n = 0
        self.sem = None
        self.cnt = 0
        self.mat_idx = []
        self.mat_ev = []
        self.seen = {}
        self.last = None


class Slot:
    def __init__(self, name, t=None):
        self.name = name
        self.t = t
        self.w = None
        self.r = {}
        self.sems = {}
        self.lastd = {}


class Ctx:
    def __init__(self):
        self.nc = bass.Bass("TRN2", target_bir_lowering=False)
        nc = self.nc
        self.root = ExitStack()
        self.stacks = [self.root]
        self.engs = {
            "pe": Eng("pe", nc.tensor),
            "act": Eng("act", nc.scalar),
            "dve": Eng("dve", nc.vector),
            "pool": Eng("pool", nc.gpsimd),
            "sp": Eng("sp", nc.sync),
        }
        self.nsem = 0
        self.nname = 0
        self.slots = []
        self.phase_slots = [[]]
        self.sempool = {"sp": [], "pool": [], "act": []}
        self.ninstr = 0

    def new_sem(self, name):
        self.nsem += 1
        return self.root.enter_context(self.nc.semaphore(f"{name}_{self.nsem}"))

    def _reg(self, s):
        self.slots.append(s)
        self.phase_slots[-1].append(s)
        return s

    def sbuf(self, name, shape, dt):
        self.nname += 1
        t = self.stacks[-1].enter_context(self.nc.sbuf_tensor(f"{name}_{self.nname}", list(shape), dt))
        return self._reg(Slot(name, t))

    def psum(self, name, shape, dt=F32):
        self.nname += 1
        t = self.stacks[-1].enter_context(self.nc.psum_tensor(f"{name}_{self.nname}", list(shape), dt))
        return self._reg(Slot(name, t))

    def vslot(self, name):
        return self._reg(Slot(name, None))

    def dram(self, name, shape, dt, kind="Internal"):
        return self.nc.dram_tensor(name, list(shape), dt, kind=kind)

    def begin(self):
        self.stacks.append(ExitStack())
        self.phase_slots.append([])

    def end(self):
        self.barrier()
        for s in self.phase_slots.pop():
            for (d_, q_), sc in s.sems.items():
                self.sempool[q_].append(sc)
            self.slots.remove(s)
        self.stacks.pop().close()

    def _getsem(self, q):
        if self.sempool[q]:
            return self.sempool[q].pop(0)
        return [self.new_sem("d" + q), 0]

    def _materialize(self, ev):
        if ev.val is not None:
            return
        eng = ev.eng
        i = bisect.bisect_left(eng.mat_idx, ev.idx)
        if i < len(eng.mat_idx):
            o = eng.mat_ev[i]
            ev.sem, ev.val = o.sem, o.val
            return
        if eng.sem is None or eng.cnt >= SEM_ROT:
            eng.sem = self.new_sem("p" + eng.name)
            eng.cnt = 0
        eng.cnt += 1
        ev.ins.then_inc(eng.sem, 1)
        ev.sem, ev.val = eng.sem, eng.cnt
        eng.mat_idx.append(ev.idx)
        eng.mat_ev.append(ev)

    def _wait(self, eng, ev):
        if ev.kind == "dma":
            ev.waited = True
        else:
            self._materialize(ev)
        k = id(ev.sem)
        if eng.seen.get(k, 0) >= ev.val:
            return
        eng.seen[k] = ev.val
        eng.e.wait_ge(ev.sem, ev.val)

    def _deps(self, eng, reads, writes, is_dma):
        deps = []
        for s in reads:
            if s.w is not None:
                if is_dma or s.w.kind == "dma" or s.w.eng is not eng or eng.name != "pe":
                    deps.append(s.w)
        for s in writes:
            if s.w is not None and (is_dma or s.w.kind == "dma" or s.w.eng is not eng or eng.name != "pe"):
                deps.append(s.w)
            for r in s.r.values():
                if is_dma or r.kind == "dma" or r.eng is not eng or eng.name != "pe":
                    deps.append(r)
        return deps

    def op(self, engname, fn, reads=(), writes=()):
        eng = self.engs[engname]
        for d in self._deps(eng, reads, writes, False):
            self._wait(eng, d)
        ins = fn(eng.e)
        self.ninstr += 1
        ev = Ev(eng, eng.n, ins)
        eng.n += 1
        eng.last = ev
        for s in reads:
            s.r[engname] = ev
        for s in writes:
            s.w = ev
            s.r = {}
        return ev

    def dma(self, qname, pairs, reads=(), writes=(), **kw):
        eng = self.engs[qname]
        for d in self._deps(eng, reads, writes, True):
            self._wait(eng, d)
        s = writes[0] if writes else reads[0]
        key = ("w" if writes else "r", qname)
        if key not in s.sems:
            s.sems[key] = self._getsem(qname)
        sc = s.sems[key]
        prev = s.lastd.get(key)
        sc[1] += 16 * len(pairs)
        if prev is not None and not prev.waited:
            prev.val = sc[1]
        ins = None
        for (o, i) in pairs:
            ins = eng.e.dma_start(out=o, in_=i, **kw).then_inc(sc[0], 16)
            self.ninstr += 1
        ev = Ev(eng, -1, ins)
        ev.kind = "dma"
        ev.sem, ev.val, ev.slot = sc[0], sc[1], s
        s.lastd[key] = ev
        for x in reads:
            x.r["dma"] = ev
        for x in writes:
            x.w = ev
            x.r = {}
        return ev

    def barrier(self):
        evs = [e.last for e in self.engs.values() if e.last is not None]
        dm = []
        for s in self.slots:
            dm.extend(s.sems.values())
        for ev in evs:
            self._materialize(ev)
        for e in self.engs.values():
            for ev in evs:
                if ev.eng is e:
                    continue
                k = id(ev.sem)
                if e.seen.get(k, 0) < ev.val:
                    e.seen[k] = ev.val
                    e.e.wait_ge(ev.sem, ev.val)
            for (sem, val) in dm:
                k = id(sem)
                if val > 0 and e.seen.get(k, 0) < val:
                    e.seen[k] = val
                    e.e.wait_ge(sem, val)
        for s in self.slots:
            s.w = None
            s.r = {}
            for ev in s.lastd.values():
                ev.waited = True


class Rot:
    def __init__(self, items):
        self.items = items
        self.i = 0

    def next(self):
        x = self.items[self.i % len(self.items)]
        self.i += 1
        return x


def rel_bucket_table(nu):
    import jax
    import jax.numpy as jnp
    with jax.default_device(jax.devices("cpu")[0]):
        dist = jnp.arange(nu, dtype=jnp.int32) - 512
        distc = jnp.maximum(dist, 0)
        max_exact = 16
        d = jnp.maximum(distc, 1).astype(jnp.float32)
        large = max_exact + (jnp.log(d / max_exact) / math.log(1024 / max_exact) * (32 - max_exact)).astype(jnp.int32)
        large = jnp.minimum(large, 31)
        b = np.asarray(jnp.where(distc < max_exact, distc, large))
        dist = np.asarray(dist)
    oh = np.zeros((33, nu), np.float32)
    for u in range(nu):
        if dist[u] < 0:
            oh[32, u] = 1.0
        else:
            oh[b[u], u] = 1.0
    return oh


def make_consts(cfg):
    S, H = cfg["S"], cfg["H"]
    NU = max(S + 512, 2048)
    NB = S // 256
    cs = {}
    cs["identb"] = np.eye(128).astype(ml_dtypes.bfloat16)
    cs["identf"] = np.eye(128, dtype=np.float32)
    cs["oh"] = rel_bucket_table(NU)
    cs["jex"] = np.ascontiguousarray(np.eye(128, dtype=np.float32)[::-1])
    es = np.zeros((16, 16 * 128), np.float32)
    for n in range(16):
        es[n, n * 128:(n + 1) * 128] = 1.0
    cs["esel"] = es.astype(ml_dtypes.bfloat16)
    e2 = np.zeros((16, S), np.float32)
    for n in range(min(16, S // 256)):
        e2[n, n * 256:(n + 1) * 256] = 1.0
    cs["esel2"] = e2.astype(ml_dtypes.bfloat16)
    j = np.arange(128)[:, None]
    k = np.arange(128)[None, :]
    cs["ustrict"] = (j > k).astype(np.float32).astype(ml_dtypes.bfloat16)
    m = np.arange(512 + 384)[None, :]
    kk = np.arange(128)[:, None]
    cs["cmask"] = ((m - 384 - kk) > 0).astype(np.float32).astype(ml_dtypes.bfloat16)
    return cs


def build(cfg):
    S, D, H, FF, FE, E, TS = (cfg[k] for k in ("S", "D", "H", "FF", "FE", "E", "TS"))
    KC = D // 128
    NT = S // 128
    NG = S // 512
    NB = S // 256
    NBP = max(NB, 8)
    NU = max(S + 512, 2048)
    WB = 1856
    DMAXNEAR = 896
    c = Ctx()
    nc = c.nc
    di = lambda n, sh, dt=F32: c.dram(n, sh, dt, kind="ExternalInput")
    x_in = di("x", [S, D])
    c_in = di("c", [1, D])
    rel_bias = di("rel_bias", [32, H])
    mod_w = di("mod_w", [4, D, 6 * D])
    mod_b = di("mod_b", [1, 4 * 6 * D])
    norm_mix_g = di("norm_mix_g", [4, D])
    norm_ffn_g = di("norm_ffn_g", [4, D])
    a_wqkv = di("a_wqkv", [2, D, 3 * D])
    a_wo = di("a_wo", [2, D, D])
    kv_norm_g = di("kv_norm_g", [1, D])
    kv_mod_w = di("kv_mod_w", [D, 2 * D])
    kv_mod_b = di("kv_mod_b", [1, 2 * D])
    b_wkv = di("b_wkv", [D, 2 * D])
    b_wq = di("b_wq", [2, D, D])
    b_wo = di("b_wo", [2, D, D])
    ffn_w13 = di("ffn_w13", [2, D, 2 * FF])
    ffn_w2 = di("ffn_w2", [2, FF, D])
    router_w = di("router_w", [2, D, E])
    moe_w13 = di("moe_w13", [2, E, D, 2 * FE])
    moe_w2 = di("moe_w2", [2, E, FE, D])
    final_norm_g = di("final_norm_g", [1, D])
    identb_d = di("identb", [128, 128], BF16)
    identf_d = di("identf", [128, 128])
    oh_d = di("oh", [33, NU])
    jex_d = di("jex", [128, 128])
    esel_d = di("esel", [16, 16 * 128], BF16)
    esel2_d = di("esel2", [16, S], BF16)
    ustrict_d = di("ustrict", [128, 128], BF16)
    cmask_d = di("cmask", [128, 896], BF16)
    out_d = c.dram("out", [S, D], F32, kind="ExternalOutput")

    xres = c.dram("xres", [S, D], F32)
    hT = c.dram("hT", [D, S], BF16)
    qT = c.dram("qT", [D, S], BF16)
    kT = c.dram("kT", [D, S], BF16)
    v65 = c.dram("v65", [S, H * 65], BF16)
    attnT = c.dram("attnT", [D, S], BF16)
    NMOD = 4 * 6 * D + 2 * D
    NVR = NMOD // 128 + 4 * KC + 4 * KC + KC
    vecscr = c.dram("vecscr", [NVR, 128], F32)
    ftab = c.dram("ftab", [H, NU], F32)
    biasT = c.dram("biasT", [H, 128, WB], BF16)
    comb_tm = c.dram("comb_tm", [S, E], F32)

    identb = c.sbuf("identb", [128, 128], BF16)
    identf = c.sbuf("identf", [128, 128], F32)
    modT = c.sbuf("modT", [128, NVR], F32)
    geff = c.sbuf("geff", [128, 9 * KC], F32)
    onesf = c.sbuf("onesf", [128, 128], F32)
    onesb = c.sbuf("onesb", [128, 128], BF16)
    c.dma("sp", [(identb.t[:, :], identb_d[:, :])], writes=[identb])
    c.dma("sp", [(identf.t[:, :], identf_d[:, :])], writes=[identf])
    c.op("pool", lambda e: e.memset(onesf.t[:, :], 1.0), writes=[onesf])
    c.op("pool", lambda e: e.memset(onesb.t[:, :], 1.0), writes=[onesb])

    def col_mod(l, j):
        return l * 6 * KC + j * KC
    COL_KV = 4 * 6 * KC
    COL_GMIX = NMOD // 128
    COL_GFFN = COL_GMIX + 4 * KC
    COL_GKV = COL_GFFN + 4 * KC

    c.begin()
    crow = c.sbuf("crow", [KC, 128], F32)
    cT = c.sbuf("cT", [128, KC], F32)
    scT = c.sbuf("scT", [128, KC], F32)
    mbrow = c.sbuf("mbrow", [1, NMOD], F32)
    pA = c.psum("pA", [128, 512], F32)
    pB = Rot([c.psum("pB0", [128, 512], F32), c.psum("pB1", [128, 512], F32)])
    mw = Rot([c.sbuf("mw0", [128, KC, 512], F32), c.sbuf("mw1", [128, KC, 512], F32)])
    mst = Rot([c.sbuf("mst0", [1, 512], F32), c.sbuf("mst1", [1, 512], F32)])
    vs = c.vslot("vecscr")
    c.dma("sp", [(crow.t[:, :], c_in[0:1, :].rearrange("o (k p) -> (o k) p", p=128))], writes=[crow])
    c.dma("sp", [(mbrow.t[0:1, 0:4 * 6 * D], mod_b[0:1, :]), (mbrow.t[0:1, 4 * 6 * D:NMOD], kv_mod_b[0:1, :])], writes=[mbrow])
    c.dma("pool", [(vecscr[COL_GMIX:COL_GMIX + 4 * KC, :], norm_mix_g[:, :].rearrange("l (k p) -> (l k) p", p=128)),
                   (vecscr[COL_GFFN:COL_GFFN + 4 * KC, :], norm_ffn_g[:, :].rearrange("l (k p) -> (l k) p", p=128)),
                   (vecscr[COL_GKV:COL_GKV + KC, :], kv_norm_g[0:1, :].rearrange("o (k p) -> (o k) p", p=128))],
          writes=[vs])
    c.op("pe", lambda e: e.transpose(out=pA.t[:, 0:KC], in_=crow.t[:, :], identity=identf.t[0:KC, 0:KC]), reads=[crow, identf], writes=[pA])
    c.op("dve", lambda e: e.tensor_copy(cT.t[:, :], pA.t[:, 0:KC]), reads=[pA], writes=[cT])
    c.op("act", lambda e: e.activation(out=scT.t[:, :], in_=cT.t[:, :], func=AF.Silu), reads=[cT], writes=[scT])
    blocks = [(mod_w[l], cb, l * 6 * D + cb * 512) for l in range(4) for cb in range(6 * D // 512)]
    blocks += [(kv_mod_w, cb, 4 * 6 * D + cb * 512) for cb in range(2 * D // 512)]
    for (wsrc, cb, off) in blocks:
        w = mw.next()
        c.dma("sp", [(w.t[:, :, :], wsrc[:, cb * 512:(cb + 1) * 512].rearrange("(k p) f -> p k f", p=128))], writes=[w])
        ps = pB.next()
        for kc in range(KC):
            c.op("pe", lambda e, kc=kc, w=w, ps=ps: e.matmul(ps.t[0:1, :], lhsT=scT.t[:, kc:kc + 1], rhs=w.t[:, kc, :],
                                                            start=(kc == 0), stop=(kc == KC - 1)), reads=[scT, w], writes=[ps])
        st = mst.next()
        c.op("dve", lambda e, st=st, ps=ps, off=off: e.tensor_tensor(out=st.t[0:1, :], in0=ps.t[0:1, :], in1=mbrow.t[0:1, off:off + 512], op=ALU.add),
             reads=[ps, mbrow], writes=[st])
        r0 = off // 128
        c.dma("sp", [(vecscr[r0:r0 + 4, :].rearrange("(o r) p -> o (r p)", o=1), st.t[0:1, :])], reads=[st], writes=[vs])
    c.barrier()
    r = 0
    vrow = Rot([c.sbuf("vrow0", [128, 128], F32), c.sbuf("vrow1", [128, 128], F32)])
    while r < NVR:
        n = min(128, NVR - r)
        vr = vrow.next()
        c.dma("sp", [(vr.t[0:n, :], vecscr[r:r + n, :])], writes=[vr])
        c.op("pe", lambda e, vr=vr, n=n: e.transpose(out=pA.t[:, 0:n], in_=vr.t[0:n, :], identity=identf.t[0:n, 0:n]), reads=[vr, identf], writes=[pA])
        c.op("dve", lambda e, r=r, n=n: e.tensor_copy(modT.t[:, r:r + n], pA.t[:, 0:n]), reads=[pA], writes=[modT])
        r += n
    for l in range(4):
        c.op("dve", lambda e, l=l: e.scalar_tensor_tensor(out=geff.t[:, l * KC:(l + 1) * KC], in0=modT.t[:, col_mod(l, 1):col_mod(l, 1) + KC], scalar=1.0,
                                                          in1=modT.t[:, COL_GMIX + l * KC:COL_GMIX + (l + 1) * KC], op0=ALU.add, op1=ALU.mult), reads=[modT], writes=[geff])
        c.op("dve", lambda e, l=l: e.scalar_tensor_tensor(out=geff.t[:, (4 + l) * KC:(5 + l) * KC], in0=modT.t[:, col_mod(l, 4):col_mod(l, 4) + KC], scalar=1.0,
                                                          in1=modT.t[:, COL_GFFN + l * KC:COL_GFFN + (l + 1) * KC], op0=ALU.add, op1=ALU.mult), reads=[modT], writes=[geff])
    c.op("dve", lambda e: e.scalar_tensor_tensor(out=geff.t[:, 8 * KC:9 * KC], in0=modT.t[:, COL_KV + KC:COL_KV + 2 * KC], scalar=1.0,
                                                 in1=modT.t[:, COL_GKV:COL_GKV + KC], op0=ALU.add, op1=ALU.mult), reads=[modT], writes=[geff])
    c.end()

    c.begin()
    rbA = c.sbuf("rbA", [33, H], F32)
    ohs = c.sbuf("ohs", [33, NU], F32)
    fsb = c.sbuf("fsb", [H, NU], F32)
    jex = c.sbuf("jex", [128, 128], F32)
    pF = Rot([c.psum("pF0", [128, 512], F32), c.psum("pF1", [128, 512], F32)])
    c.op("pool", lambda e: e.memset(rbA.t[32:33, :], -BIG), writes=[rbA])
    c.dma("sp", [(rbA.t[0:32, :], rel_bias[:, :])], writes=[rbA])
    c.dma("sp", [(ohs.t[:, :], oh_d[:, :])], writes=[ohs])
    c.dma("sp", [(jex.t[:, :], jex_d[:, :])], writes=[jex])
    for ch in range(NU // 512):
        ps = pF.next()
        c.op("pe", lambda e, ps=ps, ch=ch: e.matmul(ps.t[0:H, :], lhsT=rbA.t[:, 0:H], rhs=ohs.t[:, ch * 512:(ch + 1) * 512], start=True, stop=True),
             reads=[rbA, ohs], writes=[ps])
        c.op("act", lambda e, ps=ps, ch=ch: e.activation(out=fsb.t[:, ch * 512:(ch + 1) * 512], in_=ps.t[0:H, :], func=AF.Copy), reads=[ps], writes=[fsb])
    c.dma("sp", [(ftab[:, :], fsb.t[:, :])], reads=[fsb])
    c.barrier()
    t2 = Rot([c.sbuf("t2a", [128, WB], F32), c.sbuf("t2b", [128, WB], F32)])
    bst = Rot([c.sbuf("bsta", [128, WB], BF16), c.sbuf("bstb", [128, WB], BF16)])
    for h in range(H):
        t = t2.next()
        src = bass.AP(tensor=ftab, offset=h * NU + 1, ap=[[1, 128], [1, WB]])
        c.dma("sp", [(t.t[:, :], src)], writes=[t])
        bs = bst.next()
        for ch in range(0, WB, 512):
            w_ = min(512, WB - ch)
            ps = pF.next()
            c.op("pe", lambda e, ps=ps, t=t, ch=ch, w_=w_: e.matmul(ps.t[:, 0:w_], lhsT=jex.t[:, :], rhs=t.t[:, ch:ch + w_], start=True, stop=True),
                 reads=[jex, t], writes=[ps])
            c.op("act", lambda e, ps=ps, bs=bs, ch=ch, w_=w_: e.activation(out=bs.t[:, ch:ch + w_], in_=ps.t[:, 0:w_], func=AF.Copy), reads=[ps], writes=[bs])
        c.dma("sp", [(biasT[h], bs.t[:, :])], reads=[bs])
    c.end()

    def norm_phase(src, gcol0, shcol0, dst):
        c.begin()
        xt_r = Rot([c.sbuf("nx0", [128, 4, D], F32), c.sbuf("nx1", [128, 4, D], F32)])
        junk = c.sbuf("njunk", [128, D], BF16)
        ss_r = Rot([c.sbuf("nss0", [128, 4], F32), c.sbuf("nss1", [128, 4], F32)])
        sq_r = Rot([c.sbuf("nsq0", [128, 4], F32), c.sbuf("nsq1", [128, 4], F32)])
        rs_r = Rot([c.sbuf("nrs0", [128, 4], F32), c.sbuf("nrs1", [128, 4], F32)])
        xn_r = Rot([c.sbuf("nxn0", [128, 4, D], BF16), c.sbuf("nxn1", [128, 4, D], BF16)])
        ht_r = Rot([c.sbuf("nht0", [128, KC, 512], BF16), c.sbuf("nht1", [128, KC, 512], BF16)])
        pt_r = Rot([c.psum("npt0", [128, 512], BF16), c.psum("npt1", [128, 512], BF16), c.psum("npt2", [128, 512], BF16)])
        epsc = c.sbuf("epsc", [128, 1], F32)
        c.op("pool", lambda e: e.memset(epsc.t[:, :], EPS), writes=[epsc])
        for g in range(NG):
            xt = xt_r.next()
            c.dma("sp", [(xt.t[:, :, :], src[g * 512:(g + 1) * 512, :].rearrange("(t p) d -> p t d", p=128))], writes=[xt])
            ss, sq, rs, xn, ht = ss_r.next(), sq_r.next(), rs_r.next(), xn_r.next(), ht_r.next()
            for t in range(4):
                c.op("act", lambda e, t=t, xt=xt, ss=ss: e.activation(out=junk.t[:, :], in_=xt.t[:, t, :], func=AF.Square, accum_out=ss.t[:, t:t + 1]),
                     reads=[xt], writes=[junk, ss])
            c.op("act", lambda e, ss=ss, sq=sq: e.activation(out=sq.t[:, :], in_=ss.t[:, :], func=AF.Sqrt, scale=1.0 / D, bias=epsc.t[:, 0:1]), reads=[ss, epsc], writes=[sq])
            c.op("dve", lambda e, sq=sq, rs=rs: e.reciprocal(out=rs.t[:, :], in_=sq.t[:, :]), reads=[sq], writes=[rs])
            for t in range(4):
                c.op("act", lambda e, t=t, xt=xt, xn=xn, rs=rs: e.activation(out=xn.t[:, t, :], in_=xt.t[:, t, :], func=AF.Copy, scale=rs.t[:, t:t + 1]),
                     reads=[xt, rs], writes=[xn])
            for kc in range(KC):
                pt = pt_r.next()
                for t in range(4):
                    c.op("pe", lambda e, t=t, kc=kc, pt=pt, xn=xn: e.transpose(out=pt.t[:, t * 128:(t + 1) * 128], in_=xn.t[:, t, kc * 128:(kc + 1) * 128], identity=identb.t[:, :]),
                         reads=[xn, identb], writes=[pt])
                c.op("dve", lambda e, kc=kc, pt=pt, ht=ht: e.tensor_scalar(out=ht.t[:, kc, :], in0=pt.t[:, :], scalar1=geff.t[:, gcol0 + kc:gcol0 + kc + 1],
                                                                           scalar2=modT.t[:, shcol0 + kc:shcol0 + kc + 1], op0=ALU.mult, op1=ALU.add),
                     reads=[pt, geff, modT], writes=[ht])
            c.dma("pool", [(dst[:, g * 512:(g + 1) * 512].rearrange("(k p) s -> p k s", p=128), ht.t[:, :, :])], reads=[ht])
        c.end()

    def load_w_bf16(dst_slot, dst_ap_fn, src2d, ncols, piece=1024):
        pairs = []
        for c0 in range(0, ncols, piece):
            w_ = min(piece, ncols - c0)
            pairs.append((dst_ap_fn(c0, w_), src2d[:, c0:c0 + w_].rearrange("(k p) f -> p k f", p=128)))
        c.dma("pool", pairs, writes=[dst_slot])

    def proj_phase(src_hT, w2d, ncols_fm, ncols_tm, fm_dsts, tm_dst65, q_scale_chunks):
        c.begin()
        ncols = ncols_fm + ncols_tm
        wsb = c.sbuf("pw", [128, KC, ncols], BF16)
        load_w_bf16(wsb, lambda c0, w_: wsb.t[:, :, c0:c0 + w_], w2d, ncols)
        ht_r = Rot([c.sbuf("pht0", [128, KC, 512], BF16), c.sbuf("pht1", [128, KC, 512], BF16)])
        NJ = ncols_fm // 128
        st_r = Rot([c.sbuf("pst0", [128, max(NJ, 1), 512], BF16), c.sbuf("pst1", [128, max(NJ, 1), 512], BF16)])
        ps_r = Rot([c.psum(f"pps{i}", [128, 512], F32) for i in range(4)])
        if ncols_tm:
            vst_r = Rot([c.sbuf("pvs0", [128, 4, H, 65], BF16), c.sbuf("pvs1", [128, 4, H, 65], BF16)])
            for v in vst_r.items:
                c.op("pool", lambda e, v=v: e.memset(v.t[:, :, :, :], 1.0), writes=[v])
        for g in range(NG):
            ht = ht_r.next()
            c.dma("sp", [(ht.t[:, :, :], src_hT[:, g * 512:(g + 1) * 512].rearrange("(k p) s -> p k s", p=128))], writes=[ht])
            if NJ:
                st = st_r.next()
                for j in range(NJ):
                    ps = ps_r.next()
                    for kc in range(KC):
                        c.op("pe", lambda e, ps=ps, j=j, kc=kc, ht=ht: e.matmul(ps.t[:, :], lhsT=wsb.t[:, kc, j * 128:(j + 1) * 128], rhs=ht.t[:, kc, :],
                                                                               start=(kc == 0), stop=(kc == KC - 1)), reads=[wsb, ht], writes=[ps])
                    if j in q_scale_chunks:
                        c.op("act", lambda e, ps=ps, st=st, j=j: e.activation(out=st.t[:, j, :], in_=ps.t[:, :], func=AF.Copy, scale=0.125), reads=[ps], writes=[st])
                    else:
                        c.op("dve", lambda e, ps=ps, st=st, j=j: e.tensor_copy(st.t[:, j, :], ps.t[:, :]), reads=[ps], writes=[st])
                pairs = []
                for j in range(NJ):
                    dt_, r0 = fm_dsts[j]
                    pairs.append((dt_[r0:r0 + 128, g * 512:(g + 1) * 512], st.t[:, j, :]))
                c.dma("pool", pairs, reads=[st])
            if ncols_tm:
                vst = vst_r.next()
                for tt in range(4):
                    for half in range(ncols_tm // 512):
                        ps = ps_r.next()
                        for kc in range(KC):
                            c.op("pe", lambda e, ps=ps, kc=kc, ht=ht, tt=tt, half=half: e.matmul(ps.t[:, :], lhsT=ht.t[:, kc, tt * 128:(tt + 1) * 128],
                                                                                                rhs=wsb.t[:, kc, ncols_fm + half * 512:ncols_fm + (half + 1) * 512],
                                                                                                start=(kc == 0), stop=(kc == KC - 1)), reads=[wsb, ht], writes=[ps])
                        c.op("act", lambda e, ps=ps, vst=vst, tt=tt, half=half: e.activation(out=vst.t[:, tt, half * 8:(half + 1) * 8, 0:64],
                                                                                             in_=ps.t[:, :].rearrange("p (h d) -> p h d", d=64), func=AF.Copy),
                             reads=[ps], writes=[vst])
                c.dma("pool", [(tm_dst65[g * 512:(g + 1) * 512, :].rearrange("(t p) f -> p t f", p=128), vst.t[:, :, :, :].rearrange("p t h d -> p t (h d)"))], reads=[vst])
        c.end()

    def wo_phase(w2d, gcol_row0):
        c.begin()
        wsb = c.sbuf("ow", [128, KC, D], BF16)
        load_w_bf16(wsb, lambda c0, w_: wsb.t[:, :, c0:c0 + w_], w2d, D)
        gb = c.sbuf("ogb", [128, D], F32)
        c.dma("sp", [(gb.t[:, :], vecscr[gcol_row0:gcol_row0 + KC, :].rearrange("(o k) p -> o (k p)", o=1).partition_broadcast(128))], writes=[gb])
        at_r = Rot([c.sbuf("oat0", [128, KC, 512], BF16), c.sbuf("oat1", [128, KC, 512], BF16)])
        xt_r = Rot([c.sbuf("ox0", [128, 4, D], F32), c.sbuf("ox1", [128, 4, D], F32)])
        tmp_r = Rot([c.sbuf("otm0", [128, 512], F32), c.sbuf("otm1", [128, 512], F32)])
        ps_r = Rot([c.psum(f"ops{i}", [128, 512], F32) for i in range(4)])
        for g in range(NG):
            at = at_r.next()
            xt = xt_r.next()
            c.dma("sp", [(at.t[:, :, :], attnT[:, g * 512:(g + 1) * 512].rearrange("(k p) s -> p k s", p=128))], writes=[at])
            c.dma("sp", [(xt.t[:, :, :], xres[g * 512:(g + 1) * 512, :].rearrange("(t p) d -> p t d", p=128))], writes=[xt])
            for tt in range(4):
                for half in range(D // 512):
                    ps = ps_r.next()
                    for kc in range(KC):
                        c.op("pe", lambda e, ps=ps, kc=kc, at=at, tt=tt, half=half: e.matmul(ps.t[:, :], lhsT=at.t[:, kc, tt * 128:(tt + 1) * 128],
                                                                                            rhs=wsb.t[:, kc, half * 512:(half + 1) * 512],
                                                                                            start=(kc == 0), stop=(kc == KC - 1)), reads=[wsb, at], writes=[ps])
                    tmp = tmp_r.next()
                    c.op("dve", lambda e, ps=ps, tmp=tmp, half=half: e.tensor_tensor(out=tmp.t[:, :], in0=ps.t[:, :], in1=gb.t[:, half * 512:(half + 1) * 512], op=ALU.mult),
                         reads=[ps, gb], writes=[tmp])
                    c.op("pool", lambda e, tmp=tmp, xt=xt, tt=tt, half=half: e.tensor_tensor(out=xt.t[:, tt, half * 512:(half + 1) * 512], in0=xt.t[:, tt, half * 512:(half + 1) * 512],
                                                                                           in1=tmp.t[:, :], op=ALU.add), reads=[tmp, xt], writes=[xt])
            c.dma("pool", [(xres[g * 512:(g + 1) * 512, :].rearrange("(t p) d -> p t d", p=128), xt.t[:, :, :])], reads=[xt])
        c.end()

    def bc_last(ap2d, n):
        a = [list(x) for x in ap2d.ap]
        return bass.AP(tensor=ap2d.tensor, offset=ap2d.offset, ap=a + [[0, n]])

    def moba_phase():
        c.begin()
        NGN = NT * NB
        vall = c.sbuf("mvall", [128, NT, H * 65], BF16)
        c.dma("sp", [(vall.t[:, :, :], v65[:, :].rearrange("(t p) f -> p t f", p=128))], writes=[vall])
        qp_r = Rot([c.sbuf("mq0", [128, S], BF16), c.sbuf("mq1", [128, S], BF16)])
        kp_r = Rot([c.sbuf("mk0", [128, S], BF16), c.sbuf("mk1", [128, S], BF16)])
        for t_ in qp_r.items + kp_r.items:
            c.op("pool", lambda e, t_=t_: e.memset(t_.t[:, :], 0.0), writes=[t_])
        for t_ in kp_r.items:
            c.dma("sp", [(t_.t[64:64 + NB, :], esel2_d[0:NB, :])], writes=[t_])
        bt_r = Rot([c.sbuf("mbt0", [128, WB], BF16), c.sbuf("mbt1", [128, WB], BF16)])
        b31_r = Rot([c.sbuf("mb31a", [128, 1], F32), c.sbuf("mb31b", [128, 1], F32)])
        km = c.sbuf("mkm", [128, NB], F32)
        kmb_r = Rot([c.sbuf("mkmb0", [128, NB], BF16), c.sbuf("mkmb1", [128, NB], BF16)])
        for t_ in kmb_r.items:
            c.op("pool", lambda e, t_=t_: e.memset(t_.t[:, :], 0.0), writes=[t_])
        VM = c.sbuf("mVM", [128, NT, NB], F32)
        NV = c.sbuf("mNV", [128, NT, NB], F32)
        TM = c.sbuf("mTM", [128, NT, NB], F32)
        gmA = c.sbuf("mgmA", [128, NGN], F32)
        gmB = c.sbuf("mgmB", [128, NGN], F32)
        gmC = c.sbuf("mgmC", [128, NGN], F32)
        tA = c.sbuf("mtA", [128, NGN], F32)
        mx = c.sbuf("mmx", [128, NT], F32)
        mvs = c.sbuf("mmvs", [128, NGN], F32)
        sb_r = Rot([c.sbuf(f"msb{i}", [128, 512], F32) for i in range(3)])
        a_r = Rot([c.sbuf(f"ma{i}", [128, 512], BF16) for i in range(4)])
        rsb_r = Rot([c.sbuf("mrs0", [128, 512], F32), c.sbuf("mrs1", [128, 512], F32)])
        for t_ in rsb_r.items:
            c.op("pool", lambda e, t_=t_: e.memset(t_.t[:, :], 0.0), writes=[t_])
        sel64 = c.sbuf("msel64", [128, 64], F32)
        c.op("pool", lambda e: e.memset(sel64.t[:, :], 0.0), writes=[sel64])
        c.op("pool", lambda e: e.memset(sel64.t[64:65, :], 1.0), writes=[sel64])
        rb_r = Rot([c.sbuf("mrb0", [64, 512], F32), c.sbuf("mrb1", [64, 512], F32)])
        ast_r = Rot([c.sbuf("mas0", [64, 512], BF16), c.sbuf("mas1", [64, 512], BF16)])
        pS = Rot([c.psum(f"mpS{i}", [128, 512], F32) for i in range(3)])
        pO = Rot([c.psum("mpO0", [128, 512], F32), c.psum("mpO1", [128, 512], F32)])
        pG = c.psum("mpG", [128, 512], F32)
        pX = c.psum("mpX", [128, 512], F32)
        pR = c.psum("mpR", [128, 512], F32)
        c.op("pool", lambda e: e.memset(VM.t[:, :, :], -BIG), writes=[VM])
        c.op("pool", lambda e: e.memset(NV.t[:, :, :], 0.0), writes=[NV])
        c.op("pool", lambda e: e.memset(TM.t[:, :, :], -BIG), writes=[TM])
        for i in range(NT):
            cur = i // 2
            if cur > 0:
                c.op("pool", lambda e, i=i, cur=cur: e.memset(VM.t[:, i, 0:cur], 0.0), writes=[VM])
                c.op("pool", lambda e, i=i, cur=cur: e.memset(NV.t[:, i, 0:cur], -BIG), writes=[NV])
            c.op("pool", lambda e, i=i, cur=cur: e.memset(TM.t[:, i, 0:cur + 1], 0.0), writes=[TM])
        v3 = lambda s_: s_.t[:, :].rearrange("p (i n) -> p i n", n=NB)
        state = {}

        def prepass(h):
            qp, kp, kmb = qp_r.next(), kp_r.next(), kmb_r.next()
            c.dma("sp", [(qp.t[0:64, :], qT[h * 64:h * 64 + 64, :])], writes=[qp])
            c.dma("sp", [(kp.t[0:64, :], kT[h * 64:h * 64 + 64, :])], writes=[kp])
            c.op("dve", lambda e: e.tensor_reduce(out=km.t[0:64, :], in_=kp.t[0:64, :].rearrange("p (n b) -> p n b", b=256), axis=AX.X, op=ALU.add), reads=[kp], writes=[km])
            c.op("dve", lambda e: e.tensor_scalar(out=kmb.t[0:64, :], in0=km.t[0:64, :], scalar1=1.0 / 256, scalar2=None, op0=ALU.mult), reads=[km], writes=[kmb])
            pb = 0
            bt = bt_r.next()
            c.dma("sp", [(bt.t[:, :], biasT[h])], writes=[bt])
            b31 = b31_r.next()
            c.dma("sp", [(b31.t[:, :], ftab[h:h + 1, 1512:1513].partition_broadcast(128))], writes=[b31])
            maskT = qp
            for i in range(NT):
                c.op("pe", lambda e, i=i: e.matmul(pG.t[:, i * NB:(i + 1) * NB], lhsT=qp.t[:, i * 128:(i + 1) * 128], rhs=kmb.t[:, 0:NB], start=True, stop=True),
                     reads=[qp, kmb], writes=[pG])
            c.op("dve", lambda e: e.tensor_tensor(out=gmA.t[:, :], in0=pG.t[:, 0:NGN], in1=VM.t[:, :, :].rearrange("p i n -> p (i n)"), op=ALU.add), reads=[pG, VM], writes=[gmA])
            src = gmA
            for (dst,) in ((gmB,), (gmC,)):
                c.op("dve", lambda e, src=src: e.tensor_reduce(out=mx.t[:, :], in_=v3(src), axis=AX.X, op=ALU.max), reads=[src], writes=[mx])
                c.op("dve", lambda e, src=src: e.tensor_tensor(out=v3(tA), in0=v3(src), in1=bc_last(mx.t[:, :], NB), op=ALU.is_ge), reads=[src, mx], writes=[tA])
                c.op("dve", lambda e, src=src, dst=dst: e.scalar_tensor_tensor(out=dst.t[:, :], in0=tA.t[:, :], scalar=-BIG, in1=src.t[:, :], op0=ALU.mult, op1=ALU.add),
                     reads=[tA, src], writes=[dst])
                src = dst
            c.op("dve", lambda e: e.tensor_reduce(out=mx.t[:, :], in_=v3(gmC), axis=AX.X, op=ALU.max), reads=[gmC], writes=[mx])
            c.op("dve", lambda e: e.tensor_tensor(out=v3(tA), in0=v3(gmA), in1=bc_last(mx.t[:, :], NB), op=ALU.is_lt), reads=[gmA, mx], writes=[tA])
            c.op("dve", lambda e: e.tensor_tensor(out=tA.t[:, :], in0=tA.t[:, :], in1=NV.t[:, :, :].rearrange("p i n -> p (i n)"), op=ALU.mult), reads=[tA, NV], writes=[tA])
            c.op("dve", lambda e: e.tensor_tensor(out=mvs.t[:, :], in0=tA.t[:, :], in1=TM.t[:, :, :].rearrange("p i n -> p (i n)"), op=ALU.add), reads=[tA, TM], writes=[mvs])
            for i0 in range(0, NT, 4):
                for i in range(i0, i0 + 4):
                    c.op("pe", lambda e, i=i, i0=i0: e.transpose(out=pX.t[0:NB, (i - i0) * 128:(i - i0 + 1) * 128], in_=mvs.t[:, i * NB:(i + 1) * NB], identity=identf.t[:, :]),
                         reads=[mvs, identf], writes=[pX])
                c.op("act", lambda e, i0=i0: e.activation(out=qp.t[64:64 + NB, i0 * 128:(i0 + 4) * 128], in_=pX.t[0:NB, 0:512], func=AF.Copy), reads=[pX], writes=[qp])
            return (qp, kp, bt, b31, maskT, pb)

        def main(h, ctxh):
            qp, kp, bt, b31, maskT, pb = ctxh
            tiles = [(Q, kt) for Q in range(NG) for kt in range(4 * Q + 4)]
            T = {}
            pos = {}
            for step in range(len(tiles) + 3):
                j = step - 2
                if 0 <= j < len(tiles):
                    Q, kt = tiles[j]
                    nkt = 4 * Q + 4
                    a = T.pop(j)["a"]
                    if kt == 0:
                        pos[Q] = pO.next()
                    po = pos[Q]
                    c.op("pe", lambda e, po=po, a=a, kt=kt, nkt=nkt: e.matmul(po.t[0:65, :], lhsT=vall.t[:, kt, h * 65:(h + 1) * 65], rhs=a.t[:, :],
                                                                            start=(kt == 0), stop=(kt == nkt - 1)), reads=[vall, a], writes=[po])
                    if kt == nkt - 1:
                        rsb = rsb_r.next()
                        c.op("dve", lambda e, po=po, rsb=rsb: e.reciprocal(out=rsb.t[64:65, :], in_=po.t[64:65, :]), reads=[po], writes=[rsb])
                        c.op("pe", lambda e, rsb=rsb: e.matmul(pR.t[0:64, :], lhsT=sel64.t[:, :], rhs=rsb.t[:, :], start=True, stop=True), reads=[sel64, rsb], writes=[pR])
                        rb = rb_r.next()
                        c.op("dve", lambda e, rb=rb: e.tensor_copy(rb.t[:, :], pR.t[0:64, :]), reads=[pR], writes=[rb])
                        ast = ast_r.next()
                        c.op("dve", lambda e, po=po, rb=rb, ast=ast: e.tensor_tensor(out=ast.t[:, :], in0=po.t[0:64, :], in1=rb.t[:, :], op=ALU.mult), reads=[po, rb], writes=[ast])
                        c.dma("pool", [(attnT[h * 64:(h + 1) * 64, Q * 512:(Q + 1) * 512], ast.t[:, :])], reads=[ast])
                j = step - 1
                if 0 <= j < len(tiles):
                    Q, kt = tiles[j]
                    a = a_r.next()
                    ps = T[j].pop("ps")
                    T[j].pop("near")
                    c.op("act", lambda e, ps=ps, a=a: e.activation(out=a.t[:, :], in_=ps.t[:, :], func=AF.Exp), reads=[ps], writes=[a])
                    T[j]["a"] = a
                j = step
                if j < len(tiles):
                    Q, kt = tiles[j]
                    delta = 512 * Q - 128 * kt
                    near = delta <= DMAXNEAR
                    ps = pS.next()
                    c.op("pe", lambda e, ps=ps, kt=kt, Q=Q, near=near: e.matmul(ps.t[:, :], lhsT=kp.t[:, kt * 128:(kt + 1) * 128], rhs=qp.t[:, Q * 512:(Q + 1) * 512],
                                                                              start=True, stop=False), reads=[kp, qp], writes=[ps])
                    o = (delta + 384) if near else 1330
                    c.op("pe", lambda e, ps=ps, o=o: e.matmul(ps.t[:, :], lhsT=identb.t[:, :], rhs=bt.t[:, o:o + 512], start=False, stop=True), reads=[identb, bt], writes=[ps])
                    T[j] = {"ps": ps, "near": near}

        nxt = prepass(0)
        for h in range(H):
            cur_ctx = nxt
            if h + 1 < H:
                nxt = prepass(h + 1)
            main(h, cur_ctx)
        c.end()

    def sb_phase():
        c.begin()
        vall = c.sbuf("svall", [128, NT, H * 65], BF16)
        c.dma("sp", [(vall.t[:, :, :], v65[:, :].rearrange("(t p) f -> p t f", p=128))], writes=[vall])
        ustr = c.sbuf("sustr", [128, 128], BF16)
        cm = c.sbuf("scm", [128, 896], BF16)
        c.dma("sp", [(ustr.t[:, :], ustrict_d[:, :])], writes=[ustr])
        c.dma("sp", [(cm.t[:, :], cmask_d[:, :])], writes=[cm])
        qp_r = Rot([c.sbuf("sq0", [128, S], BF16), c.sbuf("sq1", [128, S], BF16)])
        kp_r = Rot([c.sbuf("sk0", [128, S], BF16), c.sbuf("sk1", [128, S], BF16)])
        for t_ in qp_r.items + kp_r.items:
            c.op("pool", lambda e, t_=t_: e.memset(t_.t[:, :], 0.0), writes=[t_])
        e_r = Rot([c.sbuf(f"se{i}", [128, 512], F32) for i in range(2)])
        lp_r = Rot([c.sbuf(f"slp{i}", [128, 512], BF16) for i in range(5)])
        ln_r = Rot([c.sbuf(f"sln{i}", [128, 512], BF16) for i in range(7)])
        a_r = Rot([c.sbuf(f"sa{i}", [128, 512], BF16) for i in range(4)])
        tot_r = Rot([c.sbuf(f"stot{i}", [128, 512], BF16) for i in range(4)])
        ast_r = Rot([c.sbuf("sas0", [64, 512], BF16), c.sbuf("sas1", [64, 512], BF16)])
        pZ = Rot([c.psum(f"spZ{i}", [128, 512], F32) for i in range(3)])
        pT = Rot([c.psum("spT0", [128, 512], F32), c.psum("spT1", [128, 512], F32)])
        pTOT = Rot([c.psum("spTOT0", [128, 512], F32)])
        pO = Rot([c.psum("spO0", [128, 512], F32), c.psum("spO1", [128, 512], F32)])
        for h in range(H):
            qp, kp = qp_r.next(), kp_r.next()
            c.dma("sp", [(qp.t[0:64, :], qT[h * 64:h * 64 + 64, :])], writes=[qp])
            c.dma("sp", [(kp.t[0:64, :], kT[h * 64:h * 64 + 64, :])], writes=[kp])
            tiles = [(Q, idx, 4 * Q + 3 - idx) for Q in range(NG) for idx in range(4 * Q + 4)]
            T = {}
            qstate = {}
            NST = 6
            for step in range(len(tiles) + NST):
                j = step - 5
                if 0 <= j < len(tiles):
                    Q, idx, kt = tiles[j]
                    nkt = 4 * Q + 4
                    a = T[j].pop("a")
                    po = qstate[Q]["po"]
                    c.op("pe", lambda e, po=po, a=a, kt=kt, idx=idx, nkt=nkt: e.matmul(po.t[0:64, :], lhsT=vall.t[:, kt, h * 65:h * 65 + 64], rhs=a.t[:, :],
                                                                                     start=(idx == 0), stop=(idx == nkt - 1)), reads=[vall, a], writes=[po])
                    if idx == nkt - 1:
                        ast = ast_r.next()
                        c.op("dve", lambda e, po=po, ast=ast: e.tensor_copy(ast.t[:, :], po.t[0:64, :]), reads=[po], writes=[ast])
                        c.dma("pool", [(attnT[h * 64:(h + 1) * 64, Q * 512:(Q + 1) * 512], ast.t[:, :])], reads=[ast])
                    del T[j]
                j = step - 4
                if 0 <= j < len(tiles):
                    Q, idx, kt = tiles[j]
                    pt = T[j].pop("pt")
                    a = a_r.next()
                    c.op("act", lambda e, pt=pt, a=a: e.activation(out=a.t[:, :], in_=pt.t[:, :], func=AF.Exp, scale=-1.0), reads=[pt], writes=[a])
                    if kt >= 4 * Q:
                        o = 512 * Q - 128 * kt + 384
                        c.op("pool", lambda e, a=a, o=o: e.tensor_tensor(out=a.t[:, :], in0=a.t[:, :], in1=cm.t[:, o:o + 512], op=ALU.mult), reads=[a, cm], writes=[a])
                    T[j]["a"] = a
                    if T[j].pop("ck", False):
                        qs = qstate[Q]
                        tsb = tot_r.next()
                        ptot = qs["ptot"]
                        c.op("dve", lambda e, ptot=ptot, tsb=tsb: e.tensor_copy(tsb.t[:, :], ptot.t[:, :]), reads=[ptot], writes=[tsb])
                        qs["ck"][idx + 1] = tsb
                j = step - 3
                if 0 <= j < len(tiles):
                    Q, idx, kt = tiles[j]
                    nkt = 4 * Q + 4
                    ln = T[j].pop("ln")
                    lp = T[j].pop("lp")
                    if idx == 0:
                        qstate[Q] = {"ptot": pTOT.next(), "ck": {}, "po": pO.next(), "ln": {}}
                    qs = qstate[Q]
                    qs["ln"][idx] = ln
                    n2 = max(0, idx - 2)
                    direct = list(range(n2, idx))
                    pt = pT.next()
                    c.op("pe", lambda e, pt=pt, ln=ln: e.matmul(pt.t[:, :], lhsT=ustr.t[:, :], rhs=ln.t[:, :], start=True, stop=False), reads=[ustr, ln], writes=[pt])
                    for dj in direct:
                        lnd = qs["ln"][dj]
                        c.op("pe", lambda e, pt=pt, lnd=lnd: e.matmul(pt.t[:, :], lhsT=onesb.t[:, :], rhs=lnd.t[:, :], start=False, stop=False), reads=[onesb, lnd], writes=[pt])
                    if n2 > 0:
                        tsb = qs["ck"].pop(n2)
                        c.op("pe", lambda e, pt=pt, tsb=tsb: e.matmul(pt.t[:, :], lhsT=identb.t[:, :], rhs=tsb.t[:, :], start=False, stop=False), reads=[identb, tsb], writes=[pt])
                    c.op("pe", lambda e, pt=pt, lp=lp: e.matmul(pt.t[:, :], lhsT=identb.t[:, :], rhs=lp.t[:, :], start=False, stop=True), reads=[identb, lp], writes=[pt])
                    if idx <= nkt - 4:
                        ptot = qs["ptot"]
                        c.op("pe", lambda e, ptot=ptot, ln=ln, idx=idx: e.matmul(ptot.t[:, :], lhsT=onesb.t[:, :], rhs=ln.t[:, :], start=(idx == 0), stop=True, skip_group_check=True),
                             reads=[onesb, ln], writes=[ptot])
                        T[j]["ck"] = True
                    qs["ln"].pop(idx - 2, None)
                    T[j]["pt"] = pt
                j = step - 2
                if 0 <= j < len(tiles):
                    Q, idx, kt = tiles[j]
                    pz = T[j].pop("pz")
                    lp = T[j]["lp"]
                    ln = ln_r.next()
                    c.op("dve", lambda e, pz=pz, lp=lp, ln=ln: e.tensor_tensor(out=ln.t[:, :], in0=pz.t[:, :], in1=lp.t[:, :], op=ALU.add), reads=[pz, lp], writes=[ln])
                    if kt >= 4 * Q:
                        o = 512 * Q - 128 * kt + 384
                        c.op("pool", lambda e, ln=ln, o=o: e.tensor_tensor(out=ln.t[:, :], in0=ln.t[:, :], in1=cm.t[:, o:o + 512], op=ALU.mult), reads=[ln, cm], writes=[ln])
                    T[j]["ln"] = ln
                j = step - 1
                if 0 <= j < len(tiles):
                    pz = T[j]["pz"]
                    ee, lp = e_r.next(), lp_r.next()
                    c.op("act", lambda e, pz=pz, ee=ee: e.activation(out=ee.t[:, :], in_=pz.t[:, :], func=AF.Exp, scale=-1.0), reads=[pz], writes=[ee])
                    c.op("act", lambda e, ee=ee, lp=lp: e.activation(out=lp.t[:, :], in_=ee.t[:, :], func=AF.Ln, bias=1.0), reads=[ee], writes=[lp])
                    T[j]["lp"] = lp
                j = step
                if j < len(tiles):
                    Q, idx, kt = tiles[j]
                    pz = pZ.next()
                    c.op("pe", lambda e, pz=pz, kt=kt, Q=Q: e.matmul(pz.t[:, :], lhsT=kp.t[:, kt * 128:(kt + 1) * 128], rhs=qp.t[:, Q * 512:(Q + 1) * 512],
                                                                   start=True, stop=True), reads=[kp, qp], writes=[pz])
                    T[j] = {"pz": pz}
        c.end()

    def router_phase(rw2d):
        c.begin()
        rw = c.sbuf("rrw", [128, KC, E], BF16)
        c.dma("pool", [(rw.t[:, :, :], rw2d.rearrange("(k p) e -> p k e", p=128))], writes=[rw])
        ht_r = Rot([c.sbuf("rht0", [128, KC, 512], BF16), c.sbuf("rht1", [128, KC, 512], BF16)])
        cst = c.sbuf("rcst", [128, NT, E], F32)
        mk = lambda nm, w: Rot([c.sbuf(nm + "0", [128, w], F32), c.sbuf(nm + "1", [128, w], F32)])
        lg_r, t8_r, d_r, ex_r, w1_r, w2_r, c1_r, c2_r = mk("rlg", 8), mk("rt8", 8), mk("rd", 1), mk("rex", 1), mk("rw1", 1), mk("rw2", 1), mk("rc1", 8), mk("rc2", 8)
        pL = Rot([c.psum("rpL0", [128, 512], F32), c.psum("rpL1", [128, 512], F32)])
        for g in range(NG):
            ht = ht_r.next()
            c.dma("sp", [(ht.t[:, :, :], hT[:, g * 512:(g + 1) * 512].rearrange("(k p) s -> p k s", p=128))], writes=[ht])
            for tt in range(4):
                pl = pL.next()
                for kc in range(KC):
                    c.op("pe", lambda e, pl=pl, kc=kc, ht=ht, tt=tt: e.matmul(pl.t[:, 0:E], lhsT=ht.t[:, kc, tt * 128:(tt + 1) * 128], rhs=rw.t[:, kc, :], start=(kc == 0), stop=(kc == KC - 1)),
                         reads=[ht, rw], writes=[pl])
                lg, t8, d_, ex, w1, w2, c1, c2 = (r_.next() for r_ in (lg_r, t8_r, d_r, ex_r, w1_r, w2_r, c1_r, c2_r))
                c.op("dve", lambda e, pl=pl, lg=lg: e.tensor_copy(lg.t[:, :], pl.t[:, 0:E]), reads=[pl], writes=[lg])
                c.op("dve", lambda e, lg=lg, t8=t8: e.max(out=t8.t[:, :], in_=lg.t[:, :]), reads=[lg], writes=[t8])
                c.op("dve", lambda e, t8=t8, d_=d_: e.tensor_tensor(out=d_.t[:, :], in0=t8.t[:, 1:2], in1=t8.t[:, 0:1], op=ALU.subtract), reads=[t8], writes=[d_])
                c.op("act", lambda e, d_=d_, ex=ex: e.activation(out=ex.t[:, :], in_=d_.t[:, :], func=AF.Exp), reads=[d_], writes=[ex])
                c.op("dve", lambda e, ex=ex, w1=w1: e.tensor_scalar(out=w1.t[:, :], in0=ex.t[:, :], scalar1=1.0, scalar2=None, op0=ALU.add), reads=[ex], writes=[w1])
                c.op("dve", lambda e, w1=w1: e.reciprocal(out=w1.t[:, :], in_=w1.t[:, :]), reads=[w1], writes=[w1])
                c.op("dve", lambda e, w1=w1, ex=ex, w2=w2: e.tensor_tensor(out=w2.t[:, :], in0=w1.t[:, :], in1=ex.t[:, :], op=ALU.mult), reads=[w1, ex], writes=[w2])
                c.op("dve", lambda e, lg=lg, t8=t8, w1=w1, c1=c1: e.tensor_scalar(out=c1.t[:, :], in0=lg.t[:, :], scalar1=t8.t[:, 0:1], scalar2=w1.t[:, 0:1], op0=ALU.is_equal, op1=ALU.mult),
                     reads=[lg, t8, w1], writes=[c1])
                c.op("dve", lambda e, lg=lg, t8=t8, w2=w2, c2=c2: e.tensor_scalar(out=c2.t[:, :], in0=lg.t[:, :], scalar1=t8.t[:, 1:2], scalar2=w2.t[:, 0:1], op0=ALU.is_equal, op1=ALU.mult),
                     reads=[lg, t8, w2], writes=[c2])
                ti = g * 4 + tt
                c.op("dve", lambda e, c1=c1, c2=c2, ti=ti: e.tensor_tensor(out=cst.t[:, ti, :], in0=c1.t[:, :], in1=c2.t[:, :], op=ALU.add), reads=[c1, c2], writes=[cst])
        c.dma("sp", [(comb_tm[:, :].rearrange("(t p) e -> p t e", p=128), cst.t[:, :, :])], reads=[cst])
        c.end()

    def ffn_phase(experts, F_, gcol_row0, use_comb):
        c.begin()
        NF = F_ // 128
        NTT = TS // 128
        NTG = TS // 512
        PF = 2
        PW2 = 7
        hts = c.sbuf("fht", [128, KC, TS], BF16)
        actT = c.sbuf("fact", [128, NF, TS], BF16)
        acc = c.sbuf("facc", [128, NTT, D], F32)
        gb = c.sbuf("fgb", [128, D], F32)
        c.dma("sp", [(gb.t[:, :], vecscr[gcol_row0:gcol_row0 + KC, :].rearrange("(o k) p -> o (k p)", o=1).partition_broadcast(128))], writes=[gb])
        wg_r = Rot([c.sbuf("fwg0", [128, KC, 2 * PF * 128], BF16), c.sbuf("fwg1", [128, KC, 2 * PF * 128], BF16)])
        w2_r = Rot([c.sbuf("fw20", [128, PW2, D], BF16), c.sbuf("fw21", [128, PW2, D], BF16)])
        cbt = c.sbuf("fcbt", [128, NTT, E], F32)
        sg_r = Rot([c.sbuf(f"fsg{i}", [128, 512], F32) for i in range(3)])
        xt_r = Rot([c.sbuf("fx0", [128, D], F32), c.sbuf("fx1", [128, D], F32)])
        pGU = Rot([c.psum(f"fpg{i}", [128, 512], F32) for i in range(4)])
        pY = Rot([c.psum(f"fpy{i}", [128, 512], F32) for i in range(3)])
        for st in range(S // TS):
            t0 = st * TS
            c.dma("sp", [(hts.t[:, :, :], hT[:, t0:t0 + TS].rearrange("(k p) s -> p k s", p=128))], writes=[hts])
            if use_comb:
                c.dma("sp", [(cbt.t[:, :, :], comb_tm[t0:t0 + TS, :].rearrange("(t p) e -> p t e", p=128))], writes=[cbt])
            for ei, (w13, w2) in enumerate(experts):
                for f0 in range(0, NF, PF):
                    nf = min(PF, NF - f0)
                    wg = wg_r.next()
                    c.dma("pool", [(wg.t[:, :, 0:nf * 128], w13[:, f0 * 128:(f0 + nf) * 128].rearrange("(k p) f -> p k f", p=128)),
                                   (wg.t[:, :, PF * 128:PF * 128 + nf * 128], w13[:, F_ + f0 * 128:F_ + (f0 + nf) * 128].rearrange("(k p) f -> p k f", p=128))], writes=[wg])
                    for j in range(nf):
                        fc = f0 + j
                        for tg in range(NTG):
                            pg, pu = pGU.next(), pGU.next()
                            for kc in range(KC):
                                c.op("pe", lambda e, pg=pg, wg=wg, j=j, kc=kc, tg=tg: e.matmul(pg.t[:, :], lhsT=wg.t[:, kc, j * 128:(j + 1) * 128], rhs=hts.t[:, kc, tg * 512:(tg + 1) * 512],
                                                                                              start=(kc == 0), stop=(kc == KC - 1)), reads=[wg, hts], writes=[pg])
                            for kc in range(KC):
                                c.op("pe", lambda e, pu=pu, wg=wg, j=j, kc=kc, tg=tg: e.matmul(pu.t[:, :], lhsT=wg.t[:, kc, (PF + j) * 128:(PF + j + 1) * 128], rhs=hts.t[:, kc, tg * 512:(tg + 1) * 512],
                                                                                              start=(kc == 0), stop=(kc == KC - 1)), reads=[wg, hts], writes=[pu])
                            sg = sg_r.next()
                            c.op("act", lambda e, pg=pg, sg=sg: e.activation(out=sg.t[:, :], in_=pg.t[:, :], func=AF.Silu), reads=[pg], writes=[sg])
                            sgc = sg
                            c.op("dve", lambda e, pu=pu, sgc=sgc, fc=fc, tg=tg: e.tensor_tensor(out=actT.t[:, fc, tg * 512:(tg + 1) * 512], in0=pu.t[:, :], in1=sgc.t[:, :], op=ALU.mult),
                                 reads=[pu, sgc], writes=[actT])
                for p0 in range(0, NF, PW2):
                    npc = min(PW2, NF - p0)
                    w2s = w2_r.next()
                    c.dma("pool", [(w2s.t[:, 0:npc, :], w2[p0 * 128:(p0 + npc) * 128, :].rearrange("(f p) d -> p f d", p=128))], writes=[w2s])
                    first = (ei == 0 and p0 == 0)
                    for tt in range(NTT):
                        for half in range(D // 512):
                            py = pY.next()
                            for j in range(npc):
                                c.op("pe", lambda e, py=py, j=j, p0=p0, tt=tt, half=half, w2s=w2s, npc=npc: e.matmul(py.t[:, :], lhsT=actT.t[:, p0 + j, tt * 128:(tt + 1) * 128],
                                                                                                                rhs=w2s.t[:, j, half * 512:(half + 1) * 512], start=(j == 0), stop=(j == npc - 1)),
                                     reads=[actT, w2s], writes=[py])
                            asl = acc.t[:, tt, half * 512:(half + 1) * 512]
                            if use_comb:
                                csc = cbt.t[:, tt, ei:ei + 1]
                                if first:
                                    c.op("act", lambda e, py=py, asl=asl, csc=csc: e.activation(out=asl, in_=py.t[:, :], func=AF.Copy, scale=csc), reads=[py, cbt], writes=[acc])
                                else:
                                    c.op("dve", lambda e, py=py, asl=asl, csc=csc: e.scalar_tensor_tensor(out=asl, in0=py.t[:, :], scalar=csc, in1=asl, op0=ALU.mult, op1=ALU.add),
                                         reads=[py, acc, cbt], writes=[acc])
                            elif first:
                                c.op("act", lambda e, py=py, asl=asl: e.activation(out=asl, in_=py.t[:, :], func=AF.Copy), reads=[py], writes=[acc])
                            else:
                                c.op("dve", lambda e, py=py, asl=asl: e.tensor_tensor(out=asl, in0=py.t[:, :], in1=asl, op=ALU.add), reads=[py, acc], writes=[acc])
            for tt in range(NTT):
                xt = xt_r.next()
                c.dma("sp", [(xt.t[:, :], xres[t0 + tt * 128:t0 + (tt + 1) * 128, :])], writes=[xt])
                c.op("pool", lambda e, tt=tt: e.tensor_tensor(out=acc.t[:, tt, :], in0=acc.t[:, tt, :], in1=gb.t[:, :], op=ALU.mult), reads=[acc, gb], writes=[acc])
                c.op("pool", lambda e, tt=tt, xt=xt: e.tensor_tensor(out=xt.t[:, :], in0=xt.t[:, :], in1=acc.t[:, tt, :], op=ALU.add), reads=[acc, xt], writes=[xt])
                c.dma("pool", [(xres[t0 + tt * 128:t0 + (tt + 1) * 128, :], xt.t[:, :])], reads=[xt])
        c.end()

    def final_phase():
        c.begin()
        gb = c.sbuf("zgb", [128, D], F32)
        c.dma("sp", [(gb.t[:, :], final_norm_g[0:1, :].partition_broadcast(128))], writes=[gb])
        xt_r = Rot([c.sbuf("zx0", [128, 4, D], F32), c.sbuf("zx1", [128, 4, D], F32)])
        junk = c.sbuf("zjunk", [128, D], BF16)
        ss_r = Rot([c.sbuf("zss0", [128, 4], F32), c.sbuf("zss1", [128, 4], F32)])
        sq_r = Rot([c.sbuf("zsq0", [128, 4], F32), c.sbuf("zsq1", [128, 4], F32)])
        rs_r = Rot([c.sbuf("zrs0", [128, 4], F32), c.sbuf("zrs1", [128, 4], F32)])
        epsc = c.sbuf("zeps", [128, 1], F32)
        c.op("pool", lambda e: e.memset(epsc.t[:, :], EPS), writes=[epsc])
        for g in range(NG):
            xt = xt_r.next()
            c.dma("sp", [(xt.t[:, :, :], xres[g * 512:(g + 1) * 512, :].rearrange("(t p) d -> p t d", p=128))], writes=[xt])
            ss, sq, rs = ss_r.next(), sq_r.next(), rs_r.next()
            for t in range(4):
                c.op("act", lambda e, t=t, xt=xt, ss=ss: e.activation(out=junk.t[:, :], in_=xt.t[:, t, :], func=AF.Square, accum_out=ss.t[:, t:t + 1]), reads=[xt], writes=[junk, ss])
            c.op("act", lambda e, ss=ss, sq=sq: e.activation(out=sq.t[:, :], in_=ss.t[:, :], func=AF.Sqrt, scale=1.0 / D, bias=epsc.t[:, 0:1]), reads=[ss, epsc], writes=[sq])
            c.op("dve", lambda e, sq=sq, rs=rs: e.reciprocal(out=rs.t[:, :], in_=sq.t[:, :]), reads=[sq], writes=[rs])
            for t in range(4):
                c.op("dve", lambda e, t=t, xt=xt, rs=rs: e.scalar_tensor_tensor(out=xt.t[:, t, :], in0=xt.t[:, t, :], scalar=rs.t[:, t:t + 1], in1=gb.t[:, :], op0=ALU.mult, op1=ALU.mult),
                     reads=[xt, rs, gb], writes=[xt])
            c.dma("pool", [(out_d[g * 512:(g + 1) * 512, :].rearrange("(t p) d -> p t d", p=128), xt.t[:, :, :])], reads=[xt])
        c.end()

    c.begin()
    vsx = c.vslot("xcopy")
    c.dma("sp", [(xres[:, :], x_in[:, :])], writes=[vsx])
    c.end()
    qdst = [(qT, j * 128) for j in range(KC)]
    kdst = [(kT, j * 128) for j in range(KC)]
    for l in range(4):
        norm_phase(xres, l * KC, col_mod(l, 0), hT)
        if l < 2:
            proj_phase(hT, a_wqkv[l], 2 * D, D, qdst + kdst, v65, set(range(KC)))
            moba_phase()
            wo_phase(a_wo[l], col_mod(l, 2))
        else:
            j = l - 2
            if j == 0:
                norm_phase(xres, 8 * KC, COL_KV, attnT)
                proj_phase(attnT, b_wkv, D, D, kdst, v65, set())
            proj_phase(hT, b_wq[j], D, 0, qdst, None, set(range(KC)))
            sb_phase()
            wo_phase(b_wo[j], col_mod(l, 2))
        norm_phase(xres, (4 + l) * KC, col_mod(l, 3), hT)
        if l % 2 == 0:
            ffn_phase([(ffn_w13[l // 2], ffn_w2[l // 2])], FF, col_mod(l, 5), False)
        else:
            router_phase(router_w[l // 2])
            ffn_phase([(moe_w13[l // 2, e], moe_w2[l // 2, e]) for e in range(E)], FE, col_mod(l, 5), True)
    final_phase()
    c.barrier()
    return c


_CACHE = {}


def kernel(**inputs):
    cfg = inputs.pop("_cfg", None) or CFG_FULL
    runner = inputs.pop("_runner", None)
    key = tuple(sorted(cfg.items()))
    if key not in _CACHE:
        _CACHE[key] = (build(cfg), make_consts(cfg))
    c, consts = _CACHE[key]
    S, D = cfg["S"], cfg["D"]
    x = np.asarray(inputs["x"], np.float32)
    B = x.shape[0]
    f = lambda k: np.ascontiguousarray(np.asarray(inputs[k], np.float32))
    shared = {
        "rel_bias": f("rel_bias"), "mod_w": f("mod_w"), "mod_b": f("mod_b").reshape(1, -1),
        "norm_mix_g": f("norm_mix_g"), "norm_ffn_g": f("norm_ffn_g"), "a_wqkv": f("a_wqkv"), "a_wo": f("a_wo"),
        "kv_norm_g": f("kv_norm_g").reshape(1, -1), "kv_mod_w": f("kv_mod_w"), "kv_mod_b": f("kv_mod_b").reshape(1, -1),
        "b_wkv": f("b_wkv"), "b_wq": f("b_wq"), "b_wo": f("b_wo"), "ffn_w13": f("ffn_w13"), "ffn_w2": f("ffn_w2"),
        "router_w": f("router_w"), "moe_w13": f("moe_w13"), "moe_w2": f("moe_w2"),
        "final_norm_g": f("final_norm_g").reshape(1, -1),
    }
    shared.update(consts)
    cc = np.asarray(inputs["c"], np.float32)
    in_maps = []
    for b in range(B):
        m = dict(shared)
        m["x"] = np.ascontiguousarray(x[b])
        m["c"] = np.ascontiguousarray(cc[b:b + 1])
        in_maps.append(m)
    if runner is not None:
        res = runner(c.nc, in_maps)
    else:
        res = run_bass_kernel_spmd(c.nc, in_maps, core_ids=list(range(B))).results
    return np.stack([np.asarray(r["out"], np.float32) for r in res], axis=0)
```

```python
import bisect
import math
from contextlib import ExitStack
import numpy as np
import ml_dtypes
import concourse.bass as bass
import concourse.mybir as mybir
from concourse.bass_utils import run_bass_kernel_spmd

F32 = mybir.dt.float32
BF16 = mybir.dt.bfloat16
ALU = mybir.AluOpType
AF = mybir.ActivationFunctionType
AX = mybir.AxisListType
SEM_ROT = 30000
BIG = 1.0e30
EPS = 1e-6

CFG_FULL = dict(S=4096, D=1024, H=16, FF=2816, FE=3584, E=8, TS=1024)


class Ev:
    __slots__ = ("eng", "idx", "ins", "sem", "val", "slot", "kind", "waited")

    def __init__(self, eng, idx, ins):
        self.eng, self.idx, self.ins = eng, idx, ins
        self.sem = None
        self.val = None
        self.slot = None
        self.kind = None
        self.waited = False


class Eng:
    def __init__(self, name, e):
        self.name, self.e = name, e
        self.n = 0
        self.sem = None
        self.cnt = 0
        self.mat_idx = []
        self.mat_ev = []
        self.seen = {}
        self.last = None


class Slot:
    def __init__(self, name, t=None):
        self.name = name
        self.t = t
        self.w = None
        self.r = {}
        self.sems = {}
        self.lastd = {}


class Ctx:
    def __init__(self):
        self.nc = bass.Bass("TRN2", target_bir_lowering=False)
        nc = self.nc
        self.root = ExitStack()
        self.stacks = [self.root]
        self.engs = {
            "pe": Eng("pe", nc.tensor),
            "act": Eng("act", nc.scalar),
            "dve": Eng("dve", nc.vector),
            "pool": Eng("pool", nc.gpsimd),
            "sp": Eng("sp", nc.sync),
        }
        self.nsem = 0
        self.nname = 0
        self.slots = []
        self.phase_slots = [[]]
        self.sempool = {"sp": [], "pool": [], "act": []}
        self.ninstr = 0

    def new_sem(self, name):
        self.nsem += 1
        return self.root.enter_context(self.nc.semaphore(f"{name}_{self.nsem}"))

    def _reg(self, s):
        self.slots.append(s)
        self.phase_slots[-1].append(s)
        return s

    def sbuf(self, name, shape, dt):
        self.nname += 1
        t = self.stacks[-1].enter_context(self.nc.sbuf_tensor(f"{name}_{self.nname}", list(shape), dt))
        return self._reg(Slot(name, t))

    def psum(self, name, shape, dt=F32):
        self.nname += 1
        t = self.stacks[-1].enter_context(self.nc.psum_tensor(f"{name}_{self.nname}", list(shape), dt))
        return self._reg(Slot(name, t))

    def vslot(self, name):
        return self._reg(Slot(name, None))

    def dram(self, name, shape, dt, kind="Internal"):
        return self.nc.dram_tensor(name, list(shape), dt, kind=kind)

    def begin(self):
        self.stacks.append(ExitStack())
        self.phase_slots.append([])

    def end(self):
        self.barrier()
        for s in self.phase_slots.pop():
            for (d_, q_), sc in s.sems.items():
                self.sempool[q_].append(sc)
            self.slots.remove(s)
        self.stacks.pop().close()

    def _getsem(self, q):
        if self.sempool[q]:
            return self.sempool[q].pop(0)
        return [self.new_sem("d" + q), 0]

    def _materialize(self, ev):
        if ev.val is not None:
            return
        eng = ev.eng
        i = bisect.bisect_left(eng.mat_idx, ev.idx)
        if i < len(eng.mat_idx):
            o = eng.mat_ev[i]
            ev.sem, ev.val = o.sem, o.val
            return
        if eng.sem is None or eng.cnt >= SEM_ROT:
            eng.sem = self.new_sem("p" + eng.name)
            eng.cnt = 0
        eng.cnt += 1
        ev.ins.then_inc(eng.sem, 1)
        ev.sem, ev.val = eng.sem, eng.cnt
        eng.mat_idx.append(ev.idx)
        eng.mat_ev.append(ev)

    def _wait(self, eng, ev):
        if ev.kind == "dma":
            ev.waited = True
        else:
            self._materialize(ev)
        k = id(ev.sem)
        if eng.seen.get(k, 0) >= ev.val:
            return
        eng.seen[k] = ev.val
        eng.e.wait_ge(ev.sem, ev.val)

    def _deps(self, eng, reads, writes, is_dma):
        deps = []
        for s in reads:
            if s.w is not None:
                if is_dma or s.w.kind == "dma" or s.w.eng is not eng or eng.name != "pe":
                    deps.append(s.w)
        for s in writes:
            if s.w is not None and (is_dma or s.w.kind == "dma" or s.w.eng is not eng or eng.name != "pe"):
                deps.append(s.w)
            for r in s.r.values():
                if is_dma or r.kind == "dma" or r.eng is not eng or eng.name != "pe":
                    deps.append(r)
        return deps

    def op(self, engname, fn, reads=(), writes=()):
        eng = self.engs[engname]
        for d in self._deps(eng, reads, writes, False):
            self._wait(eng, d)
        ins = fn(eng.e)
        self.ninstr += 1
        ev = Ev(eng, eng.n, ins)
        eng.n += 1
        eng.last = ev
        for s in reads:
            s.r[engname] = ev
        for s in writes:
            s.w = ev
            s.r = {}
        return ev

    def dma(self, qname, pairs, reads=(), writes=(), **kw):
        eng = self.engs[qname]
        for d in self._deps(eng, reads, writes, True):
            self._wait(eng, d)
        s = writes[0] if writes else reads[0]
        key = ("w" if writes else "r", qname)
        if key not in s.sems:
            s.sems[key] = self._getsem(qname)
        sc = s.sems[key]
        prev = s.lastd.get(key)
        sc[1] += 16 * len(pairs)
        if prev is not None and not prev.waited:
            prev.val = sc[1]
        ins = None
        for (o, i) in pairs:
            ins = eng.e.dma_start(out=o, in_=i, **kw).then_inc(sc[0], 16)
            self.ninstr += 1
        ev = Ev(eng, -1, ins)
        ev.kind = "dma"
        ev.sem, ev.val, ev.slot = sc[0], sc[1], s
        s.lastd[key] = ev
        for x in reads:
            x.r["dma"] = ev
        for x in writes:
            x.w = ev
            x.r = {}
        return ev

    def barrier(self):
        evs = [e.last for e in self.engs.values() if e.last is not None]
        dm = []
        for s in self.slots:
            dm.extend(s.sems.values())
        for ev in evs:
            self._materialize(ev)
        for e in self.engs.values():
            for ev in evs:
                if ev.eng is e:
                    continue
                k = id(ev.sem)
                if e.seen.get(k, 0) < ev.val:
                    e.seen[k] = ev.val
                    e.e.wait_ge(ev.sem, ev.val)
            for (sem, val) in dm:
                k = id(sem)
                if val > 0 and e.seen.get(k, 0) < val:
                    e.seen[k] = val
                    e.e.wait_ge(sem, val)
        for s in self.slots:
            s.w = None
            s.r = {}
            for ev in s.lastd.values():
                ev.waited = True


class Rot:
    def __init__(self, items):
        self.items = items
        self.i = 0

    def next(self):
        x = self.items[self.i % len(self.items)]
        self.i += 1
        return x


def rel_bucket_table(nu):
    import jax
    import jax.numpy as jnp
    with jax.default_device(jax.devices("cpu")[0]):
        dist = jnp.arange(nu, dtype=jnp.int32) - 512
        distc = jnp.maximum(dist, 0)
        max_exact = 16
        d = jnp.maximum(distc, 1).astype(jnp.float32)
        large = max_exact + (jnp.log(d / max_exact) / math.log(1024 / max_exact) * (32 - max_exact)).astype(jnp.int32)
        large = jnp.minimum(large, 31)
        b = np.asarray(jnp.where(distc < max_exact, distc, large))
        dist = np.asarray(dist)
    oh = np.zeros((33, nu), np.float32)
    for u in range(nu):
        if dist[u] < 0:
            oh[32, u] = 1.0
        else:
            oh[b[u], u] = 1.0
    return oh


def make_consts(cfg):
    S, H = cfg["S"], cfg["H"]
    NU = max(S + 512, 2048)
    NB = S // 256
    cs = {}
    cs["identb"] = np.eye(128).astype(ml_dtypes.bfloat16)
    cs["identf"] = np.eye(128, dtype=np.float32)
    cs["oh"] = rel_bucket_table(NU)
    cs["jex"] = np.ascontiguousarray(np.eye(128, dtype=np.float32)[::-1])
    es = np.zeros((16, 16 * 128), np.float32)
    for n in range(16):
        es[n, n * 128:(n + 1) * 128] = 1.0
    cs["esel"] = es.astype(ml_dtypes.bfloat16)
    e2 = np.zeros((16, S), np.float32)
    for n in range(min(16, S // 256)):
        e2[n, n * 256:(n + 1) * 256] = 1.0
    cs["esel2"] = e2.astype(ml_dtypes.bfloat16)
    j = np.arange(128)[:, None]
    k = np.arange(128)[None, :]
    cs["ustrict"] = (j > k).astype(np.float32).astype(ml_dtypes.bfloat16)
    m = np.arange(512 + 384)[None, :]
    kk = np.arange(128)[:, None]
    cs["cmask"] = ((m - 384 - kk) > 0).astype(np.float32).astype(ml_dtypes.bfloat16)
    return cs


def build(cfg):
    S, D, H, FF, FE, E, TS = (cfg[k] for k in ("S", "D", "H", "FF", "FE", "E", "TS"))
    KC = D // 128
    NT = S // 128
    NG = S // 512
    NB = S // 256
    NBP = max(NB, 8)
    NU = max(S + 512, 2048)
    WB = 1856
    DMAXNEAR = 896
    c = Ctx()
    nc = c.nc
    di = lambda n, sh, dt=F32: c.dram(n, sh, dt, kind="ExternalInput")
    x_in = di("x", [S, D])
    c_in = di("c", [1, D])
    rel_bias = di("rel_bias", [32, H])
    mod_w = di("mod_w", [4, D, 6 * D])
    mod_b = di("mod_b", [1, 4 * 6 * D])
    norm_mix_g = di("norm_mix_g", [4, D])
    norm_ffn_g = di("norm_ffn_g", [4, D])
    a_wqkv = di("a_wqkv", [2, D, 3 * D])
    a_wo = di("a_wo", [2, D, D])
    kv_norm_g = di("kv_norm_g", [1, D])
    kv_mod_w = di("kv_mod_w", [D, 2 * D])
    kv_mod_b = di("kv_mod_b", [1, 2 * D])
    b_wkv = di("b_wkv", [D, 2 * D])
    b_wq = di("b_wq", [2, D, D])
    b_wo = di("b_wo", [2, D, D])
    ffn_w13 = di("ffn_w13", [2, D, 2 * FF])
    ffn_w2 = di("ffn_w2", [2, FF, D])
    router_w = di("router_w", [2, D, E])
    moe_w13 = di("moe_w13", [2, E, D, 2 * FE])
    moe_w2 = di("moe_w2", [2, E, FE, D])
    final_norm_g = di("final_norm_g", [1, D])
    identb_d = di("identb", [128, 128], BF16)
    identf_d = di("identf", [128, 128])
    oh_d = di("oh", [33, NU])
    jex_d = di("jex", [128, 128])
    esel_d = di("esel", [16, 16 * 128], BF16)
    esel2_d = di("esel2", [16, S], BF16)
    ustrict_d = di("ustrict", [128, 128], BF16)
    cmask_d = di("cmask", [128, 896], BF16)
    out_d = c.dram("out", [S, D], F32, kind="ExternalOutput")

    xres = c.dram("xres", [S, D], F32)
    hT = c.dram("hT", [D, S], BF16)
    qT = c.dram("qT", [D, S], BF16)
    kT = c.dram("kT", [D, S], BF16)
    v65 = c.dram("v65", [S, H * 65], BF16)
    attnT = c.dram("attnT", [D, S], BF16)
    NMOD = 4 * 6 * D + 2 * D
    NVR = NMOD // 128 + 4 * KC + 4 * KC + KC
    vecscr = c.dram("vecscr", [NVR, 128], F32)
    ftab = c.dram("ftab", [H, NU], F32)
    biasT = c.dram("biasT", [H, 128, WB], BF16)
    comb_tm = c.dram("comb_tm", [S, E], F32)

    identb = c.sbuf("identb", [128, 128], BF16)
    identf = c.sbuf("identf", [128, 128], F32)
    modT = c.sbuf("modT", [128, NVR], F32)
    geff = c.sbuf("geff", [128, 9 * KC], F32)
    onesf = c.sbuf("onesf", [128, 128], F32)
    onesb = c.sbuf("onesb", [128, 128], BF16)
    c.dma("sp", [(identb.t[:, :], identb_d[:, :])], writes=[identb])
    c.dma("sp", [(identf.t[:, :], identf_d[:, :])], writes=[identf])
    c.op("pool", lambda e: e.memset(onesf.t[:, :], 1.0), writes=[onesf])
    c.op("pool", lambda e: e.memset(onesb.t[:, :], 1.0), writes=[onesb])

    def col_mod(l, j):
        return l * 6 * KC + j * KC
    COL_KV = 4 * 6 * KC
    COL_GMIX = NMOD // 128
    COL_GFFN = COL_GMIX + 4 * KC
    COL_GKV = COL_GFFN + 4 * KC

    c.begin()
    crow = c.sbuf("crow", [KC, 128], F32)
    cT = c.sbuf("cT", [128, KC], F32)
    scT = c.sbuf("scT", [128, KC], F32)
    pA = c.psum("pA", [128, 512], F32)
    pB = Rot([c.psum(f"pB{i}", [128, 512], F32) for i in range(3)])
    mw = Rot([c.sbuf(f"mw{i}", [128, KC, 512], F32) for i in range(4)])
    mbr = Rot([c.sbuf(f"mbr{i}", [1, 512], F32) for i in range(4)])
    mst = Rot([c.sbuf("mst0", [1, 512], F32), c.sbuf("mst1", [1, 512], F32)])
    vs = c.vslot("vecscr")
    c.dma("sp", [(crow.t[:, :], c_in[0:1, :].rearrange("o (k p) -> (o k) p", p=128))], writes=[crow])
    c.dma("pool", [(vecscr[COL_GMIX:COL_GMIX + 4 * KC, :], norm_mix_g[:, :].rearrange("l (k p) -> (l k) p", p=128)),
                   (vecscr[COL_GFFN:COL_GFFN + 4 * KC, :], norm_ffn_g[:, :].rearrange("l (k p) -> (l k) p", p=128)),
                   (vecscr[COL_GKV:COL_GKV + KC, :], kv_norm_g[0:1, :].rearrange("o (k p) -> (o k) p", p=128))],
          writes=[vs])
    c.op("pe", lambda e: e.transpose(out=pA.t[:, 0:KC], in_=crow.t[:, :], identity=identf.t[0:KC, 0:KC]), reads=[crow, identf], writes=[pA])
    c.op("dve", lambda e: e.tensor_copy(cT.t[:, :], pA.t[:, 0:KC]), reads=[pA], writes=[cT])
    c.op("act", lambda e: e.activation(out=scT.t[:, :], in_=cT.t[:, :], func=AF.Silu), reads=[cT], writes=[scT])
    blocks = [(mod_w[l], cb, l * 6 * D + cb * 512, mod_b, l * 6 * D + cb * 512) for l in range(4) for cb in range(6 * D // 512)]
    blocks += [(kv_mod_w, cb, 4 * 6 * D + cb * 512, kv_mod_b, cb * 512) for cb in range(2 * D // 512)]
    for bi, (wsrc, cb, off, bsrc, boff) in enumerate(blocks):
        qn = "sp" if bi % 2 == 0 else "act"
        w = mw.next()
        mb_ = mbr.next()
        c.dma(qn, [(w.t[:, :, :], wsrc[:, cb * 512:(cb + 1) * 512].rearrange("(k p) f -> p k f", p=128))], writes=[w])
        c.dma(qn, [(mb_.t[0:1, :], bsrc[0:1, boff:boff + 512])], writes=[mb_])
        ps = pB.next()
        for kc in range(KC):
            c.op("pe", lambda e, kc=kc, w=w, ps=ps: e.matmul(ps.t[0:1, :], lhsT=scT.t[:, kc:kc + 1], rhs=w.t[:, kc, :],
                                                            start=(kc == 0), stop=(kc == KC - 1)), reads=[scT, w], writes=[ps])
        st = mst.next()
        c.op("dve", lambda e, st=st, ps=ps, mb_=mb_: e.tensor_tensor(out=st.t[0:1, :], in0=ps.t[0:1, :], in1=mb_.t[0:1, :], op=ALU.add),
             reads=[ps, mb_], writes=[st])
        r0 = off // 128
        c.dma("sp", [(vecscr[r0:r0 + 4, :].rearrange("(o r) p -> o (r p)", o=1), st.t[0:1, :])], reads=[st], writes=[vs])
    c.barrier()
    r = 0
    vrow = Rot([c.sbuf("vrow0", [128, 128], F32), c.sbuf("vrow1", [128, 128], F32)])
    while r < NVR:
        n = min(128, NVR - r)
        vr = vrow.next()
        c.dma("sp", [(vr.t[0:n, :], vecscr[r:r + n, :])], writes=[vr])
        c.op("pe", lambda e, vr=vr, n=n: e.transpose(out=pA.t[:, 0:n], in_=vr.t[0:n, :], identity=identf.t[0:n, 0:n]), reads=[vr, identf], writes=[pA])
        c.op("dve", lambda e, r=r, n=n: e.tensor_copy(modT.t[:, r:r + n], pA.t[:, 0:n]), reads=[pA], writes=[modT])
        r += n
    for l in range(4):
        c.op("dve", lambda e, l=l: e.scalar_tensor_tensor(out=geff.t[:, l * KC:(l + 1) * KC], in0=modT.t[:, col_mod(l, 1):col_mod(l, 1) + KC], scalar=1.0,
                                                          in1=modT.t[:, COL_GMIX + l * KC:COL_GMIX + (l + 1) * KC], op0=ALU.add, op1=ALU.mult), reads=[modT], writes=[geff])
        c.op("dve", lambda e, l=l: e.scalar_tensor_tensor(out=geff.t[:, (4 + l) * KC:(5 + l) * KC], in0=modT.t[:, col_mod(l, 4):col_mod(l, 4) + KC], scalar=1.0,
                                                          in1=modT.t[:, COL_GFFN + l * KC:COL_GFFN + (l + 1) * KC], op0=ALU.add, op1=ALU.mult), reads=[modT], writes=[geff])
    c.op("dve", lambda e: e.scalar_tensor_tensor(out=geff.t[:, 8 * KC:9 * KC], in0=modT.t[:, COL_KV + KC:COL_KV + 2 * KC], scalar=1.0,
                                                 in1=modT.t[:, COL_GKV:COL_GKV + KC], op0=ALU.add, op1=ALU.mult), reads=[modT], writes=[geff])
    c.end()

    c.begin()
    rbA = c.sbuf("rbA", [33, H], F32)
    ohs = c.sbuf("ohs", [33, NU], F32)
    fsb = c.sbuf("fsb", [H, NU], F32)
    jex = c.sbuf("jex", [128, 128], F32)
    pF = Rot([c.psum("pF0", [128, 512], F32), c.psum("pF1", [128, 512], F32)])
    c.op("pool", lambda e: e.memset(rbA.t[32:33, :], -BIG), writes=[rbA])
    c.dma("sp", [(rbA.t[0:32, :], rel_bias[:, :])], writes=[rbA])
    c.dma("sp", [(ohs.t[:, :], oh_d[:, :])], writes=[ohs])
    c.dma("sp", [(jex.t[:, :], jex_d[:, :])], writes=[jex])
    for ch in range(NU // 512):
        ps = pF.next()
        c.op("pe", lambda e, ps=ps, ch=ch: e.matmul(ps.t[0:H, :], lhsT=rbA.t[:, 0:H], rhs=ohs.t[:, ch * 512:(ch + 1) * 512], start=True, stop=True),
             reads=[rbA, ohs], writes=[ps])
        c.op("act", lambda e, ps=ps, ch=ch: e.activation(out=fsb.t[:, ch * 512:(ch + 1) * 512], in_=ps.t[0:H, :], func=AF.Copy), reads=[ps], writes=[fsb])
    c.dma("sp", [(ftab[:, :], fsb.t[:, :])], reads=[fsb])
    c.barrier()
    t2 = Rot([c.sbuf("t2a", [128, WB], F32), c.sbuf("t2b", [128, WB], F32)])
    bst = Rot([c.sbuf("bsta", [128, WB], BF16), c.sbuf("bstb", [128, WB], BF16)])
    for h in range(H):
        t = t2.next()
        src = bass.AP(tensor=ftab, offset=h * NU + 1, ap=[[1, 128], [1, WB]])
        c.dma("sp", [(t.t[:, :], src)], writes=[t])
        bs = bst.next()
        for ch in range(0, WB, 512):
            w_ = min(512, WB - ch)
            ps = pF.next()
            c.op("pe", lambda e, ps=ps, t=t, ch=ch, w_=w_: e.matmul(ps.t[:, 0:w_], lhsT=jex.t[:, :], rhs=t.t[:, ch:ch + w_], start=True, stop=True),
                 reads=[jex, t], writes=[ps])
            c.op("act", lambda e, ps=ps, bs=bs, ch=ch, w_=w_: e.activation(out=bs.t[:, ch:ch + w_], in_=ps.t[:, 0:w_], func=AF.Copy), reads=[ps], writes=[bs])
        c.dma("sp", [(biasT[h], bs.t[:, :])], reads=[bs])
    c.end()

    def norm_phase(src, gcol0, shcol0, dst):
        c.begin()
        xt_r = Rot([c.sbuf("nx0", [128, 4, D], F32), c.sbuf("nx1", [128, 4, D], F32)])
        junk = c.sbuf("njunk", [128, D], BF16)
        ss_r = Rot([c.sbuf("nss0", [128, 4], F32), c.sbuf("nss1", [128, 4], F32)])
        sq_r = Rot([c.sbuf("nsq0", [128, 4], F32), c.sbuf("nsq1", [128, 4], F32)])
        rs_r = Rot([c.sbuf("nrs0", [128, 4], F32), c.sbuf("nrs1", [128, 4], F32)])
        xn_r = Rot([c.sbuf("nxn0", [128, 4, D], BF16), c.sbuf("nxn1", [128, 4, D], BF16)])
        ht_r = Rot([c.sbuf("nht0", [128, KC, 512], BF16), c.sbuf("nht1", [128, KC, 512], BF16)])
        pt_r = Rot([c.psum("npt0", [128, 512], BF16), c.psum("npt1", [128, 512], BF16), c.psum("npt2", [128, 512], BF16)])
        epsc = c.sbuf("epsc", [128, 1], F32)
        c.op("pool", lambda e: e.memset(epsc.t[:, :], EPS), writes=[epsc])
        for g in range(NG):
            xt = xt_r.next()
            c.dma("sp", [(xt.t[:, :, :], src[g * 512:(g + 1) * 512, :].rearrange("(t p) d -> p t d", p=128))], writes=[xt])
            ss, sq, rs, xn, ht = ss_r.next(), sq_r.next(), rs_r.next(), xn_r.next(), ht_r.next()
            for t in range(4):
                c.op("act", lambda e, t=t, xt=xt, ss=ss: e.activation(out=junk.t[:, :], in_=xt.t[:, t, :], func=AF.Square, accum_out=ss.t[:, t:t + 1]),
                     reads=[xt], writes=[junk, ss])
            c.op("act", lambda e, ss=ss, sq=sq: e.activation(out=sq.t[:, :], in_=ss.t[:, :], func=AF.Sqrt, scale=1.0 / D, bias=epsc.t[:, 0:1]), reads=[ss, epsc], writes=[sq])
            c.op("dve", lambda e, sq=sq, rs=rs: e.reciprocal(out=rs.t[:, :], in_=sq.t[:, :]), reads=[sq], writes=[rs])
            for t in range(4):
                c.op("act", lambda e, t=t, xt=xt, xn=xn, rs=rs: e.activation(out=xn.t[:, t, :], in_=xt.t[:, t, :], func=AF.Copy, scale=rs.t[:, t:t + 1]),
                     reads=[xt, rs], writes=[xn])
            for kc in range(KC):
                pt = pt_r.next()
                for t in range(4):
                    c.op("pe", lambda e, t=t, kc=kc, pt=pt, xn=xn: e.transpose(out=pt.t[:, t * 128:(t + 1) * 128], in_=xn.t[:, t, kc * 128:(kc + 1) * 128], identity=identb.t[:, :]),
                         reads=[xn, identb], writes=[pt])
                c.op("dve", lambda e, kc=kc, pt=pt, ht=ht: e.tensor_scalar(out=ht.t[:, kc, :], in0=pt.t[:, :], scalar1=geff.t[:, gcol0 + kc:gcol0 + kc + 1],
                                                                           scalar2=modT.t[:, shcol0 + kc:shcol0 + kc + 1], op0=ALU.mult, op1=ALU.add),
                     reads=[pt, geff, modT], writes=[ht])
            c.dma("pool", [(dst[:, g * 512:(g + 1) * 512].rearrange("(k p) s -> p k s", p=128), ht.t[:, :, :])], reads=[ht])
        c.end()

    def load_w_bf16(dst_slot, dst_ap_fn, src2d, ncols, piece=1024):
        pairs = []
        for c0 in range(0, ncols, piece):
            w_ = min(piece, ncols - c0)
            pairs.append((dst_ap_fn(c0, w_), src2d[:, c0:c0 + w_].rearrange("(k p) f -> p k f", p=128)))
        c.dma("pool", pairs, writes=[dst_slot])

    def proj_phase(src_hT, w2d, ncols_fm, ncols_tm, fm_dsts, tm_dst65, q_scale_chunks):
        c.begin()
        ncols = ncols_fm + ncols_tm
        wsb = c.sbuf("pw", [128, KC, ncols], BF16)
        load_w_bf16(wsb, lambda c0, w_: wsb.t[:, :, c0:c0 + w_], w2d, ncols)
        ht_r = Rot([c.sbuf("pht0", [128, KC, 512], BF16), c.sbuf("pht1", [128, KC, 512], BF16)])
        NJ = ncols_fm // 128
        st_r = Rot([c.sbuf("pst0", [128, max(NJ, 1), 512], BF16), c.sbuf("pst1", [128, max(NJ, 1), 512], BF16)])
        ps_r = Rot([c.psum(f"pps{i}", [128, 512], F32) for i in range(4)])
        if ncols_tm:
            vst_r = Rot([c.sbuf("pvs0", [128, 4, H, 65], BF16), c.sbuf("pvs1", [128, 4, H, 65], BF16)])
            for v in vst_r.items:
                c.op("pool", lambda e, v=v: e.memset(v.t[:, :, :, :], 1.0), writes=[v])
        for g in range(NG):
            ht = ht_r.next()
            c.dma("sp", [(ht.t[:, :, :], src_hT[:, g * 512:(g + 1) * 512].rearrange("(k p) s -> p k s", p=128))], writes=[ht])
            if NJ:
                st = st_r.next()
                for j in range(NJ):
                    ps = ps_r.next()
                    for kc in range(KC):
                        c.op("pe", lambda e, ps=ps, j=j, kc=kc, ht=ht: e.matmul(ps.t[:, :], lhsT=wsb.t[:, kc, j * 128:(j + 1) * 128], rhs=ht.t[:, kc, :],
                                                                               start=(kc == 0), stop=(kc == KC - 1)), reads=[wsb, ht], writes=[ps])
                    if j in q_scale_chunks:
                        c.op("act", lambda e, ps=ps, st=st, j=j: e.activation(out=st.t[:, j, :], in_=ps.t[:, :], func=AF.Copy, scale=0.125), reads=[ps], writes=[st])
                    else:
                        c.op("dve", lambda e, ps=ps, st=st, j=j: e.tensor_copy(st.t[:, j, :], ps.t[:, :]), reads=[ps], writes=[st])
                pairs = []
                for j in range(NJ):
                    dt_, r0 = fm_dsts[j]
                    pairs.append((dt_[r0:r0 + 128, g * 512:(g + 1) * 512], st.t[:, j, :]))
                c.dma("pool", pairs, reads=[st])
            if ncols_tm:
                vst = vst_r.next()
                for tt in range(4):
                    for half in range(ncols_tm // 512):
                        ps = ps_r.next()
                        for kc in range(KC):
                            c.op("pe", lambda e, ps=ps, kc=kc, ht=ht, tt=tt, half=half: e.matmul(ps.t[:, :], lhsT=ht.t[:, kc, tt * 128:(tt + 1) * 128],
                                                                                                rhs=wsb.t[:, kc, ncols_fm + half * 512:ncols_fm + (half + 1) * 512],
                                                                                                start=(kc == 0), stop=(kc == KC - 1)), reads=[wsb, ht], writes=[ps])
                        c.op("act", lambda e, ps=ps, vst=vst, tt=tt, half=half: e.activation(out=vst.t[:, tt, half * 8:(half + 1) * 8, 0:64],
                                                                                             in_=ps.t[:, :].rearrange("p (h d) -> p h d", d=64), func=AF.Copy),
                             reads=[ps], writes=[vst])
                c.dma("pool", [(tm_dst65[g * 512:(g + 1) * 512, :].rearrange("(t p) f -> p t f", p=128), vst.t[:, :, :, :].rearrange("p t h d -> p t (h d)"))], reads=[vst])
        c.end()

    def wo_phase(w2d, gcol_row0, xsrc=None):
        xsrc = xres if xsrc is None else xsrc
        c.begin()
        wsb = c.sbuf("ow", [128, KC, D], BF16)
        load_w_bf16(wsb, lambda c0, w_: wsb.t[:, :, c0:c0 + w_], w2d, D)
        gb = c.sbuf("ogb", [128, D], F32)
        c.dma("sp", [(gb.t[:, :], vecscr[gcol_row0:gcol_row0 + KC, :].rearrange("(o k) p -> o (k p)", o=1).partition_broadcast(128))], writes=[gb])
        at_r = Rot([c.sbuf("oat0", [128, KC, 512], BF16), c.sbuf("oat1", [128, KC, 512], BF16)])
        xt_r = Rot([c.sbuf("ox0", [128, 4, D], F32), c.sbuf("ox1", [128, 4, D], F32)])
        tmp_r = Rot([c.sbuf("otm0", [128, 512], F32), c.sbuf("otm1", [128, 512], F32)])
        ps_r = Rot([c.psum(f"ops{i}", [128, 512], F32) for i in range(4)])
        for g in range(NG):
            at = at_r.next()
            xt = xt_r.next()
            c.dma("sp", [(at.t[:, :, :], attnT[:, g * 512:(g + 1) * 512].rearrange("(k p) s -> p k s", p=128))], writes=[at])
            c.dma("sp", [(xt.t[:, :, :], xsrc[g * 512:(g + 1) * 512, :].rearrange("(t p) d -> p t d", p=128))], writes=[xt])
            for tt in range(4):
                for half in range(D // 512):
                    ps = ps_r.next()
                    for kc in range(KC):
                        c.op("pe", lambda e, ps=ps, kc=kc, at=at, tt=tt, half=half: e.matmul(ps.t[:, :], lhsT=at.t[:, kc, tt * 128:(tt + 1) * 128],
                                                                                            rhs=wsb.t[:, kc, half * 512:(half + 1) * 512],
                                                                                            start=(kc == 0), stop=(kc == KC - 1)), reads=[wsb, at], writes=[ps])
                    tmp = tmp_r.next()
                    c.op("dve", lambda e, ps=ps, tmp=tmp, half=half: e.tensor_tensor(out=tmp.t[:, :], in0=ps.t[:, :], in1=gb.t[:, half * 512:(half + 1) * 512], op=ALU.mult),
                         reads=[ps, gb], writes=[tmp])
                    c.op("pool", lambda e, tmp=tmp, xt=xt, tt=tt, half=half: e.tensor_tensor(out=xt.t[:, tt, half * 512:(half + 1) * 512], in0=xt.t[:, tt, half * 512:(half + 1) * 512],
                                                                                           in1=tmp.t[:, :], op=ALU.add), reads=[tmp, xt], writes=[xt])
            c.dma("pool", [(xres[g * 512:(g + 1) * 512, :].rearrange("(t p) d -> p t d", p=128), xt.t[:, :, :])], reads=[xt])
        c.end()

    def bc_last(ap2d, n):
        a = [list(x) for x in ap2d.ap]
        return bass.AP(tensor=ap2d.tensor, offset=ap2d.offset, ap=a + [[0, n]])

    def moba_phase():
        c.begin()
        NGN = NT * NB
        vall = c.sbuf("mvall", [128, NT, H * 65], BF16)
        c.dma("sp", [(vall.t[:, :, :], v65[:, :].rearrange("(t p) f -> p t f", p=128))], writes=[vall])
        qp_r = Rot([c.sbuf("mq0", [128, S], BF16), c.sbuf("mq1", [128, S], BF16)])
        kp_r = Rot([c.sbuf("mk0", [128, S], BF16), c.sbuf("mk1", [128, S], BF16)])
        for t_ in qp_r.items + kp_r.items:
            c.op("pool", lambda e, t_=t_: e.memset(t_.t[:, :], 0.0), writes=[t_])
        for t_ in kp_r.items:
            c.dma("sp", [(t_.t[64:64 + NB, :], esel2_d[0:NB, :])], writes=[t_])
        bt_r = Rot([c.sbuf("mbt0", [128, WB], BF16), c.sbuf("mbt1", [128, WB], BF16)])
        km = c.sbuf("mkm", [128, NB], F32)
        kmb_r = Rot([c.sbuf("mkmb0", [128, NB], BF16), c.sbuf("mkmb1", [128, NB], BF16)])
        for t_ in kmb_r.items:
            c.op("pool", lambda e, t_=t_: e.memset(t_.t[:, :], 0.0), writes=[t_])
        VM = c.sbuf("mVM", [128, NT, NB], F32)
        NV = c.sbuf("mNV", [128, NT, NB], F32)
        TM = c.sbuf("mTM", [128, NT, NB], F32)
        gmA = c.sbuf("mgmA", [128, NGN], F32)
        gmB = c.sbuf("mgmB", [128, NGN], F32)
        gmC = c.sbuf("mgmC", [128, NGN], F32)
        tA = c.sbuf("mtA", [128, NGN], F32)
        mx = c.sbuf("mmx", [128, NT], F32)
        mvs = c.sbuf("mmvs", [128, NGN], F32)
        sb_r = Rot([c.sbuf(f"msb{i}", [128, 512], F32) for i in range(3)])
        a_r = Rot([c.sbuf(f"ma{i}", [128, 512], BF16) for i in range(4)])
        rsb_r = Rot([c.sbuf("mrs0", [128, 512], F32), c.sbuf("mrs1", [128, 512], F32)])
        for t_ in rsb_r.items:
            c.op("pool", lambda e, t_=t_: e.memset(t_.t[:, :], 0.0), writes=[t_])
        sel64 = c.sbuf("msel64", [128, 64], F32)
        c.op("pool", lambda e: e.memset(sel64.t[:, :], 0.0), writes=[sel64])
        c.op("pool", lambda e: e.memset(sel64.t[64:65, :], 1.0), writes=[sel64])
        rb_r = Rot([c.sbuf("mrb0", [64, 512], F32), c.sbuf("mrb1", [64, 512], F32)])
        ast_r = Rot([c.sbuf("mas0", [64, 512], BF16), c.sbuf("mas1", [64, 512], BF16)])
        pS = Rot([c.psum(f"mpS{i}", [128, 512], F32) for i in range(3)])
        pO = Rot([c.psum("mpO0", [128, 512], F32), c.psum("mpO1", [128, 512], F32)])
        pG = c.psum("mpG", [128, 512], F32)
        pX = c.psum("mpX", [128, 512], F32)
        pR = c.psum("mpR", [128, 512], F32)
        c.op("pool", lambda e: e.memset(VM.t[:, :, :], -BIG), writes=[VM])
        c.op("pool", lambda e: e.memset(NV.t[:, :, :], 0.0), writes=[NV])
        c.op("pool", lambda e: e.memset(TM.t[:, :, :], -BIG), writes=[TM])
        for i in range(NT):
            cur = i // 2
            if cur > 0:
                c.op("pool", lambda e, i=i, cur=cur: e.memset(VM.t[:, i, 0:cur], 0.0), writes=[VM])
                c.op("pool", lambda e, i=i, cur=cur: e.memset(NV.t[:, i, 0:cur], -BIG), writes=[NV])
            c.op("pool", lambda e, i=i, cur=cur: e.memset(TM.t[:, i, 0:cur + 1], 0.0), writes=[TM])
        v3 = lambda s_: s_.t[:, :].rearrange("p (i n) -> p i n", n=NB)
        state = {}

        def pre1(h):
            qp, kp, kmb = qp_r.next(), kp_r.next(), kmb_r.next()
            c.dma("sp", [(qp.t[0:64, :], qT[h * 64:h * 64 + 64, :])], writes=[qp])
            c.dma("sp", [(kp.t[0:64, :], kT[h * 64:h * 64 + 64, :])], writes=[kp])
            bt = bt_r.next()
            c.dma("sp", [(bt.t[:, :], biasT[h])], writes=[bt])
            return [qp, kp, kmb, bt]

        def pre2(h, st_):
            qp, kp, kmb, bt = st_
            c.op("dve", lambda e: e.tensor_reduce(out=km.t[0:64, :], in_=kp.t[0:64, :].rearrange("p (n b) -> p n b", b=256), axis=AX.X, op=ALU.add), reads=[kp], writes=[km])
            c.op("dve", lambda e: e.tensor_scalar(out=kmb.t[0:64, :], in0=km.t[0:64, :], scalar1=1.0 / 256, scalar2=None, op0=ALU.mult), reads=[km], writes=[kmb])
            for i in range(NT):
                c.op("pe", lambda e, i=i: e.matmul(pG.t[:, i * NB:(i + 1) * NB], lhsT=qp.t[:, i * 128:(i + 1) * 128], rhs=kmb.t[:, 0:NB], start=True, stop=True),
                     reads=[qp, kmb], writes=[pG])
            c.op("dve", lambda e: e.tensor_tensor(out=gmA.t[:, :], in0=pG.t[:, 0:NGN], in1=VM.t[:, :, :].rearrange("p i n -> p (i n)"), op=ALU.add), reads=[pG, VM], writes=[gmA])
            src = gmA
            for (dst,) in ((gmB,), (gmC,)):
                c.op("dve", lambda e, src=src: e.tensor_reduce(out=mx.t[:, :], in_=v3(src), axis=AX.X, op=ALU.max), reads=[src], writes=[mx])
                c.op("dve", lambda e, src=src: e.tensor_tensor(out=v3(tA), in0=v3(src), in1=bc_last(mx.t[:, :], NB), op=ALU.is_ge), reads=[src, mx], writes=[tA])
                c.op("dve", lambda e, src=src, dst=dst: e.scalar_tensor_tensor(out=dst.t[:, :], in0=tA.t[:, :], scalar=-BIG, in1=src.t[:, :], op0=ALU.mult, op1=ALU.add),
                     reads=[tA, src], writes=[dst])
                src = dst
            c.op("dve", lambda e: e.tensor_reduce(out=mx.t[:, :], in_=v3(gmC), axis=AX.X, op=ALU.max), reads=[gmC], writes=[mx])
            c.op("dve", lambda e: e.tensor_tensor(out=v3(tA), in0=v3(gmA), in1=bc_last(mx.t[:, :], NB), op=ALU.is_lt), reads=[gmA, mx], writes=[tA])
            c.op("dve", lambda e: e.tensor_tensor(out=tA.t[:, :], in0=tA.t[:, :], in1=NV.t[:, :, :].rearrange("p i n -> p (i n)"), op=ALU.mult), reads=[tA, NV], writes=[tA])
            c.op("dve", lambda e: e.tensor_tensor(out=mvs.t[:, :], in0=tA.t[:, :], in1=TM.t[:, :, :].rearrange("p i n -> p (i n)"), op=ALU.add), reads=[tA, TM], writes=[mvs])

        def pre3(h, st_):
            qp, kp, kmb, bt = st_
            for i0 in range(0, NT, 4):
                for i in range(i0, i0 + 4):
                    c.op("pe", lambda e, i=i, i0=i0: e.transpose(out=pX.t[0:NB, (i - i0) * 128:(i - i0 + 1) * 128], in_=mvs.t[:, i * NB:(i + 1) * NB], identity=identf.t[:, :]),
                         reads=[mvs, identf], writes=[pX])
                c.op("act", lambda e, i0=i0: e.activation(out=qp.t[64:64 + NB, i0 * 128:(i0 + 4) * 128], in_=pX.t[0:NB, 0:512], func=AF.Copy), reads=[pX], writes=[qp])
            return (qp, kp, bt)

        def main(h, ctxh):
            qp, kp, bt = ctxh
            tiles = [(Q, kt) for Q in range(NG) for kt in range(4 * Q + 4)]
            n_ = len(tiles)
            T = {}
            pos = {}
            fin = {}
            nst = None
            res = None
            for step in range(n_ + 3 + 6):
                if h + 1 < H:
                    if step == n_ // 8:
                        nst = pre1(h + 1)
                    if step == n_ // 3:
                        pre2(h + 1, nst)
                    if step == (2 * n_) // 3:
                        res = pre3(h + 1, nst)
                for f_ in fin.pop(step, []):
                    f_()
                j = step - 2
                if 0 <= j < n_:
                    Q, kt = tiles[j]
                    nkt = 4 * Q + 4
                    a = T.pop(j)["a"]
                    if kt == 0:
                        pos[Q] = pO.next()
                    po = pos[Q]
                    c.op("pe", lambda e, po=po, a=a, kt=kt, nkt=nkt: e.matmul(po.t[0:65, :], lhsT=vall.t[:, kt, h * 65:(h + 1) * 65], rhs=a.t[:, :],
                                                                            start=(kt == 0), stop=(kt == nkt - 1)), reads=[vall, a], writes=[po])
                    if kt == nkt - 1:
                        rsb = rsb_r.next()
                        rb = rb_r.next()
                        ast = ast_r.next()

                        def f1(po=po, rsb=rsb):
                            c.op("dve", lambda e: e.reciprocal(out=rsb.t[64:65, :], in_=po.t[64:65, :]), reads=[po], writes=[rsb])

                        def f2(rsb=rsb):
                            c.op("pe", lambda e: e.matmul(pR.t[0:64, :], lhsT=sel64.t[:, :], rhs=rsb.t[:, :], start=True, stop=True), reads=[sel64, rsb], writes=[pR])

                        def f3(po=po, rb=rb, ast=ast, Q=Q):
                            c.op("dve", lambda e: e.tensor_copy(rb.t[:, :], pR.t[0:64, :]), reads=[pR], writes=[rb])
                            c.op("dve", lambda e: e.tensor_tensor(out=ast.t[:, :], in0=po.t[0:64, :], in1=rb.t[:, :], op=ALU.mult), reads=[po, rb], writes=[ast])
                            c.dma("pool", [(attnT[h * 64:(h + 1) * 64, Q * 512:(Q + 1) * 512], ast.t[:, :])], reads=[ast])
                        fin.setdefault(step + 2, []).append(f1)
                        fin.setdefault(step + 4, []).append(f2)
                        fin.setdefault(step + 6, []).append(f3)
                j = step - 1
                if 0 <= j < n_:
                    a = a_r.next()
                    ps = T[j].pop("ps")
                    c.op("act", lambda e, ps=ps, a=a: e.activation(out=a.t[:, :], in_=ps.t[:, :], func=AF.Exp), reads=[ps], writes=[a])
                    T[j]["a"] = a
                j = step
                if j < n_:
                    Q, kt = tiles[j]
                    delta = 512 * Q - 128 * kt
                    ps = pS.next()
                    c.op("pe", lambda e, ps=ps, kt=kt, Q=Q: e.matmul(ps.t[:, :], lhsT=kp.t[:, kt * 128:(kt + 1) * 128], rhs=qp.t[:, Q * 512:(Q + 1) * 512],
                                                                   start=True, stop=False), reads=[kp, qp], writes=[ps])
                    o = (delta + 384) if delta <= DMAXNEAR else 1330
                    c.op("pe", lambda e, ps=ps, o=o: e.matmul(ps.t[:, :], lhsT=identb.t[:, :], rhs=bt.t[:, o:o + 512], start=False, stop=True), reads=[identb, bt], writes=[ps])
                    T[j] = {"ps": ps}
            for k_ in sorted(fin):
                for f_ in fin[k_]:
                    f_()
            return res

        st0 = pre1(0)
        pre2(0, st0)
        nxt = pre3(0, st0)
        for h in range(H):
            nxt = main(h, nxt)
        c.end()

    def sb_phase():
        c.begin()
        vall = c.sbuf("svall", [128, NT, H * 65], BF16)
        c.dma("sp", [(vall.t[:, :, :], v65[:, :].rearrange("(t p) f -> p t f", p=128))], writes=[vall])
        ustr = c.sbuf("sustr", [128, 128], BF16)
        cm = c.sbuf("scm", [128, 896], BF16)
        c.dma("sp", [(ustr.t[:, :], ustrict_d[:, :])], writes=[ustr])
        c.dma("sp", [(cm.t[:, :], cmask_d[:, :])], writes=[cm])
        qp_r = Rot([c.sbuf("sq0", [128, S], BF16), c.sbuf("sq1", [128, S], BF16)])
        kp_r = Rot([c.sbuf("sk0", [128, S], BF16), c.sbuf("sk1", [128, S], BF16)])
        for t_ in qp_r.items + kp_r.items:
            c.op("pool", lambda e, t_=t_: e.memset(t_.t[:, :], 0.0), writes=[t_])
        e_r = Rot([c.sbuf(f"se{i}", [128, 512], F32) for i in range(2)])
        lp_r = Rot([c.sbuf(f"slp{i}", [128, 512], BF16) for i in range(5)])
        ln_r = Rot([c.sbuf(f"sln{i}", [128, 512], BF16) for i in range(7)])
        a_r = Rot([c.sbuf(f"sa{i}", [128, 512], BF16) for i in range(4)])
        tot_r = Rot([c.sbuf(f"stot{i}", [128, 512], BF16) for i in range(4)])
        ast_r = Rot([c.sbuf("sas0", [64, 512], BF16), c.sbuf("sas1", [64, 512], BF16)])
        pZ = Rot([c.psum(f"spZ{i}", [128, 512], F32) for i in range(3)])
        pT = Rot([c.psum("spT0", [128, 512], F32), c.psum("spT1", [128, 512], F32)])
        pTOT = Rot([c.psum("spTOT0", [128, 512], F32)])
        pO = Rot([c.psum("spO0", [128, 512], F32), c.psum("spO1", [128, 512], F32)])
        def sload(h_):
            q_, k_ = qp_r.next(), kp_r.next()
            c.dma("sp", [(q_.t[0:64, :], qT[h_ * 64:h_ * 64 + 64, :])], writes=[q_])
            c.dma("sp", [(k_.t[0:64, :], kT[h_ * 64:h_ * 64 + 64, :])], writes=[k_])
            return q_, k_
        nxt_qk = sload(0)
        for h in range(H):
            qp, kp = nxt_qk
            tiles = [(Q, idx, 4 * Q + 3 - idx) for Q in range(NG) for idx in range(4 * Q + 4)]
            T = {}
            qstate = {}
            NST = 6
            fin = {}
            for step in range(len(tiles) + NST + 2):
                if step == len(tiles) // 2 and h + 1 < H:
                    nxt_qk = sload(h + 1)
                for f_ in fin.pop(step, []):
                    f_()
                j = step - 5
                if 0 <= j < len(tiles):
                    Q, idx, kt = tiles[j]
                    nkt = 4 * Q + 4
                    a = T[j].pop("a")
                    po = qstate[Q]["po"]
                    c.op("pe", lambda e, po=po, a=a, kt=kt, idx=idx, nkt=nkt: e.matmul(po.t[0:64, :], lhsT=vall.t[:, kt, h * 65:h * 65 + 64], rhs=a.t[:, :],
                                                                                     start=(idx == 0), stop=(idx == nkt - 1)), reads=[vall, a], writes=[po])
                    if idx == nkt - 1:
                        ast = ast_r.next()

                        def f1(po=po, ast=ast, Q=Q):
                            c.op("dve", lambda e: e.tensor_copy(ast.t[:, :], po.t[0:64, :]), reads=[po], writes=[ast])
                            c.dma("pool", [(attnT[h * 64:(h + 1) * 64, Q * 512:(Q + 1) * 512], ast.t[:, :])], reads=[ast])
                        fin.setdefault(step + 2, []).append(f1)
                    del T[j]
                j = step - 4
                if 0 <= j < len(tiles):
                    Q, idx, kt = tiles[j]
                    pt = T[j].pop("pt")
                    a = a_r.next()
                    c.op("act", lambda e, pt=pt, a=a: e.activation(out=a.t[:, :], in_=pt.t[:, :], func=AF.Exp, scale=-1.0), reads=[pt], writes=[a])
                    if kt >= 4 * Q:
                        o = 512 * Q - 128 * kt + 384
                        c.op("pool", lambda e, a=a, o=o: e.tensor_tensor(out=a.t[:, :], in0=a.t[:, :], in1=cm.t[:, o:o + 512], op=ALU.mult), reads=[a, cm], writes=[a])
                    T[j]["a"] = a
                    if T[j].pop("ck", False):
                        qs = qstate[Q]
                        tsb = tot_r.next()
                        ptot = qs["ptot"]
                        c.op("dve", lambda e, ptot=ptot, tsb=tsb: e.tensor_copy(tsb.t[:, :], ptot.t[:, :]), reads=[ptot], writes=[tsb])
                        qs["ck"][idx + 1] = tsb
                j = step - 3
                if 0 <= j < len(tiles):
                    Q, idx, kt = tiles[j]
                    nkt = 4 * Q + 4
                    ln = T[j].pop("ln")
                    lp = T[j].pop("lp")
                    if idx == 0:
                        qstate[Q] = {"ptot": pTOT.next(), "ck": {}, "po": pO.next(), "ln": {}}
                    qs = qstate[Q]
                    qs["ln"][idx] = ln
                    n2 = max(0, idx - 2)
                    direct = list(range(n2, idx))
                    pt = pT.next()
                    c.op("pe", lambda e, pt=pt, ln=ln: e.matmul(pt.t[:, :], lhsT=ustr.t[:, :], rhs=ln.t[:, :], start=True, stop=False), reads=[ustr, ln], writes=[pt])
                    for dj in direct:
                        lnd = qs["ln"][dj]
                        c.op("pe", lambda e, pt=pt, lnd=lnd: e.matmul(pt.t[:, :], lhsT=onesb.t[:, :], rhs=lnd.t[:, :], start=False, stop=False), reads=[onesb, lnd], writes=[pt])
                    if n2 > 0:
                        tsb = qs["ck"].pop(n2)
                        c.op("pe", lambda e, pt=pt, tsb=tsb: e.matmul(pt.t[:, :], lhsT=identb.t[:, :], rhs=tsb.t[:, :], start=False, stop=False), reads=[identb, tsb], writes=[pt])
                    c.op("pe", lambda e, pt=pt, lp=lp: e.matmul(pt.t[:, :], lhsT=identb.t[:, :], rhs=lp.t[:, :], start=False, stop=True), reads=[identb, lp], writes=[pt])
                    if idx <= nkt - 4:
                        ptot = qs["ptot"]
                        c.op("pe", lambda e, ptot=ptot, ln=ln, idx=idx: e.matmul(ptot.t[:, :], lhsT=onesb.t[:, :], rhs=ln.t[:, :], start=(idx == 0), stop=True, skip_group_check=True),
                             reads=[onesb, ln], writes=[ptot])
                        T[j]["ck"] = True
                    qs["ln"].pop(idx - 2, None)
                    T[j]["pt"] = pt
                j = step - 2
                if 0 <= j < len(tiles):
                    Q, idx, kt = tiles[j]
                    pz = T[j].pop("pz")
                    lp = T[j]["lp"]
                    ln = ln_r.next()
                    c.op("dve", lambda e, pz=pz, lp=lp, ln=ln: e.tensor_tensor(out=ln.t[:, :], in0=pz.t[:, :], in1=lp.t[:, :], op=ALU.add), reads=[pz, lp], writes=[ln])
                    if kt >= 4 * Q:
                        o = 512 * Q - 128 * kt + 384
                        c.op("pool", lambda e, ln=ln, o=o: e.tensor_tensor(out=ln.t[:, :], in0=ln.t[:, :], in1=cm.t[:, o:o + 512], op=ALU.mult), reads=[ln, cm], writes=[ln])
                    T[j]["ln"] = ln
                j = step - 1
                if 0 <= j < len(tiles):
                    pz = T[j]["pz"]
                    ee, lp = e_r.next(), lp_r.next()
                    c.op("act", lambda e, pz=pz, ee=ee: e.activation(out=ee.t[:, :], in_=pz.t[:, :], func=AF.Exp, scale=-1.0), reads=[pz], writes=[ee])
                    c.op("act", lambda e, ee=ee, lp=lp: e.activation(out=lp.t[:, :], in_=ee.t[:, :], func=AF.Ln, bias=1.0), reads=[ee], writes=[lp])
                    T[j]["lp"] = lp
                j = step
                if j < len(tiles):
                    Q, idx, kt = tiles[j]
                    pz = pZ.next()
                    c.op("pe", lambda e, pz=pz, kt=kt, Q=Q: e.matmul(pz.t[:, :], lhsT=kp.t[:, kt * 128:(kt + 1) * 128], rhs=qp.t[:, Q * 512:(Q + 1) * 512],
                                                                   start=True, stop=True), reads=[kp, qp], writes=[pz])
                    T[j] = {"pz": pz}
            for k_ in sorted(fin):
                for f_ in fin[k_]:
                    f_()
        c.end()

    def router_phase(rw2d):
        c.begin()
        rw = c.sbuf("rrw", [128, KC, E], BF16)
        c.dma("pool", [(rw.t[:, :, :], rw2d.rearrange("(k p) e -> p k e", p=128))], writes=[rw])
        ht_r = Rot([c.sbuf("rht0", [128, KC, 512], BF16), c.sbuf("rht1", [128, KC, 512], BF16)])
        cst = c.sbuf("rcst", [128, NT, E], F32)
        mk = lambda nm, w: Rot([c.sbuf(nm + "0", [128, w], F32), c.sbuf(nm + "1", [128, w], F32)])
        lg_r, t8_r, d_r, ex_r, w1_r, w2_r, c1_r, c2_r = mk("rlg", 8), mk("rt8", 8), mk("rd", 1), mk("rex", 1), mk("rw1", 1), mk("rw2", 1), mk("rc1", 8), mk("rc2", 8)
        pL = Rot([c.psum("rpL0", [128, 512], F32), c.psum("rpL1", [128, 512], F32)])
        for g in range(NG):
            ht = ht_r.next()
            c.dma("sp", [(ht.t[:, :, :], hT[:, g * 512:(g + 1) * 512].rearrange("(k p) s -> p k s", p=128))], writes=[ht])
            for tt in range(4):
                pl = pL.next()
                for kc in range(KC):
                    c.op("pe", lambda e, pl=pl, kc=kc, ht=ht, tt=tt: e.matmul(pl.t[:, 0:E], lhsT=ht.t[:, kc, tt * 128:(tt + 1) * 128], rhs=rw.t[:, kc, :], start=(kc == 0), stop=(kc == KC - 1)),
                         reads=[ht, rw], writes=[pl])
                lg, t8, d_, ex, w1, w2, c1, c2 = (r_.next() for r_ in (lg_r, t8_r, d_r, ex_r, w1_r, w2_r, c1_r, c2_r))
                c.op("dve", lambda e, pl=pl, lg=lg: e.tensor_copy(lg.t[:, :], pl.t[:, 0:E]), reads=[pl], writes=[lg])
                c.op("dve", lambda e, lg=lg, t8=t8: e.max(out=t8.t[:, :], in_=lg.t[:, :]), reads=[lg], writes=[t8])
                c.op("dve", lambda e, t8=t8, d_=d_: e.tensor_tensor(out=d_.t[:, :], in0=t8.t[:, 1:2], in1=t8.t[:, 0:1], op=ALU.subtract), reads=[t8], writes=[d_])
                c.op("act", lambda e, d_=d_, ex=ex: e.activation(out=ex.t[:, :], in_=d_.t[:, :], func=AF.Exp), reads=[d_], writes=[ex])
                c.op("dve", lambda e, ex=ex, w1=w1: e.tensor_scalar(out=w1.t[:, :], in0=ex.t[:, :], scalar1=1.0, scalar2=None, op0=ALU.add), reads=[ex], writes=[w1])
                c.op("dve", lambda e, w1=w1: e.reciprocal(out=w1.t[:, :], in_=w1.t[:, :]), reads=[w1], writes=[w1])
                c.op("dve", lambda e, w1=w1, ex=ex, w2=w2: e.tensor_tensor(out=w2.t[:, :], in0=w1.t[:, :], in1=ex.t[:, :], op=ALU.mult), reads=[w1, ex], writes=[w2])
                c.op("dve", lambda e, lg=lg, t8=t8, w1=w1, c1=c1: e.tensor_scalar(out=c1.t[:, :], in0=lg.t[:, :], scalar1=t8.t[:, 0:1], scalar2=w1.t[:, 0:1], op0=ALU.is_equal, op1=ALU.mult),
                     reads=[lg, t8, w1], writes=[c1])
                c.op("dve", lambda e, lg=lg, t8=t8, w2=w2, c2=c2: e.tensor_scalar(out=c2.t[:, :], in0=lg.t[:, :], scalar1=t8.t[:, 1:2], scalar2=w2.t[:, 0:1], op0=ALU.is_equal, op1=ALU.mult),
                     reads=[lg, t8, w2], writes=[c2])
                ti = g * 4 + tt
                c.op("dve", lambda e, c1=c1, c2=c2, ti=ti: e.tensor_tensor(out=cst.t[:, ti, :], in0=c1.t[:, :], in1=c2.t[:, :], op=ALU.add), reads=[c1, c2], writes=[cst])
        c.dma("sp", [(comb_tm[:, :].rearrange("(t p) e -> p t e", p=128), cst.t[:, :, :])], reads=[cst])
        c.end()

    def ffn_phase(experts, F_, gcol_row0, use_comb):
        c.begin()
        NF = F_ // 128
        NTT = TS // 128
        NTG = TS // 512
        PF = 2
        PW2 = 7
        hts = c.sbuf("fht", [128, KC, TS], BF16)
        actT = c.sbuf("fact", [128, NF, TS], BF16)
        acc = c.sbuf("facc", [128, NTT, D], F32)
        gb = c.sbuf("fgb", [128, D], F32)
        c.dma("sp", [(gb.t[:, :], vecscr[gcol_row0:gcol_row0 + KC, :].rearrange("(o k) p -> o (k p)", o=1).partition_broadcast(128))], writes=[gb])
        wg_r = Rot([c.sbuf("fwg0", [128, KC, 2 * PF * 128], BF16), c.sbuf("fwg1", [128, KC, 2 * PF * 128], BF16)])
        w2_r = Rot([c.sbuf("fw20", [128, PW2, D], BF16), c.sbuf("fw21", [128, PW2, D], BF16)])
        cbt = c.sbuf("fcbt", [128, NTT, E], F32)
        sg_r = Rot([c.sbuf(f"fsg{i}", [128, 512], F32) for i in range(3)])
        xt_r = Rot([c.sbuf("fx0", [128, D], F32), c.sbuf("fx1", [128, D], F32)])
        pGU = Rot([c.psum(f"fpg{i}", [128, 512], F32) for i in range(4)])
        pY = Rot([c.psum(f"fpy{i}", [128, 512], F32) for i in range(3)])
        for st in range(S // TS):
            t0 = st * TS
            c.dma("sp", [(hts.t[:, :, :], hT[:, t0:t0 + TS].rearrange("(k p) s -> p k s", p=128))], writes=[hts])
            if use_comb:
                c.dma("sp", [(cbt.t[:, :, :], comb_tm[t0:t0 + TS, :].rearrange("(t p) e -> p t e", p=128))], writes=[cbt])
            for ei, (w13, w2) in enumerate(experts):
                for f0 in range(0, NF, PF):
                    nf = min(PF, NF - f0)
                    wg = wg_r.next()
                    c.dma("pool", [(wg.t[:, :, 0:nf * 128], w13[:, f0 * 128:(f0 + nf) * 128].rearrange("(k p) f -> p k f", p=128)),
                                   (wg.t[:, :, PF * 128:PF * 128 + nf * 128], w13[:, F_ + f0 * 128:F_ + (f0 + nf) * 128].rearrange("(k p) f -> p k f", p=128))], writes=[wg])
                    for j in range(nf):
                        fc = f0 + j
                        for tg in range(NTG):
                            pg, pu = pGU.next(), pGU.next()
                            for kc in range(KC):
                                c.op("pe", lambda e, pg=pg, wg=wg, j=j, kc=kc, tg=tg: e.matmul(pg.t[:, :], lhsT=wg.t[:, kc, j * 128:(j + 1) * 128], rhs=hts.t[:, kc, tg * 512:(tg + 1) * 512],
                                                                                              start=(kc == 0), stop=(kc == KC - 1)), reads=[wg, hts], writes=[pg])
                            for kc in range(KC):
                                c.op("pe", lambda e, pu=pu, wg=wg, j=j, kc=kc, tg=tg: e.matmul(pu.t[:, :], lhsT=wg.t[:, kc, (PF + j) * 128:(PF + j + 1) * 128], rhs=hts.t[:, kc, tg * 512:(tg + 1) * 512],
                                                                                              start=(kc == 0), stop=(kc == KC - 1)), reads=[wg, hts], writes=[pu])
                            sg = sg_r.next()
                            c.op("act", lambda e, pg=pg, sg=sg: e.activation(out=sg.t[:, :], in_=pg.t[:, :], func=AF.Silu), reads=[pg], writes=[sg])
                            sgc = sg
                            c.op("dve", lambda e, pu=pu, sgc=sgc, fc=fc, tg=tg: e.tensor_tensor(out=actT.t[:, fc, tg * 512:(tg + 1) * 512], in0=pu.t[:, :], in1=sgc.t[:, :], op=ALU.mult),
                                 reads=[pu, sgc], writes=[actT])
                for p0 in range(0, NF, PW2):
                    npc = min(PW2, NF - p0)
                    w2s = w2_r.next()
                    c.dma("pool", [(w2s.t[:, 0:npc, :], w2[p0 * 128:(p0 + npc) * 128, :].rearrange("(f p) d -> p f d", p=128))], writes=[w2s])
                    first = (ei == 0 and p0 == 0)
                    for tt in range(NTT):
                        for half in range(D // 512):
                            py = pY.next()
                            for j in range(npc):
                                c.op("pe", lambda e, py=py, j=j, p0=p0, tt=tt, half=half, w2s=w2s, npc=npc: e.matmul(py.t[:, :], lhsT=actT.t[:, p0 + j, tt * 128:(tt + 1) * 128],
                                                                                                                rhs=w2s.t[:, j, half * 512:(half + 1) * 512], start=(j == 0), stop=(j == npc - 1)),
                                     reads=[actT, w2s], writes=[py])
                            asl = acc.t[:, tt, half * 512:(half + 1) * 512]
                            if use_comb:
                                csc = cbt.t[:, tt, ei:ei + 1]
                                if first:
                                    c.op("act", lambda e, py=py, asl=asl, csc=csc: e.activation(out=asl, in_=py.t[:, :], func=AF.Copy, scale=csc), reads=[py, cbt], writes=[acc])
                                else:
                                    c.op("dve", lambda e, py=py, asl=asl, csc=csc: e.scalar_tensor_tensor(out=asl, in0=py.t[:, :], scalar=csc, in1=asl, op0=ALU.mult, op1=ALU.add),
                                         reads=[py, acc, cbt], writes=[acc])
                            elif first:
                                c.op("act", lambda e, py=py, asl=asl: e.activation(out=asl, in_=py.t[:, :], func=AF.Copy), reads=[py], writes=[acc])
                            else:
                                c.op("dve", lambda e, py=py, asl=asl: e.tensor_tensor(out=asl, in0=py.t[:, :], in1=asl, op=ALU.add), reads=[py, acc], writes=[acc])
            for tt in range(NTT):
                xt = xt_r.next()
                c.dma("sp", [(xt.t[:, :], xres[t0 + tt * 128:t0 + (tt + 1) * 128, :])], writes=[xt])
                c.op("pool", lambda e, tt=tt: e.tensor_tensor(out=acc.t[:, tt, :], in0=acc.t[:, tt, :], in1=gb.t[:, :], op=ALU.mult), reads=[acc, gb], writes=[acc])
                c.op("pool", lambda e, tt=tt, xt=xt: e.tensor_tensor(out=xt.t[:, :], in0=xt.t[:, :], in1=acc.t[:, tt, :], op=ALU.add), reads=[acc, xt], writes=[xt])
                c.dma("pool", [(xres[t0 + tt * 128:t0 + (tt + 1) * 128, :], xt.t[:, :])], reads=[xt])
        c.end()

    def final_phase():
        c.begin()
        gb = c.sbuf("zgb", [128, D], F32)
        c.dma("sp", [(gb.t[:, :], final_norm_g[0:1, :].partition_broadcast(128))], writes=[gb])
        xt_r = Rot([c.sbuf("zx0", [128, 4, D], F32), c.sbuf("zx1", [128, 4, D], F32)])
        junk = c.sbuf("zjunk", [128, D], BF16)
        ss_r = Rot([c.sbuf("zss0", [128, 4], F32), c.sbuf("zss1", [128, 4], F32)])
        sq_r = Rot([c.sbuf("zsq0", [128, 4], F32), c.sbuf("zsq1", [128, 4], F32)])
        rs_r = Rot([c.sbuf("zrs0", [128, 4], F32), c.sbuf("zrs1", [128, 4], F32)])
        epsc = c.sbuf("zeps", [128, 1], F32)
        c.op("pool", lambda e: e.memset(epsc.t[:, :], EPS), writes=[epsc])
        for g in range(NG):
            xt = xt_r.next()
            c.dma("sp", [(xt.t[:, :, :], xres[g * 512:(g + 1) * 512, :].rearrange("(t p) d -> p t d", p=128))], writes=[xt])
            ss, sq, rs = ss_r.next(), sq_r.next(), rs_r.next()
            for t in range(4):
                c.op("act", lambda e, t=t, xt=xt, ss=ss: e.activation(out=junk.t[:, :], in_=xt.t[:, t, :], func=AF.Square, accum_out=ss.t[:, t:t + 1]), reads=[xt], writes=[junk, ss])
            c.op("act", lambda e, ss=ss, sq=sq: e.activation(out=sq.t[:, :], in_=ss.t[:, :], func=AF.Sqrt, scale=1.0 / D, bias=epsc.t[:, 0:1]), reads=[ss, epsc], writes=[sq])
            c.op("dve", lambda e, sq=sq, rs=rs: e.reciprocal(out=rs.t[:, :], in_=sq.t[:, :]), reads=[sq], writes=[rs])
            for t in range(4):
                c.op("dve", lambda e, t=t, xt=xt, rs=rs: e.scalar_tensor_tensor(out=xt.t[:, t, :], in0=xt.t[:, t, :], scalar=rs.t[:, t:t + 1], in1=gb.t[:, :], op0=ALU.mult, op1=ALU.mult),
                     reads=[xt, rs, gb], writes=[xt])
            c.dma("pool", [(out_d[g * 512:(g + 1) * 512, :].rearrange("(t p) d -> p t d", p=128), xt.t[:, :, :])], reads=[xt])
        c.end()

    qdst = [(qT, j * 128) for j in range(KC)]
    kdst = [(kT, j * 128) for j in range(KC)]
    for l in range(4):
        norm_phase(x_in if l == 0 else xres, l * KC, col_mod(l, 0), hT)
        if l < 2:
            proj_phase(hT, a_wqkv[l], 2 * D, D, qdst + kdst, v65, set(range(KC)))
            moba_phase()
            wo_phase(a_wo[l], col_mod(l, 2), x_in if l == 0 else xres)
        else:
            j = l - 2
            if j == 0:
                norm_phase(xres, 8 * KC, COL_KV, attnT)
                proj_phase(attnT, b_wkv, D, D, kdst, v65, set())
            proj_phase(hT, b_wq[j], D, 0, qdst, None, set(range(KC)))
            sb_phase()
            wo_phase(b_wo[j], col_mod(l, 2))
        norm_phase(xres, (4 + l) * KC, col_mod(l, 3), hT)
        if l % 2 == 0:
            ffn_phase([(ffn_w13[l // 2], ffn_w2[l // 2])], FF, col_mod(l, 5), False)
        else:
            router_phase(router_w[l // 2])
            ffn_phase([(moe_w13[l // 2, e], moe_w2[l // 2, e]) for e in range(E)], FE, col_mod(l, 5), True)
    final_phase()
    c.barrier()
    return c


_CACHE = {}


def kernel(**inputs):
    cfg = inputs.pop("_cfg", None) or CFG_FULL
    runner = inputs.pop("_runner", None)
    key = tuple(sorted(cfg.items()))
    if key not in _CACHE:
        _CACHE[key] = (build(cfg), make_consts(cfg))
    c, consts = _CACHE[key]
    S, D = cfg["S"], cfg["D"]
    x = np.asarray(inputs["x"], np.float32)
    B = x.shape[0]
    f = lambda k: np.ascontiguousarray(np.asarray(inputs[k], np.float32))
    shared = {
        "rel_bias": f("rel_bias"), "mod_w": f("mod_w"), "mod_b": f("mod_b").reshape(1, -1),
        "norm_mix_g": f("norm_mix_g"), "norm_ffn_g": f("norm_ffn_g"), "a_wqkv": f("a_wqkv"), "a_wo": f("a_wo"),
        "kv_norm_g": f("kv_norm_g").reshape(1, -1), "kv_mod_w": f("kv_mod_w"), "kv_mod_b": f("kv_mod_b").reshape(1, -1),
        "b_wkv": f("b_wkv"), "b_wq": f("b_wq"), "b_wo": f("b_wo"), "ffn_w13": f("ffn_w13"), "ffn_w2": f("ffn_w2"),
        "router_w": f("router_w"), "moe_w13": f("moe_w13"), "moe_w2": f("moe_w2"),
        "final_norm_g": f("final_norm_g").reshape(1, -1),
    }
    shared.update(consts)
    cc = np.asarray(inputs["c"], np.float32)
    in_maps = []
    for b in range(B):
        m = dict(shared)
        m["x"] = np.ascontiguousarray(x[b])
        m["c"] = np.ascontiguousarray(cc[b:b + 1])
        in_maps.append(m)
    if runner is not None:
        res = runner(c.nc, in_maps)
    else:
        res = run_bass_kernel_spmd(c.nc, in_maps, core_ids=list(range(B))).results
    return np.stack([np.asarray(r["out"], np.float32) for r in res], axis=0)
```

```python
import bisect
import math
from contextlib import ExitStack
import numpy as np
import ml_dtypes
import concourse.bass as bass
import concourse.mybir as mybir
from concourse.bass_utils import run_bass_kernel_spmd

F32 = mybir.dt.float32
BF16 = mybir.dt.bfloat16
ALU = mybir.AluOpType
AF = mybir.ActivationFunctionType
AX = mybir.AxisListType
SEM_ROT = 30000
BIG = 1.0e30
EPS = 1e-6

CFG_FULL = dict(S=4096, D=1024, H=16, FF=2816, FE=3584, E=8, TS=1024)


class Ev:
    __slots__ = ("eng", "idx", "ins", "sem", "val", "slot", "kind", "waited")

    def __init__(self, eng, idx, ins):
        self.eng, self.idx, self.ins = eng, idx, ins
        self.sem = None
        self.val = None
        self.slot = None
        self.kind = None
        self.waited = False


class Eng:
    def __init__(self, name, e):
        self.name, self.e = name, e
        self.n = 0
        self.sem = None
        self.cnt = 0
        self.mat_idx = []
        self.mat_ev = []
        self.seen = {}
        self.last = None


class Slot:
    def __init__(self, name, t=None):
        self.name = name
        self.t = t
        self.w = None
        self.r = {}
        self.sems = {}
        self.lastd = {}


class Ctx:
    def __init__(self):
        self.nc = bass.Bass("TRN2", target_bir_lowering=False)
        nc = self.nc
        self.root = ExitStack()
        self.stacks = [self.root]
        self.engs = {
            "pe": Eng("pe", nc.tensor),
            "act": Eng("act", nc.scalar),
            "dve": Eng("dve", nc.vector),
            "pool": Eng("pool", nc.gpsimd),
            "sp": Eng("sp", nc.sync),
        }
        self.nsem = 0
        self.nname = 0
        self.slots = []
        self.phase_slots = [[]]
        self.sempool = {"sp": [], "pool": [], "act": []}
        self.ninstr = 0

    def new_sem(self, name):
        self.nsem += 1
        return self.root.enter_context(self.nc.semaphore(f"{name}_{self.nsem}"))

    def _reg(self, s):
        self.slots.append(s)
        self.phase_slots[-1].append(s)
        return s

    def sbuf(self, name, shape, dt):
        self.nname += 1
        t = self.stacks[-1].enter_context(self.nc.sbuf_tensor(f"{name}_{self.nname}", list(shape), dt))
        return self._reg(Slot(name, t))

    def psum(self, name, shape, dt=F32):
        self.nname += 1
        t = self.stacks[-1].enter_context(self.nc.psum_tensor(f"{name}_{self.nname}", list(shape), dt))
        return self._reg(Slot(name, t))

    def vslot(self, name):
        return self._reg(Slot(name, None))

    def dram(self, name, shape, dt, kind="Internal"):
        return self.nc.dram_tensor(name, list(shape), dt, kind=kind)

    def begin(self):
        self.stacks.append(ExitStack())
        self.phase_slots.append([])

    def end(self):
        self.barrier()
        for s in self.phase_slots.pop():
            for (d_, q_), sc in s.sems.items():
                self.sempool[q_].append(sc)
            self.slots.remove(s)
        self.stacks.pop().close()

    def _getsem(self, q):
        if self.sempool[q]:
            return self.sempool[q].pop(0)
        return [self.new_sem("d" + q), 0]

    def _materialize(self, ev):
        if ev.val is not None:
            return
        eng = ev.eng
        i = bisect.bisect_left(eng.mat_idx, ev.idx)
        if i < len(eng.mat_idx):
            o = eng.mat_ev[i]
            ev.sem, ev.val = o.sem, o.val
            return
        if eng.sem is None or eng.cnt >= SEM_ROT:
            eng.sem = self.new_sem("p" + eng.name)
            eng.cnt = 0
        eng.cnt += 1
        ev.ins.then_inc(eng.sem, 1)
        ev.sem, ev.val = eng.sem, eng.cnt
        eng.mat_idx.append(ev.idx)
        eng.mat_ev.append(ev)

    def _wait(self, eng, ev):
        if ev.kind == "dma":
            ev.waited = True
        else:
            self._materialize(ev)
        k = id(ev.sem)
        if eng.seen.get(k, 0) >= ev.val:
            return
        eng.seen[k] = ev.val
        eng.e.wait_ge(ev.sem, ev.val)

    def _deps(self, eng, reads, writes, is_dma):
        deps = []
        for s in reads:
            if s.w is not None:
                if is_dma or s.w.kind == "dma" or s.w.eng is not eng or eng.name != "pe":
                    deps.append(s.w)
        for s in writes:
            if s.w is not None and (is_dma or s.w.kind == "dma" or s.w.eng is not eng or eng.name != "pe"):
                deps.append(s.w)
            for r in s.r.values():
                if is_dma or r.kind == "dma" or r.eng is not eng or eng.name != "pe":
                    deps.append(r)
        return deps

    def op(self, engname, fn, reads=(), writes=()):
        eng = self.engs[engname]
        for d in self._deps(eng, reads, writes, False):
            self._wait(eng, d)
        ins = fn(eng.e)
        self.ninstr += 1
        ev = Ev(eng, eng.n, ins)
        eng.n += 1
        eng.last = ev
        for s in reads:
            s.r[engname] = ev
        for s in writes:
            s.w = ev
            s.r = {}
        return ev

    def dma(self, qname, pairs, reads=(), writes=(), **kw):
        eng = self.engs[qname]
        for d in self._deps(eng, reads, writes, True):
            self._wait(eng, d)
        s = writes[0] if writes else reads[0]
        key = ("w" if writes else "r", qname)
        if key not in s.sems:
            s.sems[key] = self._getsem(qname)
        sc = s.sems[key]
        prev = s.lastd.get(key)
        sc[1] += 16 * len(pairs)
        if prev is not None and not prev.waited:
            prev.val = sc[1]
        ins = None
        for (o, i) in pairs:
            ins = eng.e.dma_start(out=o, in_=i, **kw).then_inc(sc[0], 16)
            self.ninstr += 1
        ev = Ev(eng, -1, ins)
        ev.kind = "dma"
        ev.sem, ev.val, ev.slot = sc[0], sc[1], s
        s.lastd[key] = ev
        for x in reads:
            x.r["dma"] = ev
        for x in writes:
            x.w = ev
            x.r = {}
        return ev

    def barrier(self):
        evs = [e.last for e in self.engs.values() if e.last is not None]
        dm = []
        for s in self.slots:
            dm.extend(s.sems.values())
        for ev in evs:
            self._materialize(ev)
        for e in self.engs.values():
            for ev in evs:
                if ev.eng is e:
                    continue
                k = id(ev.sem)
                if e.seen.get(k, 0) < ev.val:
                    e.seen[k] = ev.val
                    e.e.wait_ge(ev.sem, ev.val)
            for (sem, val) in dm:
                k = id(sem)
                if val > 0 and e.seen.get(k, 0) < val:
                    e.seen[k] = val
                    e.e.wait_ge(sem, val)
        for s in self.slots:
            s.w = None
            s.r = {}
            for ev in s.lastd.values():
                ev.waited = True


class Rot:
    def __init__(self, items):
        self.items = items
        self.i = 0

    def next(self):
        x = self.items[self.i % len(self.items)]
        self.i += 1
        return x


def rel_bucket_table(nu):
    import jax
    import jax.numpy as jnp
    with jax.default_device(jax.devices("cpu")[0]):
        dist = jnp.arange(nu, dtype=jnp.int32) - 512
        distc = jnp.maximum(dist, 0)
        max_exact = 16
        d = jnp.maximum(distc, 1).astype(jnp.float32)
        large = max_exact + (jnp.log(d / max_exact) / math.log(1024 / max_exact) * (32 - max_exact)).astype(jnp.int32)
        large = jnp.minimum(large, 31)
        b = np.asarray(jnp.where(distc < max_exact, distc, large))
        dist = np.asarray(dist)
    oh = np.zeros((33, nu), np.float32)
    for u in range(nu):
        if dist[u] < 0:
            oh[32, u] = 1.0
        else:
            oh[b[u], u] = 1.0
    return oh


def make_consts(cfg):
    S, H = cfg["S"], cfg["H"]
    NU = max(S + 512, 2048)
    NB = S // 256
    cs = {}
    cs["identb"] = np.eye(128).astype(ml_dtypes.bfloat16)
    cs["identf"] = np.eye(128, dtype=np.float32)
    cs["oh"] = rel_bucket_table(NU)
    cs["jex"] = np.ascontiguousarray(np.eye(128, dtype=np.float32)[::-1])
    es = np.zeros((16, 16 * 128), np.float32)
    for n in range(16):
        es[n, n * 128:(n + 1) * 128] = 1.0
    cs["esel"] = es.astype(ml_dtypes.bfloat16)
    e2 = np.zeros((16, S), np.float32)
    for n in range(min(16, S // 256)):
        e2[n, n * 256:(n + 1) * 256] = 1.0
    cs["esel2"] = e2.astype(ml_dtypes.bfloat16)
    j = np.arange(128)[:, None]
    k = np.arange(128)[None, :]
    cs["ustrict"] = (j > k).astype(np.float32).astype(ml_dtypes.bfloat16)
    m = np.arange(512 + 384)[None, :]
    kk = np.arange(128)[:, None]
    cs["cmask"] = ((m - 384 - kk) > 0).astype(np.float32).astype(ml_dtypes.bfloat16)
    return cs


def build(cfg):
    S, D, H, FF, FE, E, TS = (cfg[k] for k in ("S", "D", "H", "FF", "FE", "E", "TS"))
    KC = D // 128
    NT = S // 128
    NG = S // 512
    NB = S // 256
    NBP = max(NB, 8)
    NU = max(S + 512, 2048)
    WB = 1856
    DMAXNEAR = 896
    c = Ctx()
    nc = c.nc
    di = lambda n, sh, dt=F32: c.dram(n, sh, dt, kind="ExternalInput")
    x_in = di("x", [S, D])
    c_in = di("c", [1, D])
    rel_bias = di("rel_bias", [32, H])
    mod_w = di("mod_w", [4, D, 6 * D])
    mod_b = di("mod_b", [1, 4 * 6 * D])
    norm_mix_g = di("norm_mix_g", [4, D])
    norm_ffn_g = di("norm_ffn_g", [4, D])
    a_wqkv = di("a_wqkv", [2, D, 3 * D])
    a_wo = di("a_wo", [2, D, D])
    kv_norm_g = di("kv_norm_g", [1, D])
    kv_mod_w = di("kv_mod_w", [D, 2 * D])
    kv_mod_b = di("kv_mod_b", [1, 2 * D])
    b_wkv = di("b_wkv", [D, 2 * D])
    b_wq = di("b_wq", [2, D, D])
    b_wo = di("b_wo", [2, D, D])
    ffn_w13 = di("ffn_w13", [2, D, 2 * FF])
    ffn_w2 = di("ffn_w2", [2, FF, D])
    router_w = di("router_w", [2, D, E])
    moe_w13 = di("moe_w13", [2, E, D, 2 * FE])
    moe_w2 = di("moe_w2", [2, E, FE, D])
    final_norm_g = di("final_norm_g", [1, D])
    identb_d = di("identb", [128, 128], BF16)
    identf_d = di("identf", [128, 128])
    oh_d = di("oh", [33, NU])
    jex_d = di("jex", [128, 128])
    esel_d = di("esel", [16, 16 * 128], BF16)
    esel2_d = di("esel2", [16, S], BF16)
    ustrict_d = di("ustrict", [128, 128], BF16)
    cmask_d = di("cmask", [128, 896], BF16)
    out_d = c.dram("out", [S, D], F32, kind="ExternalOutput")

    xres = c.dram("xres", [S, D], F32)
    hT = c.dram("hT", [D, S], BF16)
    qT = c.dram("qT", [D, S], BF16)
    kT = c.dram("kT", [D, S], BF16)
    v65 = c.dram("v65", [S, H * 65], BF16)
    attnT = c.dram("attnT", [D, S], BF16)
    NMOD = 4 * 6 * D + 2 * D
    NVR = NMOD // 128 + 4 * KC + 4 * KC + KC
    vecscr = c.dram("vecscr", [NVR, 128], F32)
    ftab = c.dram("ftab", [H, NU], F32)
    biasT = c.dram("biasT", [H, 128, WB], BF16)
    comb_tm = c.dram("comb_tm", [S, E], F32)

    identb = c.sbuf("identb", [128, 128], BF16)
    identf = c.sbuf("identf", [128, 128], F32)
    modT = c.sbuf("modT", [128, NVR], F32)
    geff = c.sbuf("geff", [128, 9 * KC], F32)
    onesf = c.sbuf("onesf", [128, 128], F32)
    onesb = c.sbuf("onesb", [128, 128], BF16)
    c.dma("sp", [(identb.t[:, :], identb_d[:, :])], writes=[identb])
    c.dma("sp", [(identf.t[:, :], identf_d[:, :])], writes=[identf])
    c.op("pool", lambda e: e.memset(onesf.t[:, :], 1.0), writes=[onesf])
    c.op("pool", lambda e: e.memset(onesb.t[:, :], 1.0), writes=[onesb])

    def col_mod(l, j):
        return l * 6 * KC + j * KC
    COL_KV = 4 * 6 * KC
    COL_GMIX = NMOD // 128
    COL_GFFN = COL_GMIX + 4 * KC
    COL_GKV = COL_GFFN + 4 * KC

    c.begin()
    crow = c.sbuf("crow", [KC, 128], F32)
    cT = c.sbuf("cT", [128, KC], F32)
    scT = c.sbuf("scT", [128, KC], F32)
    pA = c.psum("pA", [128, 512], F32)
    pB = Rot([c.psum(f"pB{i}", [128, 512], F32) for i in range(3)])
    mw = Rot([c.sbuf(f"mw{i}", [128, KC, 512], F32) for i in range(4)])
    mbr = Rot([c.sbuf(f"mbr{i}", [1, 512], F32) for i in range(4)])
    mst = Rot([c.sbuf("mst0", [1, 512], F32), c.sbuf("mst1", [1, 512], F32)])
    vs = c.vslot("vecscr")
    c.dma("sp", [(crow.t[:, :], c_in[0:1, :].rearrange("o (k p) -> (o k) p", p=128))], writes=[crow])
    c.dma("pool", [(vecscr[COL_GMIX:COL_GMIX + 4 * KC, :], norm_mix_g[:, :].rearrange("l (k p) -> (l k) p", p=128)),
                   (vecscr[COL_GFFN:COL_GFFN + 4 * KC, :], norm_ffn_g[:, :].rearrange("l (k p) -> (l k) p", p=128)),
                   (vecscr[COL_GKV:COL_GKV + KC, :], kv_norm_g[0:1, :].rearrange("o (k p) -> (o k) p", p=128))],
          writes=[vs])
    c.op("pe", lambda e: e.transpose(out=pA.t[:, 0:KC], in_=crow.t[:, :], identity=identf.t[0:KC, 0:KC]), reads=[crow, identf], writes=[pA])
    c.op("dve", lambda e: e.tensor_copy(cT.t[:, :], pA.t[:, 0:KC]), reads=[pA], writes=[cT])
    c.op("act", lambda e: e.activation(out=scT.t[:, :], in_=cT.t[:, :], func=AF.Silu), reads=[cT], writes=[scT])
    blocks = [(mod_w[l], cb, l * 6 * D + cb * 512, mod_b, l * 6 * D + cb * 512) for l in range(4) for cb in range(6 * D // 512)]
    blocks += [(kv_mod_w, cb, 4 * 6 * D + cb * 512, kv_mod_b, cb * 512) for cb in range(2 * D // 512)]
    for bi, (wsrc, cb, off, bsrc, boff) in enumerate(blocks):
        qn = "sp" if bi % 2 == 0 else "act"
        w = mw.next()
        mb_ = mbr.next()
        c.dma(qn, [(w.t[:, :, :], wsrc[:, cb * 512:(cb + 1) * 512].rearrange("(k p) f -> p k f", p=128))], writes=[w])
        c.dma(qn, [(mb_.t[0:1, :], bsrc[0:1, boff:boff + 512])], writes=[mb_])
        ps = pB.next()
        for kc in range(KC):
            c.op("pe", lambda e, kc=kc, w=w, ps=ps: e.matmul(ps.t[0:1, :], lhsT=scT.t[:, kc:kc + 1], rhs=w.t[:, kc, :],
                                                            start=(kc == 0), stop=(kc == KC - 1)), reads=[scT, w], writes=[ps])
        st = mst.next()
        c.op("dve", lambda e, st=st, ps=ps, mb_=mb_: e.tensor_tensor(out=st.t[0:1, :], in0=ps.t[0:1, :], in1=mb_.t[0:1, :], op=ALU.add),
             reads=[ps, mb_], writes=[st])
        r0 = off // 128
        c.dma("sp", [(vecscr[r0:r0 + 4, :].rearrange("(o r) p -> o (r p)", o=1), st.t[0:1, :])], reads=[st], writes=[vs])
    c.barrier()
    r = 0
    vrow = Rot([c.sbuf("vrow0", [128, 128], F32), c.sbuf("vrow1", [128, 128], F32)])
    while r < NVR:
        n = min(128, NVR - r)
        vr = vrow.next()
        c.dma("sp", [(vr.t[0:n, :], vecscr[r:r + n, :])], writes=[vr])
        c.op("pe", lambda e, vr=vr, n=n: e.transpose(out=pA.t[:, 0:n], in_=vr.t[0:n, :], identity=identf.t[0:n, 0:n]), reads=[vr, identf], writes=[pA])
        c.op("dve", lambda e, r=r, n=n: e.tensor_copy(modT.t[:, r:r + n], pA.t[:, 0:n]), reads=[pA], writes=[modT])
        r += n
    for l in range(4):
        c.op("dve", lambda e, l=l: e.scalar_tensor_tensor(out=geff.t[:, l * KC:(l + 1) * KC], in0=modT.t[:, col_mod(l, 1):col_mod(l, 1) + KC], scalar=1.0,
                                                          in1=modT.t[:, COL_GMIX + l * KC:COL_GMIX + (l + 1) * KC], op0=ALU.add, op1=ALU.mult), reads=[modT], writes=[geff])
        c.op("dve", lambda e, l=l: e.scalar_tensor_tensor(out=geff.t[:, (4 + l) * KC:(5 + l) * KC], in0=modT.t[:, col_mod(l, 4):col_mod(l, 4) + KC], scalar=1.0,
                                                          in1=modT.t[:, COL_GFFN + l * KC:COL_GFFN + (l + 1) * KC], op0=ALU.add, op1=ALU.mult), reads=[modT], writes=[geff])
    c.op("dve", lambda e: e.scalar_tensor_tensor(out=geff.t[:, 8 * KC:9 * KC], in0=modT.t[:, COL_KV + KC:COL_KV + 2 * KC], scalar=1.0,
                                                 in1=modT.t[:, COL_GKV:COL_GKV + KC], op0=ALU.add, op1=ALU.mult), reads=[modT], writes=[geff])
    c.end()

    c.begin()
    rbA = c.sbuf("rbA", [33, H], F32)
    ohs = c.sbuf("ohs", [33, NU], F32)
    fsb = c.sbuf("fsb", [H, NU], F32)
    jex = c.sbuf("jex", [128, 128], F32)
    pF = Rot([c.psum("pF0", [128, 512], F32), c.psum("pF1", [128, 512], F32)])
    c.op("pool", lambda e: e.memset(rbA.t[32:33, :], -BIG), writes=[rbA])
    c.dma("sp", [(rbA.t[0:32, :], rel_bias[:, :])], writes=[rbA])
    c.dma("sp", [(ohs.t[:, :], oh_d[:, :])], writes=[ohs])
    c.dma("sp", [(jex.t[:, :], jex_d[:, :])], writes=[jex])
    for ch in range(NU // 512):
        ps = pF.next()
        c.op("pe", lambda e, ps=ps, ch=ch: e.matmul(ps.t[0:H, :], lhsT=rbA.t[:, 0:H], rhs=ohs.t[:, ch * 512:(ch + 1) * 512], start=True, stop=True),
             reads=[rbA, ohs], writes=[ps])
        c.op("act", lambda e, ps=ps, ch=ch: e.activation(out=fsb.t[:, ch * 512:(ch + 1) * 512], in_=ps.t[0:H, :], func=AF.Copy), reads=[ps], writes=[fsb])
    c.dma("sp", [(ftab[:, :], fsb.t[:, :])], reads=[fsb])
    c.barrier()
    t2 = Rot([c.sbuf("t2a", [128, WB], F32), c.sbuf("t2b", [128, WB], F32)])
    bst = Rot([c.sbuf("bsta", [128, WB], BF16), c.sbuf("bstb", [128, WB], BF16)])
    for h in range(H):
        t = t2.next()
        src = bass.AP(tensor=ftab, offset=h * NU + 1, ap=[[1, 128], [1, WB]])
        c.dma("sp", [(t.t[:, :], src)], writes=[t])
        bs = bst.next()
        for ch in range(0, WB, 512):
            w_ = min(512, WB - ch)
            ps = pF.next()
            c.op("pe", lambda e, ps=ps, t=t, ch=ch, w_=w_: e.matmul(ps.t[:, 0:w_], lhsT=jex.t[:, :], rhs=t.t[:, ch:ch + w_], start=True, stop=True),
                 reads=[jex, t], writes=[ps])
            c.op("act", lambda e, ps=ps, bs=bs, ch=ch, w_=w_: e.activation(out=bs.t[:, ch:ch + w_], in_=ps.t[:, 0:w_], func=AF.Copy), reads=[ps], writes=[bs])
        c.dma("sp", [(biasT[h], bs.t[:, :])], reads=[bs])
    c.end()

    def norm_phase(src, gcol0, shcol0, dst):
        c.begin()
        xt_r = Rot([c.sbuf("nx0", [128, 4, D], F32), c.sbuf("nx1", [128, 4, D], F32)])
        junk = c.sbuf("njunk", [128, D], BF16)
        ss_r = Rot([c.sbuf("nss0", [128, 4], F32), c.sbuf("nss1", [128, 4], F32)])
        sq_r = Rot([c.sbuf("nsq0", [128, 4], F32), c.sbuf("nsq1", [128, 4], F32)])
        rs_r = Rot([c.sbuf("nrs0", [128, 4], F32), c.sbuf("nrs1", [128, 4], F32)])
        xn_r = Rot([c.sbuf("nxn0", [128, 4, D], BF16), c.sbuf("nxn1", [128, 4, D], BF16)])
        ht_r = Rot([c.sbuf("nht0", [128, KC, 512], BF16), c.sbuf("nht1", [128, KC, 512], BF16)])
        pt_r = Rot([c.psum("npt0", [128, 512], BF16), c.psum("npt1", [128, 512], BF16), c.psum("npt2", [128, 512], BF16)])
        epsc = c.sbuf("epsc", [128, 1], F32)
        c.op("pool", lambda e: e.memset(epsc.t[:, :], EPS), writes=[epsc])
        for g in range(NG):
            xt = xt_r.next()
            c.dma("sp", [(xt.t[:, :, :], src[g * 512:(g + 1) * 512, :].rearrange("(t p) d -> p t d", p=128))], writes=[xt])
            ss, sq, rs, xn, ht = ss_r.next(), sq_r.next(), rs_r.next(), xn_r.next(), ht_r.next()
            for t in range(4):
                c.op("act", lambda e, t=t, xt=xt, ss=ss: e.activation(out=junk.t[:, :], in_=xt.t[:, t, :], func=AF.Square, accum_out=ss.t[:, t:t + 1]),
                     reads=[xt], writes=[junk, ss])
            c.op("act", lambda e, ss=ss, sq=sq: e.activation(out=sq.t[:, :], in_=ss.t[:, :], func=AF.Sqrt, scale=1.0 / D, bias=epsc.t[:, 0:1]), reads=[ss, epsc], writes=[sq])
            c.op("dve", lambda e, sq=sq, rs=rs: e.reciprocal(out=rs.t[:, :], in_=sq.t[:, :]), reads=[sq], writes=[rs])
            for t in range(4):
                c.op("dve", lambda e, t=t, xt=xt, xn=xn, rs=rs: e.tensor_scalar(out=xn.t[:, t, :], in0=xt.t[:, t, :], scalar1=rs.t[:, t:t + 1], scalar2=None, op0=ALU.mult),
                     reads=[xt, rs], writes=[xn])
            for kc in range(KC):
                pt = pt_r.next()
                for t in range(4):
                    c.op("pe", lambda e, t=t, kc=kc, pt=pt, xn=xn: e.transpose(out=pt.t[:, t * 128:(t + 1) * 128], in_=xn.t[:, t, kc * 128:(kc + 1) * 128], identity=identb.t[:, :]),
                         reads=[xn, identb], writes=[pt])
                c.op("dve", lambda e, kc=kc, pt=pt, ht=ht: e.tensor_scalar(out=ht.t[:, kc, :], in0=pt.t[:, :], scalar1=geff.t[:, gcol0 + kc:gcol0 + kc + 1],
                                                                           scalar2=modT.t[:, shcol0 + kc:shcol0 + kc + 1], op0=ALU.mult, op1=ALU.add),
                     reads=[pt, geff, modT], writes=[ht])
            c.dma("pool", [(dst[:, g * 512:(g + 1) * 512].rearrange("(k p) s -> p k s", p=128), ht.t[:, :, :])], reads=[ht])
        c.end()

    def load_w_bf16(dst_slot, dst_ap_fn, src2d, ncols, piece=1024):
        pairs = []
        for c0 in range(0, ncols, piece):
            w_ = min(piece, ncols - c0)
            pairs.append((dst_ap_fn(c0, w_), src2d[:, c0:c0 + w_].rearrange("(k p) f -> p k f", p=128)))
        c.dma("pool", pairs, writes=[dst_slot])

    def proj_phase(src_hT, w2d, ncols_fm, ncols_tm, fm_dsts, tm_dst65, q_scale_chunks):
        c.begin()
        ncols = ncols_fm + ncols_tm
        wsb = c.sbuf("pw", [128, KC, ncols], BF16)
        load_w_bf16(wsb, lambda c0, w_: wsb.t[:, :, c0:c0 + w_], w2d, ncols)
        ht_r = Rot([c.sbuf("pht0", [128, KC, 512], BF16), c.sbuf("pht1", [128, KC, 512], BF16)])
        NJ = ncols_fm // 128
        st_r = Rot([c.sbuf("pst0", [128, max(NJ, 1), 512], BF16), c.sbuf("pst1", [128, max(NJ, 1), 512], BF16)])
        ps_r = Rot([c.psum(f"pps{i}", [128, 512], F32) for i in range(4)])
        if ncols_tm:
            vst_r = Rot([c.sbuf("pvs0", [128, 4, H, 65], BF16), c.sbuf("pvs1", [128, 4, H, 65], BF16)])
            for v in vst_r.items:
                c.op("pool", lambda e, v=v: e.memset(v.t[:, :, :, :], 1.0), writes=[v])
        for g in range(NG):
            ht = ht_r.next()
            c.dma("sp", [(ht.t[:, :, :], src_hT[:, g * 512:(g + 1) * 512].rearrange("(k p) s -> p k s", p=128))], writes=[ht])
            if NJ:
                st = st_r.next()
                for j in range(NJ):
                    ps = ps_r.next()
                    for kc in range(KC):
                        c.op("pe", lambda e, ps=ps, j=j, kc=kc, ht=ht: e.matmul(ps.t[:, :], lhsT=wsb.t[:, kc, j * 128:(j + 1) * 128], rhs=ht.t[:, kc, :],
                                                                               start=(kc == 0), stop=(kc == KC - 1)), reads=[wsb, ht], writes=[ps])
                    if j in q_scale_chunks:
                        c.op("act", lambda e, ps=ps, st=st, j=j: e.activation(out=st.t[:, j, :], in_=ps.t[:, :], func=AF.Copy, scale=0.125), reads=[ps], writes=[st])
                    else:
                        c.op("dve", lambda e, ps=ps, st=st, j=j: e.tensor_copy(st.t[:, j, :], ps.t[:, :]), reads=[ps], writes=[st])
                pairs = []
                for j in range(NJ):
                    dt_, r0 = fm_dsts[j]
                    pairs.append((dt_[r0:r0 + 128, g * 512:(g + 1) * 512], st.t[:, j, :]))
                c.dma("pool", pairs, reads=[st])
            if ncols_tm:
                vst = vst_r.next()
                for tt in range(4):
                    for half in range(ncols_tm // 512):
                        ps = ps_r.next()
                        for kc in range(KC):
                            c.op("pe", lambda e, ps=ps, kc=kc, ht=ht, tt=tt, half=half: e.matmul(ps.t[:, :], lhsT=ht.t[:, kc, tt * 128:(tt + 1) * 128],
                                                                                                rhs=wsb.t[:, kc, ncols_fm + half * 512:ncols_fm + (half + 1) * 512],
                                                                                                start=(kc == 0), stop=(kc == KC - 1)), reads=[wsb, ht], writes=[ps])
                        c.op("act", lambda e, ps=ps, vst=vst, tt=tt, half=half: e.activation(out=vst.t[:, tt, half * 8:(half + 1) * 8, 0:64],
                                                                                             in_=ps.t[:, :].rearrange("p (h d) -> p h d", d=64), func=AF.Copy),
                             reads=[ps], writes=[vst])
                c.dma("pool", [(tm_dst65[g * 512:(g + 1) * 512, :].rearrange("(t p) f -> p t f", p=128), vst.t[:, :, :, :].rearrange("p t h d -> p t (h d)"))], reads=[vst])
        c.end()

    def wo_phase(w2d, gcol_row0, xsrc=None):
        xsrc = xres if xsrc is None else xsrc
        c.begin()
        wsb = c.sbuf("ow", [128, KC, D], BF16)
        load_w_bf16(wsb, lambda c0, w_: wsb.t[:, :, c0:c0 + w_], w2d, D)
        gb = c.sbuf("ogb", [128, D], F32)
        c.dma("sp", [(gb.t[:, :], vecscr[gcol_row0:gcol_row0 + KC, :].rearrange("(o k) p -> o (k p)", o=1).partition_broadcast(128))], writes=[gb])
        at_r = Rot([c.sbuf("oat0", [128, KC, 512], BF16), c.sbuf("oat1", [128, KC, 512], BF16)])
        xt_r = Rot([c.sbuf("ox0", [128, 4, D], F32), c.sbuf("ox1", [128, 4, D], F32)])
        tmp_r = Rot([c.sbuf("otm0", [128, 512], F32), c.sbuf("otm1", [128, 512], F32)])
        ps_r = Rot([c.psum(f"ops{i}", [128, 512], F32) for i in range(4)])
        for g in range(NG):
            at = at_r.next()
            xt = xt_r.next()
            c.dma("sp", [(at.t[:, :, :], attnT[:, g * 512:(g + 1) * 512].rearrange("(k p) s -> p k s", p=128))], writes=[at])
            c.dma("sp", [(xt.t[:, :, :], xsrc[g * 512:(g + 1) * 512, :].rearrange("(t p) d -> p t d", p=128))], writes=[xt])
            for tt in range(4):
                for half in range(D // 512):
                    ps = ps_r.next()
                    for kc in range(KC):
                        c.op("pe", lambda e, ps=ps, kc=kc, at=at, tt=tt, half=half: e.matmul(ps.t[:, :], lhsT=at.t[:, kc, tt * 128:(tt + 1) * 128],
                                                                                            rhs=wsb.t[:, kc, half * 512:(half + 1) * 512],
                                                                                            start=(kc == 0), stop=(kc == KC - 1)), reads=[wsb, at], writes=[ps])
                    tmp = tmp_r.next()
                    c.op("dve", lambda e, ps=ps, tmp=tmp, half=half: e.tensor_tensor(out=tmp.t[:, :], in0=ps.t[:, :], in1=gb.t[:, half * 512:(half + 1) * 512], op=ALU.mult),
                         reads=[ps, gb], writes=[tmp])
                    c.op("pool", lambda e, tmp=tmp, xt=xt, tt=tt, half=half: e.tensor_tensor(out=xt.t[:, tt, half * 512:(half + 1) * 512], in0=xt.t[:, tt, half * 512:(half + 1) * 512],
                                                                                           in1=tmp.t[:, :], op=ALU.add), reads=[tmp, xt], writes=[xt])
            c.dma("pool", [(xres[g * 512:(g + 1) * 512, :].rearrange("(t p) d -> p t d", p=128), xt.t[:, :, :])], reads=[xt])
        c.end()

    def bc_last(ap2d, n):
        a = [list(x) for x in ap2d.ap]
        return bass.AP(tensor=ap2d.tensor, offset=ap2d.offset, ap=a + [[0, n]])

    def moba_phase():
        c.begin()
        NGN = NT * NB
        vall = c.sbuf("mvall", [128, NT, H * 65], BF16)
        c.dma("sp", [(vall.t[:, :, :], v65[:, :].rearrange("(t p) f -> p t f", p=128))], writes=[vall])
        qp_r = Rot([c.sbuf("mq0", [128, S], BF16), c.sbuf("mq1", [128, S], BF16)])
        kp_r = Rot([c.sbuf("mk0", [128, S], BF16), c.sbuf("mk1", [128, S], BF16)])
        for t_ in qp_r.items + kp_r.items:
            c.op("pool", lambda e, t_=t_: e.memset(t_.t[:, :], 0.0), writes=[t_])
        for t_ in kp_r.items:
            c.dma("sp", [(t_.t[64:64 + NB, :], esel2_d[0:NB, :])], writes=[t_])
        bt_r = Rot([c.sbuf("mbt0", [128, WB], BF16), c.sbuf("mbt1", [128, WB], BF16)])
        km = c.sbuf("mkm", [128, NB], F32)
        kmb_r = Rot([c.sbuf("mkmb0", [128, NB], BF16), c.sbuf("mkmb1", [128, NB], BF16)])
        for t_ in kmb_r.items:
            c.op("pool", lambda e, t_=t_: e.memset(t_.t[:, :], 0.0), writes=[t_])
        VM = c.sbuf("mVM", [128, NT, NB], F32)
        NV = c.sbuf("mNV", [128, NT, NB], F32)
        TM = c.sbuf("mTM", [128, NT, NB], F32)
        gmA = c.sbuf("mgmA", [128, NGN], F32)
        gmB = c.sbuf("mgmB", [128, NGN], F32)
        gmC = c.sbuf("mgmC", [128, NGN], F32)
        tA = c.sbuf("mtA", [128, NGN], F32)
        mx = c.sbuf("mmx", [128, NT], F32)
        mvs = c.sbuf("mmvs", [128, NGN], F32)
        sb_r = Rot([c.sbuf(f"msb{i}", [128, 512], F32) for i in range(3)])
        a_r = Rot([c.sbuf(f"ma{i}", [128, 512], BF16) for i in range(4)])
        rsb_r = Rot([c.sbuf("mrs0", [128, 512], F32), c.sbuf("mrs1", [128, 512], F32)])
        for t_ in rsb_r.items:
            c.op("pool", lambda e, t_=t_: e.memset(t_.t[:, :], 0.0), writes=[t_])
        sel64 = c.sbuf("msel64", [128, 64], F32)
        c.op("pool", lambda e: e.memset(sel64.t[:, :], 0.0), writes=[sel64])
        c.op("pool", lambda e: e.memset(sel64.t[64:65, :], 1.0), writes=[sel64])
        rb_r = Rot([c.sbuf("mrb0", [64, 512], F32), c.sbuf("mrb1", [64, 512], F32)])
        ast_r = Rot([c.sbuf("mas0", [64, 512], BF16), c.sbuf("mas1", [64, 512], BF16)])
        pS = Rot([c.psum(f"mpS{i}", [128, 512], F32) for i in range(3)])
        pO = Rot([c.psum("mpO0", [128, 512], F32), c.psum("mpO1", [128, 512], F32)])
        pG = c.psum("mpG", [128, 512], F32)
        pX = c.psum("mpX", [128, 512], F32)
        pR = c.psum("mpR", [128, 512], F32)
        c.op("pool", lambda e: e.memset(VM.t[:, :, :], -BIG), writes=[VM])
        c.op("pool", lambda e: e.memset(NV.t[:, :, :], 0.0), writes=[NV])
        c.op("pool", lambda e: e.memset(TM.t[:, :, :], -BIG), writes=[TM])
        for i in range(NT):
            cur = i // 2
            if cur > 0:
                c.op("pool", lambda e, i=i, cur=cur: e.memset(VM.t[:, i, 0:cur], 0.0), writes=[VM])
                c.op("pool", lambda e, i=i, cur=cur: e.memset(NV.t[:, i, 0:cur], -BIG), writes=[NV])
            c.op("pool", lambda e, i=i, cur=cur: e.memset(TM.t[:, i, 0:cur + 1], 0.0), writes=[TM])
        v3 = lambda s_: s_.t[:, :].rearrange("p (i n) -> p i n", n=NB)
        state = {}

        def pre1(h):
            qp, kp, kmb = qp_r.next(), kp_r.next(), kmb_r.next()
            c.dma("sp", [(qp.t[0:64, :], qT[h * 64:h * 64 + 64, :])], writes=[qp])
            c.dma("sp", [(kp.t[0:64, :], kT[h * 64:h * 64 + 64, :])], writes=[kp])
            bt = bt_r.next()
            c.dma("sp", [(bt.t[:, :], biasT[h])], writes=[bt])
            return [qp, kp, kmb, bt]

        def pre2(h, st_):
            qp, kp, kmb, bt = st_
            c.op("dve", lambda e: e.tensor_reduce(out=km.t[0:64, :], in_=kp.t[0:64, :].rearrange("p (n b) -> p n b", b=256), axis=AX.X, op=ALU.add), reads=[kp], writes=[km])
            c.op("dve", lambda e: e.tensor_scalar(out=kmb.t[0:64, :], in0=km.t[0:64, :], scalar1=1.0 / 256, scalar2=None, op0=ALU.mult), reads=[km], writes=[kmb])
            for i in range(NT):
                c.op("pe", lambda e, i=i: e.matmul(pG.t[:, i * NB:(i + 1) * NB], lhsT=qp.t[:, i * 128:(i + 1) * 128], rhs=kmb.t[:, 0:NB], start=True, stop=True),
                     reads=[qp, kmb], writes=[pG])
            c.op("dve", lambda e: e.tensor_tensor(out=gmA.t[:, :], in0=pG.t[:, 0:NGN], in1=VM.t[:, :, :].rearrange("p i n -> p (i n)"), op=ALU.add), reads=[pG, VM], writes=[gmA])
            src = gmA
            for (dst,) in ((gmB,), (gmC,)):
                c.op("dve", lambda e, src=src: e.tensor_reduce(out=mx.t[:, :], in_=v3(src), axis=AX.X, op=ALU.max), reads=[src], writes=[mx])
                c.op("dve", lambda e, src=src: e.tensor_tensor(out=v3(tA), in0=v3(src), in1=bc_last(mx.t[:, :], NB), op=ALU.is_ge), reads=[src, mx], writes=[tA])
                c.op("dve", lambda e, src=src, dst=dst: e.scalar_tensor_tensor(out=dst.t[:, :], in0=tA.t[:, :], scalar=-BIG, in1=src.t[:, :], op0=ALU.mult, op1=ALU.add),
                     reads=[tA, src], writes=[dst])
                src = dst
            c.op("dve", lambda e: e.tensor_reduce(out=mx.t[:, :], in_=v3(gmC), axis=AX.X, op=ALU.max), reads=[gmC], writes=[mx])
            c.op("dve", lambda e: e.tensor_tensor(out=v3(tA), in0=v3(gmA), in1=bc_last(mx.t[:, :], NB), op=ALU.is_lt), reads=[gmA, mx], writes=[tA])
            c.op("dve", lambda e: e.tensor_tensor(out=tA.t[:, :], in0=tA.t[:, :], in1=NV.t[:, :, :].rearrange("p i n -> p (i n)"), op=ALU.mult), reads=[tA, NV], writes=[tA])
            c.op("dve", lambda e: e.tensor_tensor(out=mvs.t[:, :], in0=tA.t[:, :], in1=TM.t[:, :, :].rearrange("p i n -> p (i n)"), op=ALU.add), reads=[tA, TM], writes=[mvs])

        def pre3(h, st_):
            qp, kp, kmb, bt = st_
            for i0 in range(0, NT, 4):
                for i in range(i0, i0 + 4):
                    c.op("pe", lambda e, i=i, i0=i0: e.transpose(out=pX.t[0:NB, (i - i0) * 128:(i - i0 + 1) * 128], in_=mvs.t[:, i * NB:(i + 1) * NB], identity=identf.t[:, :]),
                         reads=[mvs, identf], writes=[pX])
                c.op("act", lambda e, i0=i0: e.activation(out=qp.t[64:64 + NB, i0 * 128:(i0 + 4) * 128], in_=pX.t[0:NB, 0:512], func=AF.Copy), reads=[pX], writes=[qp])
            return (qp, kp, bt)

        def main(h, ctxh):
            qp, kp, bt = ctxh
            tiles = [(Q, kt) for Q in range(NG) for kt in range(4 * Q + 4)]
            n_ = len(tiles)
            T = {}
            pos = {}
            fin = {}
            nst = None
            res = None
            for step in range(n_ + 3 + 6):
                if h + 1 < H:
                    if step == n_ // 8:
                        nst = pre1(h + 1)
                    if step == n_ // 3:
                        pre2(h + 1, nst)
                    if step == (2 * n_) // 3:
                        res = pre3(h + 1, nst)
                for f_ in fin.pop(step, []):
                    f_()
                j = step - 2
                if 0 <= j < n_:
                    Q, kt = tiles[j]
                    nkt = 4 * Q + 4
                    a = T.pop(j)["a"]
                    if kt == 0:
                        pos[Q] = pO.next()
                    po = pos[Q]
                    c.op("pe", lambda e, po=po, a=a, kt=kt, nkt=nkt: e.matmul(po.t[0:65, :], lhsT=vall.t[:, kt, h * 65:(h + 1) * 65], rhs=a.t[:, :],
                                                                            start=(kt == 0), stop=(kt == nkt - 1)), reads=[vall, a], writes=[po])
                    if kt == nkt - 1:
                        rsb = rsb_r.next()
                        rb = rb_r.next()
                        ast = ast_r.next()

                        def f1(po=po, rsb=rsb):
                            c.op("dve", lambda e: e.reciprocal(out=rsb.t[64:65, :], in_=po.t[64:65, :]), reads=[po], writes=[rsb])

                        def f2(rsb=rsb):
                            c.op("pe", lambda e: e.matmul(pR.t[0:64, :], lhsT=sel64.t[:, :], rhs=rsb.t[:, :], start=True, stop=True), reads=[sel64, rsb], writes=[pR])

                        def f3(po=po, rb=rb, ast=ast, Q=Q):
                            c.op("dve", lambda e: e.tensor_copy(rb.t[:, :], pR.t[0:64, :]), reads=[pR], writes=[rb])
                            c.op("dve", lambda e: e.tensor_tensor(out=ast.t[:, :], in0=po.t[0:64, :], in1=rb.t[:, :], op=ALU.mult), reads=[po, rb], writes=[ast])
                            c.dma("pool", [(attnT[h * 64:(h + 1) * 64, Q * 512:(Q + 1) * 512], ast.t[:, :])], reads=[ast])
                        fin.setdefault(step + 2, []).append(f1)
                        fin.setdefault(step + 4, []).append(f2)
                        fin.setdefault(step + 6, []).append(f3)
                j = step - 1
                if 0 <= j < n_:
                    a = a_r.next()
                    ps = T[j].pop("ps")
                    c.op("act", lambda e, ps=ps, a=a: e.activation(out=a.t[:, :], in_=ps.t[:, :], func=AF.Exp), reads=[ps], writes=[a])
                    T[j]["a"] = a
                j = step
                if j < n_:
                    Q, kt = tiles[j]
                    delta = 512 * Q - 128 * kt
                    ps = pS.next()
                    c.op("pe", lambda e, ps=ps, kt=kt, Q=Q: e.matmul(ps.t[:, :], lhsT=kp.t[:, kt * 128:(kt + 1) * 128], rhs=qp.t[:, Q * 512:(Q + 1) * 512],
                                                                   start=True, stop=False), reads=[kp, qp], writes=[ps])
                    o = (delta + 384) if delta <= DMAXNEAR else 1330
                    c.op("pe", lambda e, ps=ps, o=o: e.matmul(ps.t[:, :], lhsT=identb.t[:, :], rhs=bt.t[:, o:o + 512], start=False, stop=True), reads=[identb, bt], writes=[ps])
                    T[j] = {"ps": ps}
            for k_ in sorted(fin):
                for f_ in fin[k_]:
                    f_()
            return res

        st0 = pre1(0)
        pre2(0, st0)
        nxt = pre3(0, st0)
        for h in range(H):
            nxt = main(h, nxt)
        c.end()

    def sb_phase():
        c.begin()
        vall = c.sbuf("svall", [128, NT, H * 65], BF16)
        c.dma("sp", [(vall.t[:, :, :], v65[:, :].rearrange("(t p) f -> p t f", p=128))], writes=[vall])
        ustr = c.sbuf("sustr", [128, 128], BF16)
        cm = c.sbuf("scm", [128, 896], BF16)
        c.dma("sp", [(ustr.t[:, :], ustrict_d[:, :])], writes=[ustr])
        c.dma("sp", [(cm.t[:, :], cmask_d[:, :])], writes=[cm])
        qp_r = Rot([c.sbuf("sq0", [128, S], BF16), c.sbuf("sq1", [128, S], BF16)])
        kp_r = Rot([c.sbuf("sk0", [128, S], BF16), c.sbuf("sk1", [128, S], BF16)])
        for t_ in qp_r.items + kp_r.items:
            c.op("pool", lambda e, t_=t_: e.memset(t_.t[:, :], 0.0), writes=[t_])
        e_r = Rot([c.sbuf(f"se{i}", [128, 512], F32) for i in range(2)])
        lp_r = Rot([c.sbuf(f"slp{i}", [128, 512], BF16) for i in range(5)])
        ln_r = Rot([c.sbuf(f"sln{i}", [128, 512], BF16) for i in range(7)])
        a_r = Rot([c.sbuf(f"sa{i}", [128, 512], BF16) for i in range(4)])
        tot_r = Rot([c.sbuf(f"stot{i}", [128, 512], BF16) for i in range(4)])
        ast_r = Rot([c.sbuf("sas0", [64, 512], BF16), c.sbuf("sas1", [64, 512], BF16)])
        pZ = Rot([c.psum(f"spZ{i}", [128, 512], F32) for i in range(3)])
        pT = Rot([c.psum("spT0", [128, 512], F32), c.psum("spT1", [128, 512], F32)])
        pTOT = Rot([c.psum("spTOT0", [128, 512], F32)])
        pO = Rot([c.psum("spO0", [128, 512], F32), c.psum("spO1", [128, 512], F32)])
        def sload(h_):
            q_, k_ = qp_r.next(), kp_r.next()
            c.dma("sp", [(q_.t[0:64, :], qT[h_ * 64:h_ * 64 + 64, :])], writes=[q_])
            c.dma("sp", [(k_.t[0:64, :], kT[h_ * 64:h_ * 64 + 64, :])], writes=[k_])
            return q_, k_
        nxt_qk = sload(0)
        for h in range(H):
            qp, kp = nxt_qk
            tiles = [(Q, idx, 4 * Q + 3 - idx) for Q in range(NG) for idx in range(4 * Q + 4)]
            T = {}
            qstate = {}
            NST = 6
            fin = {}
            for step in range(len(tiles) + NST + 2):
                if step == len(tiles) // 2 and h + 1 < H:
                    nxt_qk = sload(h + 1)
                for f_ in fin.pop(step, []):
                    f_()
                j = step - 5
                if 0 <= j < len(tiles):
                    Q, idx, kt = tiles[j]
                    nkt = 4 * Q + 4
                    a = T[j].pop("a")
                    po = qstate[Q]["po"]
                    c.op("pe", lambda e, po=po, a=a, kt=kt, idx=idx, nkt=nkt: e.matmul(po.t[0:64, :], lhsT=vall.t[:, kt, h * 65:h * 65 + 64], rhs=a.t[:, :],
                                                                                     start=(idx == 0), stop=(idx == nkt - 1)), reads=[vall, a], writes=[po])
                    if idx == nkt - 1:
                        ast = ast_r.next()

                        def f1(po=po, ast=ast, Q=Q):
                            c.op("dve", lambda e: e.tensor_copy(ast.t[:, :], po.t[0:64, :]), reads=[po], writes=[ast])
                            c.dma("pool", [(attnT[h * 64:(h + 1) * 64, Q * 512:(Q + 1) * 512], ast.t[:, :])], reads=[ast])
                        fin.setdefault(step + 2, []).append(f1)
                    del T[j]
                j = step - 4
                if 0 <= j < len(tiles):
                    Q, idx, kt = tiles[j]
                    pt = T[j].pop("pt")
                    a = a_r.next()
                    c.op("act", lambda e, pt=pt, a=a: e.activation(out=a.t[:, :], in_=pt.t[:, :], func=AF.Exp, scale=-1.0), reads=[pt], writes=[a])
                    if kt >= 4 * Q:
                        o = 512 * Q - 128 * kt + 384
                        c.op("pool", lambda e, a=a, o=o: e.tensor_tensor(out=a.t[:, :], in0=a.t[:, :], in1=cm.t[:, o:o + 512], op=ALU.mult), reads=[a, cm], writes=[a])
                    T[j]["a"] = a
                    if T[j].pop("ck", False):
                        qs = qstate[Q]
                        tsb = tot_r.next()
                        ptot = qs["ptot"]
                        c.op("dve", lambda e, ptot=ptot, tsb=tsb: e.tensor_copy(tsb.t[:, :], ptot.t[:, :]), reads=[ptot], writes=[tsb])
                        qs["ck"][idx + 1] = tsb
                j = step - 3
                if 0 <= j < len(tiles):
                    Q, idx, kt = tiles[j]
                    nkt = 4 * Q + 4
                    ln = T[j].pop("ln")
                    lp = T[j].pop("lp")
                    if idx == 0:
                        qstate[Q] = {"ptot": pTOT.next(), "ck": {}, "po": pO.next(), "ln": {}}
                    qs = qstate[Q]
                    qs["ln"][idx] = ln
                    n2 = max(0, idx - 2)
                    direct = list(range(n2, idx))
                    pt = pT.next()
                    c.op("pe", lambda e, pt=pt, ln=ln: e.matmul(pt.t[:, :], lhsT=ustr.t[:, :], rhs=ln.t[:, :], start=True, stop=False), reads=[ustr, ln], writes=[pt])
                    for dj in direct:
                        lnd = qs["ln"][dj]
                        c.op("pe", lambda e, pt=pt, lnd=lnd: e.matmul(pt.t[:, :], lhsT=onesb.t[:, :], rhs=lnd.t[:, :], start=False, stop=False), reads=[onesb, lnd], writes=[pt])
                    if n2 > 0:
                        tsb = qs["ck"].pop(n2)
                        c.op("pe", lambda e, pt=pt, tsb=tsb: e.matmul(pt.t[:, :], lhsT=identb.t[:, :], rhs=tsb.t[:, :], start=False, stop=False), reads=[identb, tsb], writes=[pt])
                    c.op("pe", lambda e, pt=pt, lp=lp: e.matmul(pt.t[:, :], lhsT=identb.t[:, :], rhs=lp.t[:, :], start=False, stop=True), reads=[identb, lp], writes=[pt])
                    if idx <= nkt - 4:
                        ptot = qs["ptot"]
                        c.op("pe", lambda e, ptot=ptot, ln=ln, idx=idx: e.matmul(ptot.t[:, :], lhsT=onesb.t[:, :], rhs=ln.t[:, :], start=(idx == 0), stop=True, skip_group_check=True),
                             reads=[onesb, ln], writes=[ptot])
                        T[j]["ck"] = True
                    qs["ln"].pop(idx - 2, None)
                    T[j]["pt"] = pt
                j = step - 2
                if 0 <= j < len(tiles):
                    Q, idx, kt = tiles[j]
                    pz = T[j].pop("pz")
                    lp = T[j]["lp"]
                    ln = ln_r.next()
                    c.op("dve", lambda e, pz=pz, lp=lp, ln=ln: e.tensor_tensor(out=ln.t[:, :], in0=pz.t[:, :], in1=lp.t[:, :], op=ALU.add), reads=[pz, lp], writes=[ln])
                    if kt >= 4 * Q:
                        o = 512 * Q - 128 * kt + 384
                        c.op("pool", lambda e, ln=ln, o=o: e.tensor_tensor(out=ln.t[:, :], in0=ln.t[:, :], in1=cm.t[:, o:o + 512], op=ALU.mult), reads=[ln, cm], writes=[ln])
                    T[j]["ln"] = ln
                j = step - 1
                if 0 <= j < len(tiles):
                    pz = T[j]["pz"]
                    ee, lp = e_r.next(), lp_r.next()
                    c.op("act", lambda e, pz=pz, ee=ee: e.activation(out=ee.t[:, :], in_=pz.t[:, :], func=AF.Exp, scale=-1.0), reads=[pz], writes=[ee])
                    c.op("act", lambda e, ee=ee, lp=lp: e.activation(out=lp.t[:, :], in_=ee.t[:, :], func=AF.Ln, bias=1.0), reads=[ee], writes=[lp])
                    T[j]["lp"] = lp
                j = step
                if j < len(tiles):
                    Q, idx, kt = tiles[j]
                    pz = pZ.next()
                    c.op("pe", lambda e, pz=pz, kt=kt, Q=Q: e.matmul(pz.t[:, :], lhsT=kp.t[:, kt * 128:(kt + 1) * 128], rhs=qp.t[:, Q * 512:(Q + 1) * 512],
                                                                   start=True, stop=True), reads=[kp, qp], writes=[pz])
                    T[j] = {"pz": pz}
            for k_ in sorted(fin):
                for f_ in fin[k_]:
                    f_()
        c.end()

    def router_phase(rw2d):
        c.begin()
        rw = c.sbuf("rrw", [128, KC, E], BF16)
        c.dma("pool", [(rw.t[:, :, :], rw2d.rearrange("(k p) e -> p k e", p=128))], writes=[rw])
        ht_r = Rot([c.sbuf("rht0", [128, KC, 512], BF16), c.sbuf("rht1", [128, KC, 512], BF16)])
        cst = c.sbuf("rcst", [128, NT, E], F32)
        mk = lambda nm, w: Rot([c.sbuf(nm + "0", [128, w], F32), c.sbuf(nm + "1", [128, w], F32)])
        lg_r, t8_r, d_r, ex_r, w1_r, w2_r, c1_r, c2_r = mk("rlg", 8), mk("rt8", 8), mk("rd", 1), mk("rex", 1), mk("rw1", 1), mk("rw2", 1), mk("rc1", 8), mk("rc2", 8)
        pL = Rot([c.psum("rpL0", [128, 512], F32), c.psum("rpL1", [128, 512], F32)])
        for g in range(NG):
            ht = ht_r.next()
            c.dma("sp", [(ht.t[:, :, :], hT[:, g * 512:(g + 1) * 512].rearrange("(k p) s -> p k s", p=128))], writes=[ht])
            for tt in range(4):
                pl = pL.next()
                for kc in range(KC):
                    c.op("pe", lambda e, pl=pl, kc=kc, ht=ht, tt=tt: e.matmul(pl.t[:, 0:E], lhsT=ht.t[:, kc, tt * 128:(tt + 1) * 128], rhs=rw.t[:, kc, :], start=(kc == 0), stop=(kc == KC - 1)),
                         reads=[ht, rw], writes=[pl])
                lg, t8, d_, ex, w1, w2, c1, c2 = (r_.next() for r_ in (lg_r, t8_r, d_r, ex_r, w1_r, w2_r, c1_r, c2_r))
                c.op("dve", lambda e, pl=pl, lg=lg: e.tensor_copy(lg.t[:, :], pl.t[:, 0:E]), reads=[pl], writes=[lg])
                c.op("dve", lambda e, lg=lg, t8=t8: e.max(out=t8.t[:, :], in_=lg.t[:, :]), reads=[lg], writes=[t8])
                c.op("dve", lambda e, t8=t8, d_=d_: e.tensor_tensor(out=d_.t[:, :], in0=t8.t[:, 1:2], in1=t8.t[:, 0:1], op=ALU.subtract), reads=[t8], writes=[d_])
                c.op("act", lambda e, d_=d_, ex=ex: e.activation(out=ex.t[:, :], in_=d_.t[:, :], func=AF.Exp), reads=[d_], writes=[ex])
                c.op("dve", lambda e, ex=ex, w1=w1: e.tensor_scalar(out=w1.t[:, :], in0=ex.t[:, :], scalar1=1.0, scalar2=None, op0=ALU.add), reads=[ex], writes=[w1])
                c.op("dve", lambda e, w1=w1: e.reciprocal(out=w1.t[:, :], in_=w1.t[:, :]), reads=[w1], writes=[w1])
                c.op("dve", lambda e, w1=w1, ex=ex, w2=w2: e.tensor_tensor(out=w2.t[:, :], in0=w1.t[:, :], in1=ex.t[:, :], op=ALU.mult), reads=[w1, ex], writes=[w2])
                c.op("dve", lambda e, lg=lg, t8=t8, w1=w1, c1=c1: e.tensor_scalar(out=c1.t[:, :], in0=lg.t[:, :], scalar1=t8.t[:, 0:1], scalar2=w1.t[:, 0:1], op0=ALU.is_equal, op1=ALU.mult),
                     reads=[lg, t8, w1], writes=[c1])
                c.op("dve", lambda e, lg=lg, t8=t8, w2=w2, c2=c2: e.tensor_scalar(out=c2.t[:, :], in0=lg.t[:, :], scalar1=t8.t[:, 1:2], scalar2=w2.t[:, 0:1], op0=ALU.is_equal, op1=ALU.mult),
                     reads=[lg, t8, w2], writes=[c2])
                ti = g * 4 + tt
                c.op("dve", lambda e, c1=c1, c2=c2, ti=ti: e.tensor_tensor(out=cst.t[:, ti, :], in0=c1.t[:, :], in1=c2.t[:, :], op=ALU.add), reads=[c1, c2], writes=[cst])
        c.dma("sp", [(comb_tm[:, :].rearrange("(t p) e -> p t e", p=128), cst.t[:, :, :])], reads=[cst])
        c.end()

    def ffn_phase(experts, F_, gcol_row0, use_comb):
        c.begin()
        NF = F_ // 128
        NTT = TS // 128
        NTG = TS // 512
        PF = 2
        PW2 = 7
        hts = c.sbuf("fht", [128, KC, TS], BF16)
        actT = c.sbuf("fact", [128, NF, TS], BF16)
        acc = c.sbuf("facc", [128, NTT, D], F32)
        gb = c.sbuf("fgb", [128, D], F32)
        c.dma("sp", [(gb.t[:, :], vecscr[gcol_row0:gcol_row0 + KC, :].rearrange("(o k) p -> o (k p)", o=1).partition_broadcast(128))], writes=[gb])
        wg_r = Rot([c.sbuf("fwg0", [128, KC, 2 * PF * 128], BF16), c.sbuf("fwg1", [128, KC, 2 * PF * 128], BF16)])
        w2_r = Rot([c.sbuf("fw20", [128, PW2, D], BF16), c.sbuf("fw21", [128, PW2, D], BF16)])
        cbt = c.sbuf("fcbt", [128, NTT, E], F32)
        sg_r = Rot([c.sbuf(f"fsg{i}", [128, 512], F32) for i in range(3)])
        xt_r = Rot([c.sbuf("fx0", [128, D], F32), c.sbuf("fx1", [128, D], F32)])
        pGU = Rot([c.psum(f"fpg{i}", [128, 512], F32) for i in range(4)])
        pY = Rot([c.psum(f"fpy{i}", [128, 512], F32) for i in range(3)])
        for st in range(S // TS):
            t0 = st * TS
            c.dma("sp", [(hts.t[:, :, :], hT[:, t0:t0 + TS].rearrange("(k p) s -> p k s", p=128))], writes=[hts])
            if use_comb:
                c.dma("sp", [(cbt.t[:, :, :], comb_tm[t0:t0 + TS, :].rearrange("(t p) e -> p t e", p=128))], writes=[cbt])
            for ei, (w13, w2) in enumerate(experts):
                for f0 in range(0, NF, PF):
                    nf = min(PF, NF - f0)
                    wg = wg_r.next()
                    c.dma("pool", [(wg.t[:, :, 0:nf * 128], w13[:, f0 * 128:(f0 + nf) * 128].rearrange("(k p) f -> p k f", p=128)),
                                   (wg.t[:, :, PF * 128:PF * 128 + nf * 128], w13[:, F_ + f0 * 128:F_ + (f0 + nf) * 128].rearrange("(k p) f -> p k f", p=128))], writes=[wg])
                    for j in range(nf):
                        fc = f0 + j
                        for tg in range(NTG):
                            pg, pu = pGU.next(), pGU.next()
                            for kc in range(KC):
                                c.op("pe", lambda e, pg=pg, wg=wg, j=j, kc=kc, tg=tg: e.matmul(pg.t[:, :], lhsT=wg.t[:, kc, j * 128:(j + 1) * 128], rhs=hts.t[:, kc, tg * 512:(tg + 1) * 512],
                                                                                              start=(kc == 0), stop=(kc == KC - 1)), reads=[wg, hts], writes=[pg])
                            for kc in range(KC):
                                c.op("pe", lambda e, pu=pu, wg=wg, j=j, kc=kc, tg=tg: e.matmul(pu.t[:, :], lhsT=wg.t[:, kc, (PF + j) * 128:(PF + j + 1) * 128], rhs=hts.t[:, kc, tg * 512:(tg + 1) * 512],
                                                                                              start=(kc == 0), stop=(kc == KC - 1)), reads=[wg, hts], writes=[pu])
                            sg = sg_r.next()
                            c.op("act", lambda e, pg=pg, sg=sg: e.activation(out=sg.t[:, :], in_=pg.t[:, :], func=AF.Silu), reads=[pg], writes=[sg])
                            sgc = sg
                            c.op("dve", lambda e, pu=pu, sgc=sgc, fc=fc, tg=tg: e.tensor_tensor(out=actT.t[:, fc, tg * 512:(tg + 1) * 512], in0=pu.t[:, :], in1=sgc.t[:, :], op=ALU.mult),
                                 reads=[pu, sgc], writes=[actT])
                for p0 in range(0, NF, PW2):
                    npc = min(PW2, NF - p0)
                    w2s = w2_r.next()
                    c.dma("pool", [(w2s.t[:, 0:npc, :], w2[p0 * 128:(p0 + npc) * 128, :].rearrange("(f p) d -> p f d", p=128))], writes=[w2s])
                    first = (ei == 0 and p0 == 0)
                    for tt in range(NTT):
                        for half in range(D // 512):
                            py = pY.next()
                            for j in range(npc):
                                c.op("pe", lambda e, py=py, j=j, p0=p0, tt=tt, half=half, w2s=w2s, npc=npc: e.matmul(py.t[:, :], lhsT=actT.t[:, p0 + j, tt * 128:(tt + 1) * 128],
                                                                                                                rhs=w2s.t[:, j, half * 512:(half + 1) * 512], start=(j == 0), stop=(j == npc - 1)),
                                     reads=[actT, w2s], writes=[py])
                            asl = acc.t[:, tt, half * 512:(half + 1) * 512]
                            if use_comb:
                                csc = cbt.t[:, tt, ei:ei + 1]
                                if first:
                                    c.op("act", lambda e, py=py, asl=asl, csc=csc: e.activation(out=asl, in_=py.t[:, :], func=AF.Copy, scale=csc), reads=[py, cbt], writes=[acc])
                                else:
                                    c.op("dve", lambda e, py=py, asl=asl, csc=csc: e.scalar_tensor_tensor(out=asl, in0=py.t[:, :], scalar=csc, in1=asl, op0=ALU.mult, op1=ALU.add),
                                         reads=[py, acc, cbt], writes=[acc])
                            elif first:
                                c.op("act", lambda e, py=py, asl=asl: e.activation(out=asl, in_=py.t[:, :], func=AF.Copy), reads=[py], writes=[acc])
                            else:
                                c.op("dve", lambda e, py=py, asl=asl: e.tensor_tensor(out=asl, in0=py.t[:, :], in1=asl, op=ALU.add), reads=[py, acc], writes=[acc])
            for tt in range(NTT):
                xt = xt_r.next()
                c.dma("sp", [(xt.t[:, :], xres[t0 + tt * 128:t0 + (tt + 1) * 128, :])], writes=[xt])
                c.op("pool", lambda e, tt=tt: e.tensor_tensor(out=acc.t[:, tt, :], in0=acc.t[:, tt, :], in1=gb.t[:, :], op=ALU.mult), reads=[acc, gb], writes=[acc])
                c.op("pool", lambda e, tt=tt, xt=xt: e.tensor_tensor(out=xt.t[:, :], in0=xt.t[:, :], in1=acc.t[:, tt, :], op=ALU.add), reads=[acc, xt], writes=[xt])
                c.dma("pool", [(xres[t0 + tt * 128:t0 + (tt + 1) * 128, :], xt.t[:, :])], reads=[xt])
        c.end()

    def final_phase():
        c.begin()
        gb = c.sbuf("zgb", [128, D], F32)
        c.dma("sp", [(gb.t[:, :], final_norm_g[0:1, :].partition_broadcast(128))], writes=[gb])
        xt_r = Rot([c.sbuf("zx0", [128, 4, D], F32), c.sbuf("zx1", [128, 4, D], F32)])
        junk = c.sbuf("zjunk", [128, D], BF16)
        ss_r = Rot([c.sbuf("zss0", [128, 4], F32), c.sbuf("zss1", [128, 4], F32)])
        sq_r = Rot([c.sbuf("zsq0", [128, 4], F32), c.sbuf("zsq1", [128, 4], F32)])
        rs_r = Rot([c.sbuf("zrs0", [128, 4], F32), c.sbuf("zrs1", [128, 4], F32)])
        epsc = c.sbuf("zeps", [128, 1], F32)
        c.op("pool", lambda e: e.memset(epsc.t[:, :], EPS), writes=[epsc])
        for g in range(NG):
            xt = xt_r.next()
            c.dma("sp", [(xt.t[:, :, :], xres[g * 512:(g + 1) * 512, :].rearrange("(t p) d -> p t d", p=128))], writes=[xt])
            ss, sq, rs = ss_r.next(), sq_r.next(), rs_r.next()
            for t in range(4):
                c.op("act", lambda e, t=t, xt=xt, ss=ss: e.activation(out=junk.t[:, :], in_=xt.t[:, t, :], func=AF.Square, accum_out=ss.t[:, t:t + 1]), reads=[xt], writes=[junk, ss])
            c.op("act", lambda e, ss=ss, sq=sq: e.activation(out=sq.t[:, :], in_=ss.t[:, :], func=AF.Sqrt, scale=1.0 / D, bias=epsc.t[:, 0:1]), reads=[ss, epsc], writes=[sq])
            c.op("dve", lambda e, sq=sq, rs=rs: e.reciprocal(out=rs.t[:, :], in_=sq.t[:, :]), reads=[sq], writes=[rs])
            for t in range(4):
                c.op("dve", lambda e, t=t, xt=xt, rs=rs: e.scalar_tensor_tensor(out=xt.t[:, t, :], in0=xt.t[:, t, :], scalar=rs.t[:, t:t + 1], in1=gb.t[:, :], op0=ALU.mult, op1=ALU.mult),
                     reads=[xt, rs, gb], writes=[xt])
            c.dma("pool", [(out_d[g * 512:(g + 1) * 512, :].rearrange("(t p) d -> p t d", p=128), xt.t[:, :, :])], reads=[xt])
        c.end()

    qdst = [(qT, j * 128) for j in range(KC)]
    kdst = [(kT, j * 128) for j in range(KC)]
    for l in range(4):
        norm_phase(x_in if l == 0 else xres, l * KC, col_mod(l, 0), hT)
        if l < 2:
            proj_phase(hT, a_wqkv[l], 2 * D, D, qdst + kdst, v65, set(range(KC)))
            moba_phase()
            wo_phase(a_wo[l], col_mod(l, 2), x_in if l == 0 else xres)
        else:
            j = l - 2
            if j == 0:
                norm_phase(xres, 8 * KC, COL_KV, attnT)
                proj_phase(attnT, b_wkv, D, D, kdst, v65, set())
            proj_phase(hT, b_wq[j], D, 0, qdst, None, set(range(KC)))
            sb_phase()
            wo_phase(b_wo[j], col_mod(l, 2))
        norm_phase(xres, (4 + l) * KC, col_mod(l, 3), hT)
        if l % 2 == 0:
            ffn_phase([(ffn_w13[l // 2], ffn_w2[l // 2])], FF, col_mod(l, 5), False)
        else:
            router_phase(router_w[l // 2])
            ffn_phase([(moe_w13[l // 2, e], moe_w2[l // 2, e]) for e in range(E)], FE, col_mod(l, 5), True)
    final_phase()
    c.barrier()
    return c


_CACHE = {}


def kernel(**inputs):
    cfg = inputs.pop("_cfg", None) or CFG_FULL
    runner = inputs.pop("_runner", None)
    key = tuple(sorted(cfg.items()))
    if key not in _CACHE:
        _CACHE[key] = (build(cfg), make_consts(cfg))
    c, consts = _CACHE[key]
    S, D = cfg["S"], cfg["D"]
    x = np.asarray(inputs["x"], np.float32)
    B = x.shape[0]
    f = lambda k: np.ascontiguousarray(np.asarray(inputs[k], np.float32))
    shared = {
        "rel_bias": f("rel_bias"), "mod_w": f("mod_w"), "mod_b": f("mod_b").reshape(1, -1),
        "norm_mix_g": f("norm_mix_g"), "norm_ffn_g": f("norm_ffn_g"), "a_wqkv": f("a_wqkv"), "a_wo": f("a_wo"),
        "kv_norm_g": f("kv_norm_g").reshape(1, -1), "kv_mod_w": f("kv_mod_w"), "kv_mod_b": f("kv_mod_b").reshape(1, -1),
        "b_wkv": f("b_wkv"), "b_wq": f("b_wq"), "b_wo": f("b_wo"), "ffn_w13": f("ffn_w13"), "ffn_w2": f("ffn_w2"),
        "router_w": f("router_w"), "moe_w13": f("moe_w13"), "moe_w2": f("moe_w2"),
        "final_norm_g": f("final_norm_g").reshape(1, -1),
    }
    shared.update(consts)
    cc = np.asarray(inputs["c"], np.float32)
    in_maps = []
    for b in range(B):
        m = dict(shared)
        m["x"] = np.ascontiguousarray(x[b])
        m["c"] = np.ascontiguousarray(cc[b:b + 1])
        in_maps.append(m)
    if runner is not None:
        res = runner(c.nc, in_maps)
    else:
        res = run_bass_kernel_spmd(c.nc, in_maps, core_ids=list(range(B))).results
    return np.stack([np.asarray(r["out"], np.float32) for r in res], axis=0)
```
